# Optimizing a Trainium2 kernel written in Bass

```python
import jax, jax.numpy as jnp
from jax import lax
import numpy as np


D_MODEL = 1024
BATCH = 4
SEQ = 8192
DEPTH = 1

MLA_HEADS = 8
MLA_NOPE = 64
MLA_ROPE = 32
MLA_QK = MLA_NOPE + MLA_ROPE
MLA_V = 64
MLA_W = MLA_HEADS * MLA_V
MLA_Q_RANK = 256
MLA_KV_RANK = 128
ROPE_THETA = 10000.0
Q_BLOCK = 128

RWKV_HEADS = 8
RWKV_N = 64
RWKV_W = RWKV_HEADS * RWKV_N
DECAY_LORA = 64
AAA_LORA = 64
GATE_LORA = 128

MIX_W = MLA_W + RWKV_W
MLA_IN = MLA_Q_RANK + MLA_KV_RANK + MLA_ROPE
RWKV_IN = 3 * RWKV_W + DECAY_LORA + AAA_LORA + GATE_LORA
IN_W = MLA_IN + RWKV_IN

N_GROUPS = 4
EXPERTS_PER_GROUP = 8
N_EXPERTS = N_GROUPS * EXPERTS_PER_GROUP
TOP_K = 2
D_EXPERT = 256

NORM_EPS = 1e-6
LNX_EPS = 64e-5

kernel_name = 'hymba_mla_rwkv7_hier_moe_block'


def rms_norm(x, g, eps=NORM_EPS):
    xf = x.astype(jnp.float32)
    y = xf * lax.rsqrt(jnp.mean(xf * xf, axis=-1, keepdims=True) + eps)
    return (y * g.astype(jnp.float32)).astype(x.dtype)


def rope_tables(positions):
    half = MLA_ROPE // 2
    inv_freq = 1.0 / (ROPE_THETA ** (jnp.arange(half, dtype=jnp.float32) / half))
    ang = positions.astype(jnp.float32)[..., None] * inv_freq
    return jnp.cos(ang)[:, :, None, :], jnp.sin(ang)[:, :, None, :]


def apply_rope(x, cos, sin):
    half = MLA_ROPE // 2
    xf = x.astype(jnp.float32)
    x1, x2 = xf[..., :half], xf[..., half:]
    return jnp.concatenate([x1 * cos - x2 * sin, x2 * cos + x1 * sin], axis=-1).astype(x.dtype)


def mla_group(c_q, c_kv, k_rope, positions, g_cq, w_uq, g_ckv, w_uk, w_uv, g_qn, g_kn):
    bsz, s, _ = c_q.shape
    q = jnp.einsum('bsr,rhd->bshd', rms_norm(c_q, g_cq), w_uq)
    ckv = rms_norm(c_kv, g_ckv)
    k_nope = jnp.einsum('bsr,rhd->bshd', ckv, w_uk)
    v = jnp.einsum('bsr,rhd->bshd', ckv, w_uv)
    k_rope_h = jnp.broadcast_to(k_rope[:, :, None, :], (bsz, s, MLA_HEADS, MLA_ROPE))
    k = jnp.concatenate([k_nope, k_rope_h], axis=-1)
    q = rms_norm(q, g_qn)
    k = rms_norm(k, g_kn)
    cos, sin = rope_tables(positions)
    q = jnp.concatenate([q[..., :MLA_NOPE], apply_rope(q[..., MLA_NOPE:], cos, sin)], axis=-1)
    k = jnp.concatenate([k[..., :MLA_NOPE], apply_rope(k[..., MLA_NOPE:], cos, sin)], axis=-1)
    scale = MLA_QK ** -0.5
    outs = []
    for blk in range(s // Q_BLOCK):
        q0 = blk * Q_BLOCK
        kend = q0 + Q_BLOCK
        scores = jnp.einsum('bqhd,bkhd->bhqk', q[:, q0:kend], k[:, :kend]).astype(jnp.float32) * scale
        causal = (q0 + jnp.arange(Q_BLOCK))[:, None] >= jnp.arange(kend)[None, :]
        scores = jnp.where(causal, scores, -jnp.inf)
        probs = jax.nn.softmax(scores, axis=-1).astype(v.dtype)
        outs.append(jnp.einsum('bhqk,bkhd->bqhd', probs, v[:, :kend]))
    return jnp.concatenate(outs, axis=1).reshape(bsz, s, MLA_W)


def rwkv7_group(feat, mu, w0, w2, a0, a2, g2, k_k, k_a, r_k, lnx_g, lnx_b):
    bsz, s, _ = feat.shape
    prev = jnp.pad(feat, ((0, 0), (1, 0), (0, 0)))[:, :-1]
    feat = feat + (prev - feat) * mu
    splits = np.cumsum([RWKV_W, RWKV_W, RWKV_W, DECAY_LORA, AAA_LORA]).tolist()
    r, k, v, wl, al, gl = jnp.split(feat, splits, axis=-1)
    w = -jax.nn.softplus(-(w0 + jnp.tanh(wl) @ w2)) - 0.5
    a = jax.nn.sigmoid(a0 + al @ a2)
    g = jax.nn.sigmoid(gl) @ g2
    heads = lambda t: t.reshape(bsz, s, RWKV_HEADS, RWKV_N).astype(jnp.float32)
    kk = heads(k * k_k)
    kk = kk * lax.rsqrt(jnp.sum(kk * kk, axis=-1, keepdims=True) + 1e-12)
    k = k * (1.0 + (a - 1.0) * k_a)
    rh, kh, vh, ah = heads(r), heads(k), heads(v), heads(a)
    decay = jnp.exp(-jnp.exp(heads(w)))

    def step(state, inp):
        r_t, k_t, v_t, kk_t, b_t, d_t = inp
        sa = jnp.einsum('bhvk,bhk->bhv', state, -kk_t)
        state = (state * d_t[:, :, None, :] + sa[..., None] * b_t[:, :, None, :]
                 + v_t[..., None] * k_t[:, :, None, :])
        return state, jnp.einsum('bhvk,bhk->bhv', state, r_t)

    tm = lambda t: jnp.swapaxes(t, 0, 1)
    state0 = jnp.zeros((bsz, RWKV_HEADS, RWKV_N, RWKV_N), jnp.float32)
    _, y = lax.scan(step, state0, (tm(rh), tm(kh), tm(vh), tm(kk), tm(kk * ah), tm(decay)))
    y = tm(y)
    mean = jnp.mean(y, axis=-1, keepdims=True)
    var = jnp.mean(jnp.square(y - mean), axis=-1, keepdims=True)
    y = ((y - mean) * lax.rsqrt(var + LNX_EPS)).reshape(bsz, s, RWKV_W)
    y = y * lnx_g.astype(jnp.float32) + lnx_b.astype(jnp.float32)
    bonus = jnp.sum(rh * kh * r_k.astype(jnp.float32), axis=-1, keepdims=True) * vh
    y = y + bonus.reshape(bsz, s, RWKV_W)
    return (y * g.astype(jnp.float32)).astype(feat.dtype)


def hier_moe(h, w_group, b_group, w_expert, b_expert, w_gate, w_up, w_down):
    bsz, s, d = h.shape
    t = h.reshape(bsz * s, d)
    n = t.shape[0]
    rows = jnp.arange(n)
    group_logits = (t @ w_group).astype(jnp.float32) + b_group.astype(jnp.float32)
    group_probs = jax.nn.softmax(group_logits, axis=-1)
    _, gsel = lax.top_k(group_logits, 1)
    gsel = gsel[:, 0]
    expert_logits = ((t @ w_expert).astype(jnp.float32) + b_expert.astype(jnp.float32)).reshape(
        n, N_GROUPS, EXPERTS_PER_GROUP)
    in_group = expert_logits[rows, gsel]
    top_vals, top_idx = lax.top_k(in_group, TOP_K)
    weights = jax.nn.softmax(top_vals, axis=-1) * group_probs[rows, gsel][:, None]
    expert_ids = gsel[:, None] * EXPERTS_PER_GROUP + top_idx
    combine = jnp.sum(jax.nn.one_hot(expert_ids, N_EXPERTS, dtype=jnp.float32) * weights[..., None],
                      axis=1).astype(t.dtype)
    y = jnp.zeros_like(t)
    for e in range(N_EXPERTS):
        hidden = jax.nn.silu(t @ w_gate[e]) * (t @ w_up[e])
        y = y + combine[:, e:e + 1] * (hidden @ w_down[e])
    return y.reshape(bsz, s, d)


def hybrid_layer(x, positions, g_mix, w_in, g_cq, w_uq, g_ckv, w_uk, w_uv, g_qn, g_kn, g_mla_out,
                 rw_mu, rw_w0, rw_w2, rw_a0, rw_a2, rw_g2, rw_k_k, rw_k_a, rw_r_k, rw_lnx_g, rw_lnx_b,
                 w_o, g_ffn, w_group, b_group, w_expert, b_expert, w_gate, w_up, w_down):
    h = rms_norm(x, g_mix)
    proj = h @ w_in
    c_q = proj[..., :MLA_Q_RANK]
    c_kv = proj[..., MLA_Q_RANK:MLA_Q_RANK + MLA_KV_RANK]
    k_rope = proj[..., MLA_Q_RANK + MLA_KV_RANK:MLA_IN]
    feat = proj[..., MLA_IN:]
    mla_out = rms_norm(mla_group(c_q, c_kv, k_rope, positions, g_cq, w_uq, g_ckv, w_uk, w_uv, g_qn, g_kn),
                       g_mla_out)
    rw_out = rwkv7_group(feat, rw_mu, rw_w0, rw_w2, rw_a0, rw_a2, rw_g2, rw_k_k, rw_k_a, rw_r_k,
                         rw_lnx_g, rw_lnx_b)
    x = x + jnp.concatenate([mla_out, rw_out], axis=-1) @ w_o
    x = x + hier_moe(rms_norm(x, g_ffn), w_group, b_group, w_expert, b_expert, w_gate, w_up, w_down)
    return x


def setup_inputs(seed: int = 0) -> dict:
    key = jax.random.key(seed)
    ks = iter(jax.random.split(key, 40))
    L = DEPTH

    def nrm(shape, scale):
        return jax.random.normal(next(ks), shape, jnp.float32) * scale

    def gain(shape):
        return 1.0 + nrm(shape, 0.02)

    x = nrm((BATCH, SEQ, D_MODEL), 1.0)
    positions = (jnp.arange(SEQ, dtype=jnp.int32)[None, :]
                 + jax.random.randint(next(ks), (BATCH, 1), 0, 1024, jnp.int32))
    return {
        'x': x,
        'positions': positions,
        'g_mix': gain((L, D_MODEL)),
        'w_in': nrm((L, D_MODEL, IN_W), D_MODEL ** -0.5),
        'g_cq': gain((L, MLA_Q_RANK)),
        'w_uq': nrm((L, MLA_Q_RANK, MLA_HEADS, MLA_QK), MLA_Q_RANK ** -0.5),
        'g_ckv': gain((L, MLA_KV_RANK)),
        'w_uk': nrm((L, MLA_KV_RANK, MLA_HEADS, MLA_NOPE), MLA_KV_RANK ** -0.5),
        'w_uv': nrm((L, MLA_KV_RANK, MLA_HEADS, MLA_V), MLA_KV_RANK ** -0.5),
        'g_qn': gain((L, MLA_QK)),
        'g_kn': gain((L, MLA_QK)),
        'g_mla_out': gain((L, MLA_W)),
        'rw_mu': jax.random.uniform(next(ks), (L, RWKV_IN), jnp.float32, 0.0, 1.0),
        'rw_w0': jax.random.uniform(next(ks), (L, RWKV_W), jnp.float32, -4.0, 2.0),
        'rw_w2': nrm((L, DECAY_LORA, RWKV_W), 0.5 * DECAY_LORA ** -0.5),
        'rw_a0': nrm((L, RWKV_W), 0.1),
        'rw_a2': nrm((L, AAA_LORA, RWKV_W), 0.5 * AAA_LORA ** -0.5),
        'rw_g2': nrm((L, GATE_LORA, RWKV_W), GATE_LORA ** -0.5),
        'rw_k_k': 0.85 + nrm((L, RWKV_W), 0.02),
        'rw_k_a': 1.0 + nrm((L, RWKV_W), 0.02),
        'rw_r_k': nrm((L, RWKV_HEADS, RWKV_N), 0.1),
        'rw_lnx_g': gain((L, RWKV_W)),
        'rw_lnx_b': nrm((L, RWKV_W), 0.01),
        'w_o': nrm((L, MIX_W, D_MODEL), MIX_W ** -0.5),
        'g_ffn': gain((L, D_MODEL)),
        'w_group': nrm((L, D_MODEL, N_GROUPS), D_MODEL ** -0.5),
        'b_group': nrm((L, N_GROUPS), 0.01),
        'w_expert': nrm((L, D_MODEL, N_EXPERTS), D_MODEL ** -0.5),
        'b_expert': nrm((L, N_EXPERTS), 0.01),
        'w_gate': nrm((L, N_EXPERTS, D_MODEL, D_EXPERT), D_MODEL ** -0.5),
        'w_up': nrm((L, N_EXPERTS, D_MODEL, D_EXPERT), D_MODEL ** -0.5),
        'w_down': nrm((L, N_EXPERTS, D_EXPERT, D_MODEL), D_EXPERT ** -0.5),
    }


def reference(x, positions, g_mix, w_in, g_cq, w_uq, g_ckv, w_uk, w_uv, g_qn, g_kn, g_mla_out,
              rw_mu, rw_w0, rw_w2, rw_a0, rw_a2, rw_g2, rw_k_k, rw_k_a, rw_r_k, rw_lnx_g, rw_lnx_b,
              w_o, g_ffn, w_group, b_group, w_expert, b_expert, w_gate, w_up, w_down):
    for l in range(DEPTH):
        x = hybrid_layer(x, positions, g_mix[l], w_in[l], g_cq[l], w_uq[l], g_ckv[l], w_uk[l], w_uv[l],
                         g_qn[l], g_kn[l], g_mla_out[l], rw_mu[l], rw_w0[l], rw_w2[l], rw_a0[l],
                         rw_a2[l], rw_g2[l], rw_k_k[l], rw_k_a[l], rw_r_k[l], rw_lnx_g[l], rw_lnx_b[l],
                         w_o[l], g_ffn[l], w_group[l], b_group[l], w_expert[l], b_expert[l],
                         w_gate[l], w_up[l], w_down[l])
    return x
```

```python
import math
from contextlib import ExitStack

import numpy as np
import concourse.bass as bass
import concourse.mybir as mybir
from concourse.bass_utils import run_bass_kernel_spmd

F32 = mybir.dt.float32
BF16 = mybir.dt.bfloat16
I32 = mybir.dt.int32
AF = mybir.ActivationFunctionType
ALU = mybir.AluOpType
AX = mybir.AxisListType

D = 1024
NH = 8
QK = 96
NOPE = 64
ROPE = 32
INW = 2208
NE = 32
DE = 256
C0 = -math.exp(-0.5)


class Trk:
    __slots__ = ("w", "r", "dsem", "dcnt", "q")

    def __init__(self):
        self.w = None
        self.r = {}
        self.dsem = None
        self.dcnt = 0
        self.q = None


class Prog:
    ENG = ("pe", "act", "dve", "pool", "sp")

    def __init__(self, nc):
        self.nc = nc
        self.streams = {e: [] for e in self.ENG}
        self.cnt = {e: 0 for e in self.ENG}
        self.sems = {}
        for e in ("pe", "act", "dve", "pool"):
            self.sems[e] = nc.alloc_semaphore(name="s_" + e)
        self.seen = {e: {} for e in self.ENG}
        self.ndsem = 0
        self.dtrk = []
        self.pending = {e: {} for e in self.ENG}

    def _need(self, eng, waits, key, val):
        if val <= 0 or self.seen[eng].get(key, 0) >= val:
            return
        if key == eng and val > self.cnt[eng]:
            return
        if waits.get(key, 0) < val:
            waits[key] = val

    def _deps(self, eng, reads, writes):
        waits = {}
        if self.pending[eng]:
            for k, v in self.pending[eng].items():
                self._need(eng, waits, k, v)
            self.pending[eng] = {}
        for t in reads:
            if t.w is not None:
                self._need(eng, waits, t.w[0], t.w[1])
        for t in writes:
            if t.w is not None:
                self._need(eng, waits, t.w[0], t.w[1])
            for k, v in t.r.items():
                self._need(eng, waits, k, v)
        for k, v in waits.items():
            self.seen[eng][k] = v
        return waits

    @staticmethod
    def _flat(ts):
        o = []
        for t in ts:
            if isinstance(t, (list, tuple)):
                o.extend(Prog._flat(t))
            else:
                o.append(t)
        return o

    def op(self, eng, fn, reads=(), writes=(), sig=True):
        reads = self._flat(reads)
        writes = self._flat(writes)
        waits = self._deps(eng, reads, writes)
        if sig:
            self.cnt[eng] += 1
            val = self.cnt[eng]
        else:
            val = self.cnt[eng] + 1
        for t in reads:
            if t.r.get(eng, 0) < val:
                t.r[eng] = val
        for t in writes:
            t.w = (eng, val)
            t.r = {}
        self.streams[eng].append((fn, waits, (eng, 1) if sig else None))

    def dma(self, q, out, in_, sb, load, **kw):
        if sb.q is None:
            sb.q = q
        q = sb.q
        if sb.dsem is None:
            sb.dsem = self.nc.alloc_semaphore(name="d%d" % self.ndsem)
            self.ndsem += 1
            self.dtrk.append(sb)
        key = ("d", id(sb))
        self.sems[key] = sb.dsem
        ex = self._flat(kw.pop("extra", []))
        reads, writes = (list(ex), [sb]) if load else ([sb], list(ex))
        if not load:
            reads, writes = [sb] + [], []
            reads = [sb]
            reads = [sb] + list(ex)
        else:
            writes = [sb] + list(ex)
            reads = []
        waits = self._deps(q, reads, writes)
        sb.dcnt += 16
        val = sb.dcnt
        for t in writes:
            t.w = (key, val)
            t.r = {}
        for t in reads:
            if t.r.get(key, 0) < val:
                t.r[key] = val

        def fn(e, out=out, in_=in_, kw=kw):
            return e.dma_start(out=out, in_=in_, **kw)
        self.streams[q].append((fn, waits, (key, 16)))

    def barrier(self):
        waits = {}
        for e in ("pe", "act", "dve", "pool"):
            if self.cnt[e] > 0:
                waits[e] = self.cnt[e]
        for t in self.dtrk:
            waits[("d", id(t))] = t.dcnt
        for e in self.ENG:
            for k, v in waits.items():
                if self.pending[e].get(k, 0) < v:
                    self.pending[e][k] = v

    def final_wait(self, eng="sp"):
        waits = {}
        for t in self.dtrk:
            self._need(eng, waits, ("d", id(t)), t.dcnt)

        def fn(e, waits=waits):
            for k, v in waits.items():
                e.wait_ge(self.sems[k], v)
            return None
        self.streams[eng].append((fn, {}, None))

    def emit(self, block):
        engmap = {"pe": "tensor", "act": "scalar", "dve": "vector", "pool": "gpsimd", "sp": "sync"}
        for e in self.ENG:
            stream = self.streams[e]
            if not stream:
                continue

            def body(engobj, stream=stream):
                for fn, waits, inc in stream:
                    for k, v in waits.items():
                        engobj.wait_ge(self.sems[k], v)
                    ins = fn(engobj)
                    if inc is not None:
                        ins.then_inc(self.sems[inc[0]], inc[1])
            getattr(block, engmap[e])(body)


WNAMES = [
    ("g_mix", [D]), ("w_in", [D, INW]), ("g_cq", [256]), ("w_uq", [256, 768]), ("g_ckv", [128]),
    ("w_uk", [128, 512]), ("w_uv", [128, 512]), ("g_qn", [QK]), ("g_kn", [QK]), ("g_mla_out", [512]),
    ("rw_mu", [1792]), ("rw_w0", [512]), ("rw_w2", [64, 512]), ("rw_a0", [512]), ("rw_a2", [64, 512]),
    ("rw_g2", [128, 512]), ("rw_k_k", [512]), ("rw_k_a", [512]), ("rw_r_k", [512]), ("rw_lnx_g", [512]),
    ("rw_lnx_b", [512]), ("w_o", [D, D]), ("g_ffn", [D]), ("w_group", [D, 4]), ("b_group", [4]),
    ("w_expert", [D, NE]), ("b_expert", [NE]), ("w_gate", [NE, D, DE]), ("w_up", [NE, D, DE]),
    ("w_down", [NE, DE, D]),
]


def build(NT, dbg=False):
    S = NT * 128
    OWN0 = NT // 2
    SO = (NT - OWN0) * 128
    nc = bass.Bass("TRN2", target_bir_lowering=False)
    dr = {}
    dr["x"] = nc.dram_tensor("x", [S, D], F32, kind="ExternalInput").ap()
    dr["pos"] = nc.dram_tensor("pos", [S], I32, kind="ExternalInput").ap()
    for n, shp in WNAMES:
        dr[n] = nc.dram_tensor(n, shp, F32, kind="ExternalInput").ap()
    dr["kbias"] = nc.dram_tensor("kbias", [128, NT], F32, kind="ExternalInput").ap()
    out = nc.dram_tensor("out", [SO, D], F32, kind="ExternalOutput").ap()
    xmid = nc.dram_tensor("xmid", [SO, D], F32, kind="Internal").ap()
    dbgo = {}
    if dbg:
        for n, w in (("d_mla", 512), ("d_rw", 512), ("d_xmid", 1024)):
            dbgo[n] = nc.dram_tensor(n, [SO, w], F32, kind="ExternalOutput").ap()
    P = Prog(nc)
    x = dr["x"]

    with ExitStack() as es:
        mes = ExitStack()
        cures = {"es": es}

        def sb(name, shape, dt=F32):
            return cures["es"].enter_context(nc.sbuf_tensor(name, shape, dt))

        banks = [es.enter_context(nc.psum_tensor("bank%d" % i, [128, 512], F32)) for i in range(8)]
        btrk = [Trk() for _ in range(8)]
        bstate = {"i": 0}

        def bank():
            i = bstate["i"]
            bstate["i"] = (i + 1) % 6
            return banks[i], btrk[i]

        def mm(o, lhsT, rhs, start, stop, reads, writes, sig=None):
            P.op("pe", lambda e: e.matmul(o, lhsT=lhsT, rhs=rhs, start=start, stop=stop,
                                          skip_group_check=True), reads, writes, sig=(stop if sig is None else sig))

        def tr(o, in_, ident, reads, writes, sig=True):
            P.op("pe", lambda e: e.transpose(out=o, in_=in_, identity=ident), reads, writes, sig=sig)

        def cp(eng, o, i, reads, writes):
            if eng == "act":
                P.op("act", lambda e: e.activation(out=o, in_=i, func=AF.Copy), reads, writes)
            else:
                P.op(eng, lambda e: e.tensor_copy(out=o, in_=i), reads, writes)

        def tt(eng, o, a, b, op_, reads, writes):
            P.op(eng, lambda e: e.tensor_tensor(out=o, in0=a, in1=b, op=op_), reads, writes)

        def ts(eng, o, a, s1, op0, reads, writes, s2=None, op1=None):
            if op1 is None:
                P.op(eng, lambda e: e.tensor_scalar(out=o, in0=a, scalar1=s1, scalar2=None, op0=op0), reads, writes)
            else:
                P.op(eng, lambda e: e.tensor_scalar(out=o, in0=a, scalar1=s1, scalar2=s2, op0=op0, op1=op1), reads, writes)

        def stt(o, a, s, b, op0, op1, reads, writes):
            P.op("dve", lambda e: e.scalar_tensor_tensor(out=o, in0=a, scalar=s, in1=b, op0=op0, op1=op1), reads, writes)

        def act(o, i, func, reads, writes, scale=None, bias=None, accum=None):
            kw = {}
            if scale is not None:
                kw["scale"] = scale
            if bias is not None:
                kw["bias"] = bias
            if accum is not None:
                kw["accum_out"] = accum
            P.op("act", lambda e: e.activation(out=o, in_=i, func=func, **kw), reads, writes)

        def rsqrt_inplace(v, tv, mult, eps):
            ts("dve", v, v, mult, ALU.mult, [tv], [tv], s2=eps, op1=ALU.add)
            act(v, v, AF.Sqrt, [tv], [tv])
            P.op("dve", lambda e: e.reciprocal(out=v, in_=v), [tv], [tv])

        ones_f = sb("ones_f", [128, 512], BF16); t_ones = Trk()
        P.op("pool", lambda e: e.memset(ones_f[:], 1.0), [], [t_ones])
        ones_c = sb("ones_c", [128, 1])
        P.op("pool", lambda e: e.memset(ones_c[:], 1.0), [], [t_ones])
        ident_f = sb("ident_f", [128, 128]); ident_b = sb("ident_b", [128, 128], BF16); t_id = Trk()
        P.op("pool", lambda e: e.affine_select(out=ident_f[:], in_=ones_f[:, 0:128], pattern=[[-1, 128]],
                                               compare_op=ALU.is_equal, fill=0.0, base=0, channel_multiplier=1),
             [t_ones], [t_id])
        cp("dve", ident_b[:], ident_f[:], [t_id], [t_id])
        cures["es"] = mes
        tri_f = sb("tri_f", [128, 128]); t_tri = Trk()
        P.op("pool", lambda e: e.affine_select(out=tri_f[:], in_=ones_f[:, 0:128], pattern=[[1, 128]],
                                               compare_op=ALU.is_ge, fill=0.0, base=0, channel_multiplier=-1),
             [t_ones], [t_tri])
        P.op("pool", lambda e: e.memset(tri_f[0:64, 64:128], 0.0), [], [t_tri])
        sel63 = sb("sel63", [128, 64]); sel127 = sb("sel127", [128, 64]); t_sel = Trk()
        P.op("pool", lambda e: e.affine_select(out=sel63[:], in_=ones_f[:, 0:64], pattern=[[0, 64]],
                                               compare_op=ALU.is_equal, fill=0.0, base=-63, channel_multiplier=1),
             [t_ones], [t_sel])
        P.op("pool", lambda e: e.affine_select(out=sel127[:], in_=ones_f[:, 0:64], pattern=[[0, 64]],
                                               compare_op=ALU.is_equal, fill=0.0, base=-127, channel_multiplier=1),
             [t_ones], [t_sel])
        shift_hi = sb("shift_hi", [128, 64], BF16); t_sh = Trk()
        P.op("pool", lambda e: e.affine_select(out=shift_hi[:], in_=ones_f[:, 0:64], pattern=[[-1, 64]],
                                               compare_op=ALU.is_equal, fill=0.0, base=-64, channel_multiplier=1),
             [t_ones], [t_sh])
        m_su = sb("m_su", [64, 8, 64], BF16); m_ui = sb("m_ui", [64, 8, 64], BF16); m_sl = sb("m_sl", [64, 8, 64], BF16); t_msk = Trk()
        ones3 = ones_f[0:64, :].rearrange("p (h t) -> p h t", h=8)
        P.op("pool", lambda e: e.affine_select(out=m_su[:], in_=ones3, pattern=[[0, 8], [1, 64]],
                                               compare_op=ALU.is_gt, fill=0.0, base=0, channel_multiplier=-1),
             [t_ones], [t_msk])
        P.op("pool", lambda e: e.affine_select(out=m_ui[:], in_=ones3, pattern=[[0, 8], [1, 64]],
                                               compare_op=ALU.is_ge, fill=0.0, base=0, channel_multiplier=-1),
             [t_ones], [t_msk])
        P.op("pool", lambda e: e.affine_select(out=m_sl[:], in_=ones3, pattern=[[0, 8], [-1, 64]],
                                               compare_op=ALU.is_gt, fill=0.0, base=0, channel_multiplier=1),
             [t_ones], [t_msk])
        id8 = sb("id8", [64, 8, 64], BF16); t_id8 = Trk()
        P.op("pool", lambda e: e.affine_select(out=id8[:], in_=ones3, pattern=[[0, 8], [-1, 64]],
                                               compare_op=ALU.is_equal, fill=0.0, base=0, channel_multiplier=1),
             [t_ones], [t_id8])
        sel_lo_hi = sb("sel_lo_hi", [64, 128]); t_selhi = Trk()
        P.op("pool", lambda e: e.affine_select(out=sel_lo_hi[:], in_=ones_f[0:64, 0:128], pattern=[[1, 128]],
                                               compare_op=ALU.is_equal, fill=0.0, base=-64, channel_multiplier=-1),
             [t_ones], [t_selhi])
        jidx = sb("jidx", [128, 16]); invf = sb("invf", [128, 16]); t_invf = Trk()
        P.op("pool", lambda e: e.iota(jidx[:], pattern=[[1, 16]], base=0, channel_multiplier=0,
                                      allow_small_or_imprecise_dtypes=True), [], [t_invf])
        act(invf[:], jidx[:], AF.Exp, [t_invf], [t_invf], scale=-math.log(10000.0) / 16.0)

        t_vec = Trk()

        def bc_load(name, src, n):
            tile_ = sb(name, [128, n])
            trk = Trk()
            P.dma("sp", tile_[:], src.partition_broadcast(128), trk, True)
            return tile_, trk

        def col_load(name, src, ncol):
            tile_ = sb(name, [128, ncol])
            trk = Trk()
            P.dma("sp", tile_[:], src.rearrange("(c p) -> p c", p=128), trk, True,
                  allow_slow_non_contiguous=True)
            return tile_, trk

        gmix_c, t_gmix = col_load("gmix_c", dr["g_mix"], 8)
        gcq_c, t_gcq = col_load("gcq_c", dr["g_cq"], 2)
        gckv_c, t_gckv = col_load("gckv_c", dr["g_ckv"], 1)
        gmo_c, t_gmo = col_load("gmo_c", dr["g_mla_out"], 4)
        mu_bc, t_mu = bc_load("mu_bc", dr["rw_mu"][0:1536], 1536)
        mul_c = sb("mul_c", [128, 3]); t_mul = Trk()
        P.dma("sp", mul_c[0:64, 0:1], dr["rw_mu"][1536:1600].rearrange("(p o) -> p o", o=1), t_mul, True)
        P.dma("sp", mul_c[0:64, 1:2], dr["rw_mu"][1600:1664].rearrange("(p o) -> p o", o=1), t_mul, True)
        P.dma("sp", mul_c[:, 2:3], dr["rw_mu"][1664:1792].rearrange("(p o) -> p o", o=1), t_mul, True)
        w0_bc, t_w0 = bc_load("w0_bc", dr["rw_w0"], 512)
        a0_bc, t_a0 = bc_load("a0_bc", dr["rw_a0"], 512)
        kk_bc, t_kk = bc_load("kk_bc", dr["rw_k_k"], 512)
        ka_bc, t_ka = bc_load("ka_bc", dr["rw_k_a"], 512)
        rk_bc, t_rk = bc_load("rk_bc", dr["rw_r_k"], 512)
        lng_bc, t_lng = bc_load("lng_bc", dr["rw_lnx_g"], 512)
        lnb_bc, t_lnb = bc_load("lnb_bc", dr["rw_lnx_b"], 512)
        gqn_bc, t_gqn = bc_load("gqn_bc", dr["g_qn"], QK)
        gkn_bc, t_gkn = bc_load("gkn_bc", dr["g_kn"], QK)
        gqk_bc = sb("gqk_bc", [128, 64]); t_gqk = Trk()
        tt("dve", gqk_bc[:], gqn_bc[:, 0:64], gkn_bc[:, 0:64], ALU.mult, [t_gqn, t_gkn], [t_gqk])

        NPG = 31
        AR = sb("arena", [128, NPG * 512])
        ARb = AR[:].bitcast(BF16)
        pg_t = [Trk() for _ in range(NPG)]

        def af(p0, ncols, rows=128):
            npg = (ncols + 511) // 512
            return AR[0:rows, p0 * 512:p0 * 512 + ncols], pg_t[p0:p0 + npg]

        def ab(p0, off, ncols, rows=128):
            c0 = p0 * 1024 + off
            p1 = (c0 + ncols - 1) // 1024
            return ARb[0:rows, c0:c0 + ncols], pg_t[c0 // 1024:p1 + 1]

        stage = [AR[:, 0:2208], AR[:, 9 * 512:9 * 512 + 2208]]
        t_stage = [pg_t[0:5], pg_t[9:14]]
        sstate = {"i": 0}

        def load_scaled(dst, dst_trk, src_ap, ncols, rows=128, scale_col=None, scale_trk=None, q="sp"):
            i = sstate["i"]
            sstate["i"] = 1 - i
            st, tst = stage[i], t_stage[i]
            P.dma(q, st[0:rows, 0:ncols], src_ap, tst[0], True, extra=tst[1:])
            if scale_col is None:
                cp("dve", dst, st[0:rows, 0:ncols], [tst], [dst_trk])
            else:
                ts("dve", dst, st[0:rows, 0:ncols], scale_col, ALU.mult, [tst, scale_trk], [dst_trk])

        Win = sb("Win", [128, 8, INW], BF16); t_Win = Trk()
        for c in range(8):
            load_scaled(Win[:, c, :], t_Win, dr["w_in"][c * 128:(c + 1) * 128, :], INW,
                        scale_col=gmix_c[:, c:c + 1], scale_trk=t_gmix, q=("sp" if c % 2 == 0 else "pool"))
        Wuq = sb("Wuq", [128, 2, 768], BF16); t_Wuq = Trk()
        for c in range(2):
            load_scaled(Wuq[:, c, :], t_Wuq, dr["w_uq"][c * 128:(c + 1) * 128, :], 768,
                        scale_col=gcq_c[:, c:c + 1], scale_trk=t_gcq)
        Wuk = sb("Wuk", [128, 512], BF16); t_Wuk = Trk()
        load_scaled(Wuk[:], t_Wuk, dr["w_uk"][:, :], 512, scale_col=gckv_c[:, 0:1], scale_trk=t_gckv)
        Wuv = sb("Wuv", [128, 512], BF16); t_Wuv = Trk()
        load_scaled(Wuv[:], t_Wuv, dr["w_uv"][:, :], 512, scale_col=gckv_c[:, 0:1], scale_trk=t_gckv)
        W2 = sb("W2", [64, 512], BF16); t_W2 = Trk()
        load_scaled(W2[:], t_W2, dr["rw_w2"][:, :], 512, rows=64)
        A2 = sb("A2", [64, 512], BF16); t_A2 = Trk()
        load_scaled(A2[:], t_A2, dr["rw_a2"][:, :], 512, rows=64)
        G2 = sb("G2", [128, 512], BF16); t_G2 = Trk()
        load_scaled(G2[:], t_G2, dr["rw_g2"][:, :], 512)
        WukT = sb("WukT", [64, 8, 128], BF16); t_WukT = Trk()
        bk, tb = bank()
        bkb = bk[:].bitcast(BF16)
        for h in range(8):
            tr(bkb[0:64, h * 128:(h + 1) * 128], Wuk[:, h * 64:(h + 1) * 64], ident_b[:], [t_Wuk, t_id], [tb], sig=(h == 7))
        cp("dve", WukT[:].rearrange("p h r -> p (h r)"), bkb[0:64, :], [tb], [t_WukT])

        ckvT_all = sb("ckvT_all", [128, S], BF16)
        krT_all = sb("krT_all", [128, S], BF16)
        t_krz = Trk()
        P.op("pool", lambda e: e.memset(krT_all[:, :], 0.0), [], [t_krz])
        ckv_tok = sb("ckv_tok", [128, NT, 128], BF16)
        sc_all = sb("sc_all", [128, NT, 8])
        t_kv = [Trk() for _ in range(NT)]
        for t_ in t_kv:
            t_.w = t_krz.w
        kb_all = sb("kb_all", [128, NT]); t_nb = Trk()
        P.dma("sp", kb_all[:], dr["kbias"][:, :], t_nb, True)
        stTb = sb("stTb", [64, 512], BF16); t_stb = Trk()
        P.op("pool", lambda e: e.memset(stTb[:], 0.0), [], [t_stb])
        hT1 = sb("hT1", [128, 8, 129], BF16); t_hT1 = Trk()
        P.op("pool", lambda e: e.memset(hT1[:].rearrange("p c t -> p (c t)"), 0.0), [], [t_hT1])

        sm = sb("sm", [128, 64]); t_sm = Trk()
        t_smx, t_smkv, t_smks, t_smcq, t_smmla, t_smq, t_smkk, t_smgn = [Trk() for _ in range(8)]
        mla_f = sb("mla_f", [128, 416]); t_mla = Trk()
        feat = sb("feat", [128, 1536]); t_feat = Trk()
        lor = sb("lor", [128, 3, 128]); t_lor = Trk()
        lorb = sb("lorb", [128, 3, 128], BF16); t_lorb = Trk()
        posi = sb("posi", [128, 1], I32); posf = sb("posf", [128, 1]); t_pos = Trk()
        ang = sb("ang", [128, 32]); angi = sb("angi", [128, 32], I32); t_ang = Trk()
        cs4 = sb("cs4", [128, 4, 32]); t_cs4 = [Trk() for _ in range(4)]
        kr = sb("kr", [128, 64]); t_kr = Trk()
        krb = sb("krb", [128, 32], BF16); t_krb = Trk()
        cqn4 = sb("cqn4", [128, 4, 256], BF16); t_cqn4 = [Trk() for _ in range(4)]
        bonus = sb("bonus", [128, 8]); t_bonus = Trk()
        rden = sb("rden", [128, 8]); t_rden = Trk()
        mixr = sb("mixr", [128, 4, 512], BF16); t_mixr = [Trk() for _ in range(4)]

        rw_sig, t_sig = af(0, 512)
        rw_a, t_rwa = af(1, 512)
        rw_g, t_rwg = af(2, 512)
        cur_f, t_cur = af(3, 512); kkn, t_kkn = cur_f, t_cur
        dif_f, t_dif = af(4, 512); kp, t_kp = dif_f, t_dif
        tmp1, t_tmp1 = af(5, 512)
        tmp2, t_tmp2 = af(6, 512)
        Lraw, t_L = af(7, 512); y_f, t_y = Lraw, t_L
        Ebuf, t_E = af(8, 512)
        DCb = []; t_DC = []
        for i in range(2):
            a_, t_ = af(9 + i, 512, rows=64)
            DCb.append(a_); t_DC.append(t_)
        XB = {}; t_XB = {}
        for i, n in enumerate(("a", "b", "k", "r", "v")):
            XB[n], t_XB[n] = ab(11, i * 512, 512)
        XH = {}; t_XH = {}
        for i, n in enumerate(("a", "b", "k", "v")):
            XH[n], t_XH[n] = ab(14, i * 512, 512, rows=64)
        XF = {}; t_XF = {}
        for i, n in enumerate(("a", "b", "k", "r")):
            a_, t_ = ab(16 + i, 0, 1024, rows=64)
            XF[n] = a_.rearrange("p (h t) -> p h t", h=8); t_XF[n] = t_
        yc = [tmp1[0:64, :], tmp2[0:64, :]]; t_yc = [t_tmp1, t_tmp2]
        xt_, t_xt_ = af(20, 1024)
        junk, t_junk = ab(22, 0, 1024)
        hb, t_hb = ab(23, 0, 1024)
        sq, t_sq = af(24, 768)
        ckvn_, t_ckvn_ = ab(26, 0, 128)

        CBNAMES = ("N", "NT", "Arb", "Ark", "AakT", "M", "MT", "M2", "MT2", "T", "T2")
        CBALIAS = {"P1T": "N", "P2T": "NT", "Q1": "M", "Q2": "MT", "G1": "M2", "G2": "MT2"}
        CB = []
        for ch in range(2):
            d = {}
            for i, n in enumerate(CBNAMES):
                idx = ch * 11 + i
                a_, t_ = ab(20 + idx // 2, (idx % 2) * 512, 512, rows=64)
                d[n] = a_.rearrange("p (h t) -> p h t", h=8)
                d["t_" + n] = t_
            for n, o in CBALIAS.items():
                d[n] = d[o]
                d["t_" + n] = d["t_" + o]
            CB.append(d)

        cqT_, t_cqT = ab(0, 0, 256)
        cqT = cqT_.rearrange("p (c t) -> p c t", c=2)
        q_f_, t_q = af(1, 768)
        q_f = q_f_.rearrange("p (h d) -> p h d", h=8)
        sqq, t_sqq = af(3, 768)
        qnb_, t_qnb = ab(5, 0, 512); qnb = qnb_.rearrange("p (h d) -> p h d", h=8)
        qr1_, t_qr1 = af(6, 256); qr1 = qr1_.rearrange("p (h d) -> p h d", h=8)
        qr2_, t_qr2 = af(7, 256); qr2 = qr2_.rearrange("p (h d) -> p h d", h=8)
        qrb_, t_qrb = ab(8, 0, 256); qrb = qrb_.rearrange("p (h d) -> p h d", h=8)
        qnT_, t_qnT = ab(9, 0, 1024, rows=64); qnT = qnT_.rearrange("p (h t) -> p h t", h=8)
        qlatT_, t_qlat = ab(10, 0, 4096); qlatT = qlatT_.rearrange("p (h t) -> p h t", h=8)
        qrT_, t_qrT = ab(14, 0, 4096); qrT = qrT_.rearrange("p (h t) -> p h t", h=8)
        pT = []; t_pT = []
        for i in range(3):
            a_, t_ = ab(18 + i, 0, 512)
            pT.append(a_); t_pT.append(t_)
        den, t_den = af(21, 512)
        oT, t_oT = ab(22, 0, 512)
        pT2 = [[], []]; t_pT2 = [[], []]
        for i_, pgs in enumerate(((18, 19, 20), (0, 1, 2))):
            for p_ in pgs:
                a_, t_ = ab(p_, 0, 512)
                pT2[i_].append(a_); t_pT2[i_].append(t_)
        den3_, t_den3 = af(3, 512)
        den2 = [den, den3_]; t_den2 = [t_den, t_den3]
        oT4_, t_oT4 = ab(4, 0, 512)
        oT2 = [oT, oT4_]; t_oT2 = [t_oT, t_oT4]
        attn_, t_attn = af(23, 2048); attn = attn_.rearrange("p (j d) -> p j d", j=4)
        xr, t_xr = af(0, 1024)
        xm, t_xm = af(2, 1024)
        Woh_, t_Woh = ab(4, 0, 4096); Woh = Woh_.rearrange("p (c n) -> p c n", c=8)
        wst0, t_wst0 = af(8, 512)
        wst1, t_wst1 = af(29, 512)
        mixT_, t_mixT = ab(9, 0, 1024); mixT = mixT_.rearrange("p (c t) -> p c t", c=8)
        mixm_, t_mixm = ab(27, 0, 2048); mixm = mixm_.rearrange("p (j d) -> p j d", j=4)

        h3 = lambda ap: ap.rearrange("p (h d) -> p h d", h=8)

        def phase1(t):
            j = t % 4
            xb, txb = xt_, t_xt_
            P.dma("sp", xb, x[t * 128:(t + 1) * 128, :], txb[0], True, extra=txb[1:])
            P.dma("sp", posi[:], dr["pos"][t * 128:(t + 1) * 128].rearrange("(p o) -> p o", o=1), t_pos, True)
            cs = cs4[:, j, :]
            t_cs = t_cs4[j]
            cp("dve", posf[:], posi[:], [t_pos], [t_pos])
            ts("dve", ang[:, 16:32], invf[:], posf[:, 0:1], ALU.mult, [t_pos, t_invf], [t_ang],
               s2=1.0 / (2 * math.pi), op1=ALU.mult)
            ts("dve", ang[:, 0:16], ang[:, 16:32], 0.25, ALU.add, [t_ang], [t_ang])
            cp("dve", angi[:], ang[:], [t_ang], [t_ang])
            cp("dve", cs, angi[:], [t_ang], [t_cs])
            tt("dve", ang[:], ang[:], cs, ALU.subtract, [t_ang, t_cs], [t_ang])
            act(cs, ang[:], AF.Sin, [t_ang], [t_cs], scale=6.2831845)
            act(junk, xb, AF.Square, [txb], [t_junk, t_smx], accum=sm[:, 0:1])
            rsqrt_inplace(sm[:, 0:1], t_smx, 1.0 / D, 1e-6)
            ts("dve", hb, xb, sm[:, 0:1], ALU.mult, [txb, t_smx], [t_hb])
            hcur, thc = hT1, t_hT1
            bk, tb = bank()
            bkb = bk[:].bitcast(BF16)
            for c in range(8):
                tr(bkb[:, c * 128:(c + 1) * 128], hb[:, c * 128:(c + 1) * 128], ident_b[:], [t_hb, t_id], [tb], sig=(c == 7))
            cp("dve", hcur[:, :, 0:1], hcur[:, :, 128:129], [thc], [thc])
            cp("act", hcur[:, :, 1:129], bkb[:, :].rearrange("p (c t) -> p c t", c=8), [tb], [thc])
            bk, tb = bank()
            for c in range(8):
                mm(bk[:, 0:416], hcur[:, c, 1:129], Win[:, c, 0:416], c == 0, c == 7, [thc, t_Win], [tb])
            cp("act", mla_f[:], bk[:, 0:416], [tb], [t_mla])
            for g in range(3):
                c0 = 416 + g * 512
                bkc, tbc = bank()
                for c in range(8):
                    mm(bkc[:, :], hcur[:, c, 1:129], Win[:, c, c0:c0 + 512], c == 0, c == 7, [thc, t_Win], [tbc])
                bkp, tbp = bank()
                for c in range(8):
                    mm(bkp[:, :], hcur[:, c, 0:128], Win[:, c, c0:c0 + 512], c == 0, c == 7, [thc, t_Win], [tbp])
                cp("act", cur_f, bkc[:, :], [tbc], [t_cur])
                tt("dve", dif_f, bkp[:, :], cur_f, ALU.subtract, [tbp, t_cur], [t_dif])
                tt("pool", dif_f, dif_f, mu_bc[:, g * 512:(g + 1) * 512], ALU.mult, [t_dif, t_mu], [t_dif])
                tt("pool", feat[:, g * 512:(g + 1) * 512], dif_f, cur_f, ALU.add, [t_dif, t_cur], [t_feat])
            for li, (c0, n) in enumerate(((1952, 64), (2016, 64), (2080, 128))):
                bkc, tbc = bank()
                for c in range(8):
                    mm(bkc[0:n, 0:128], Win[:, c, c0:c0 + n], hcur[:, c, 1:129], c == 0, c == 7, [thc, t_Win], [tbc])
                for c in range(8):
                    mm(bkc[0:n, 128:256], Win[:, c, c0:c0 + n], hcur[:, c, 0:128], c == 0, c == 7, [thc, t_Win], [tbc])
                cp("act", lor[0:n, li, :], bkc[0:n, 0:128], [tbc], [t_lor])
                tt("dve", dif_f[0:n, 0:128], bkc[0:n, 128:256], lor[0:n, li, :], ALU.subtract, [tbc, t_lor], [t_dif])
                stt(lor[0:n, li, :], dif_f[0:n, 0:128], mul_c[0:n, li:li + 1], lor[0:n, li, :], ALU.mult, ALU.add,
                    [t_dif, t_mul, t_lor], [t_lor])
            act(lorb[0:64, 0, :], lor[0:64, 0, :], AF.Tanh, [t_lor], [t_lorb])
            cp("dve", lorb[0:64, 1, :], lor[0:64, 1, :], [t_lor], [t_lorb])
            act(lorb[:, 2, :], lor[:, 2, :], AF.Sigmoid, [t_lor], [t_lorb])
            bk, tb = bank()
            mm(bk[:, :], lorb[0:64, 0, :], W2[:, :], True, True, [t_lorb, t_W2], [tb])
            tt("dve", rw_sig, bk[:, :], w0_bc[:], ALU.add, [tb, t_w0], [t_sig])
            act(rw_sig, rw_sig, AF.Sigmoid, [t_sig], [t_sig])
            bk, tb = bank()
            mm(bk[:, :], lorb[0:64, 1, :], A2[:, :], True, True, [t_lorb, t_A2], [tb])
            tt("dve", rw_a, bk[:, :], a0_bc[:], ALU.add, [tb, t_a0], [t_rwa])
            act(rw_a, rw_a, AF.Sigmoid, [t_rwa], [t_rwa])
            bk, tb = bank()
            mm(bk[:, :], lorb[:, 2, :], G2[:, :], True, True, [t_lorb, t_G2], [tb])
            cp("act", rw_g, bk[:, :], [tb], [t_rwg])

            tk = t_kv[t]
            act(junk[:, 0:128], mla_f[:, 256:384], AF.Square, [t_mla], [t_junk, t_smkv], accum=sm[:, 1:2])
            rsqrt_inplace(sm[:, 1:2], t_smkv, 1.0 / 128, 1e-6)
            ts("dve", ckv_tok[:, t, :], mla_f[:, 256:384], sm[:, 1:2], ALU.mult, [t_mla, t_smkv], [tk])
            bk, tb = bank()
            bkb = bk[:].bitcast(BF16)
            tr(bkb[:, 0:128], ckv_tok[:, t, :], ident_b[:], [tk, t_id], [tb])
            cp("dve", ckvT_all[:, t * 128:(t + 1) * 128], bkb[:, 0:128], [tb], [tk])
            bk, tb = bank()
            mm(bk[:, :], ckvT_all[:, t * 128:(t + 1) * 128], Wuk[:, :], True, True, [tk, t_Wuk], [tb])
            act(sq[:, 0:512], bk[:, :], AF.Square, [tb], [t_sq])
            P.op("dve", lambda e: e.tensor_reduce(out=sm[:, 8:16], in_=h3(sq[:, 0:512]), axis=AX.X, op=ALU.add),
                 [t_sq], [t_smks])
            act(junk[:, 0:32], mla_f[:, 384:416], AF.Square, [t_mla], [t_junk, t_smks], accum=sm[:, 2:3])
            ts("dve", sm[:, 8:16], sm[:, 8:16], sm[:, 2:3], ALU.add, [t_smks], [t_smks])
            rsqrt_inplace(sm[:, 8:16], t_smks, 1.0 / QK, 1e-6)
            ts("dve", sc_all[:, t, :], sm[:, 8:16], QK ** -0.5, ALU.mult, [t_smks], [tk])
            tt("dve", kr[:, 0:32], mla_f[:, 384:416], gkn_bc[:, 64:96], ALU.mult, [t_mla, t_gkn], [t_kr])
            tt("dve", kr[:, 32:48], kr[:, 0:16], cs[:, 0:16], ALU.mult, [t_kr, t_cs], [t_kr])
            tt("dve", kr[:, 48:64], kr[:, 16:32], cs[:, 16:32], ALU.mult, [t_kr, t_cs], [t_kr])
            tt("dve", krb[:, 0:16], kr[:, 32:48], kr[:, 48:64], ALU.subtract, [t_kr], [t_krb])
            tt("dve", kr[:, 32:48], kr[:, 16:32], cs[:, 0:16], ALU.mult, [t_kr, t_cs], [t_kr])
            tt("dve", kr[:, 48:64], kr[:, 0:16], cs[:, 16:32], ALU.mult, [t_kr, t_cs], [t_kr])
            tt("dve", krb[:, 16:32], kr[:, 32:48], kr[:, 48:64], ALU.add, [t_kr], [t_krb])
            bk, tb = bank()
            bkb = bk[:].bitcast(BF16)
            tr(bkb[0:32, 0:128], krb[:], ident_b[:], [t_krb, t_id], [tb])
            cp("dve", krT_all[0:32, t * 128:(t + 1) * 128], bkb[0:32, 0:128], [tb], [tk])
            if t >= OWN0:
                act(junk[:, 0:256], mla_f[:, 0:256], AF.Square, [t_mla], [t_junk, t_smcq], accum=sm[:, 3:4])
                rsqrt_inplace(sm[:, 3:4], t_smcq, 1.0 / 256, 1e-6)
                ts("dve", cqn4[:, j, :], mla_f[:, 0:256], sm[:, 3:4], ALU.mult, [t_mla, t_smcq], [t_cqn4[j]])

        def qside(j):
            cs = cs4[:, j, :]
            t_cs = t_cs4[j]
            bk, tb = bank()
            bkb = bk[:].bitcast(BF16)
            for c in range(2):
                tr(bkb[:, c * 128:(c + 1) * 128], cqn4[:, j, c * 128:(c + 1) * 128], ident_b[:], [t_cqn4[j], t_id], [tb], sig=(c == 1))
            cp("dve", cqT_, bkb[:, 0:256], [tb], [t_cqT])
            bk1, tb1 = bank()
            for c in range(2):
                mm(bk1[:, :], cqT[:, c, :], Wuq[:, c, 0:512], c == 0, c == 1, [t_cqT, t_Wuq], [tb1])
            bk2, tb2 = bank()
            for c in range(2):
                mm(bk2[:, 0:256], cqT[:, c, :], Wuq[:, c, 512:768], c == 0, c == 1, [t_cqT, t_Wuq], [tb2])
            cp("act", q_f_[:, 0:512], bk1[:, :], [tb1], [t_q])
            cp("act", q_f_[:, 512:768], bk2[:, 0:256], [tb2], [t_q])
            act(sqq, q_f_, AF.Square, [t_q], [t_sqq])
            P.op("dve", lambda e: e.tensor_reduce(out=sm[:, 16:24], in_=sqq.rearrange("p (h d) -> p h d", h=8),
                                                  axis=AX.X, op=ALU.add), [t_sqq], [t_sm])
            rsqrt_inplace(sm[:, 16:24], t_sm, 1.0 / QK, 1e-6)
            rq_b64 = sm[:, 16:24].unsqueeze(2).to_broadcast([128, 8, 64])
            rq_b32 = sm[:, 16:24].unsqueeze(2).to_broadcast([128, 8, 32])
            tt("dve", q_f[:, :, 0:64], q_f[:, :, 0:64], rq_b64, ALU.mult, [t_q, t_sm], [t_q])
            tt("dve", qnb, q_f[:, :, 0:64], gqk_bc[:].unsqueeze(1).to_broadcast([128, 8, 64]), ALU.mult,
               [t_q, t_gqk], [t_qnb])
            tt("dve", q_f[:, :, 64:96], q_f[:, :, 64:96], rq_b32, ALU.mult, [t_q, t_sm], [t_q])
            tt("dve", q_f[:, :, 64:96], q_f[:, :, 64:96], gqn_bc[:, 64:96].unsqueeze(1).to_broadcast([128, 8, 32]),
               ALU.mult, [t_q, t_gqn], [t_q])
            cosb = cs[:, 0:16].unsqueeze(1).to_broadcast([128, 8, 16])
            sinb = cs[:, 16:32].unsqueeze(1).to_broadcast([128, 8, 16])
            tt("dve", qr1[:, :, 0:16], q_f[:, :, 64:80], cosb, ALU.mult, [t_q, t_cs], [t_qr1])
            tt("dve", qr1[:, :, 16:32], q_f[:, :, 80:96], sinb, ALU.mult, [t_q, t_cs], [t_qr1])
            tt("dve", qr2[:, :, 0:16], q_f[:, :, 80:96], cosb, ALU.mult, [t_q, t_cs], [t_qr2])
            tt("dve", qr2[:, :, 16:32], q_f[:, :, 64:80], sinb, ALU.mult, [t_q, t_cs], [t_qr2])
            tt("dve", qrb[:, :, 0:16], qr1[:, :, 0:16], qr1[:, :, 16:32], ALU.subtract, [t_qr1], [t_qrb])
            tt("dve", qrb[:, :, 16:32], qr2[:, :, 0:16], qr2[:, :, 16:32], ALU.add, [t_qr2], [t_qrb])
            bk, tb = bank()
            bkb = bk[:].bitcast(BF16)
            for h in range(8):
                tr(bkb[0:64, h * 128:(h + 1) * 128], qnb[:, h, :], ident_b[:], [t_qnb, t_id], [tb], sig=(h == 7))
            cp("dve", qnT_, bkb[0:64, :], [tb], [t_qnT])
            bk, tb = bank()
            bkb = bk[:].bitcast(BF16)
            for h in range(8):
                tr(bkb[0:32, h * 128:(h + 1) * 128], qrb[:, h, :], ident_b[:], [t_qrb, t_id], [tb], sig=(h == 7))
            if j == 0:
                P.op("pool", lambda e: e.memset(qrT_[:, :], 0.0), [], [t_qrT])
            cp("act", qrT[0:32, :, j * 128:(j + 1) * 128], bkb[0:32, :].rearrange("p (h t) -> p h t", h=8),
               [tb], [t_qrT])
            for hh in range(2):
                bk, tb = bank()
                for h4 in range(4):
                    h = hh * 4 + h4
                    mm(bk[:, h4 * 128:(h4 + 1) * 128], WukT[:, h, :], qnT[:, h, :], True, True,
                       [t_WukT, t_qnT], [tb], sig=(h4 == 3))
                cp("act", qlatT[:, hh * 4:(hh + 1) * 4, j * 128:(j + 1) * 128],
                   bk[:, :].rearrange("p (h t) -> p h t", h=4), [tb], [t_qlat])

        def rwkv(t):
            own = t >= OWN0
            r_ = feat[:, 0:512]; k_ = feat[:, 512:1024]; v_ = feat[:, 1024:1536]
            tt("dve", kkn, k_, kk_bc[:], ALU.mult, [t_feat, t_kk], [t_kkn])
            act(tmp1, kkn, AF.Square, [t_kkn], [t_tmp1])
            P.op("dve", lambda e: e.tensor_reduce(out=sm[:, 24:32], in_=h3(tmp1), axis=AX.X, op=ALU.add),
                 [t_tmp1], [t_smkk])
            rsqrt_inplace(sm[:, 24:32], t_smkk, 1.0, 1e-12)
            tt("dve", h3(kkn), h3(kkn), sm[:, 24:32].unsqueeze(2).to_broadcast([128, 8, 64]), ALU.mult,
               [t_kkn, t_smkk], [t_kkn])
            stt(tmp1, rw_a, -1.0, ka_bc[:], ALU.add, ALU.mult, [t_rwa, t_ka], [t_tmp1])
            stt(kp, tmp1, 1.0, k_, ALU.add, ALU.mult, [t_tmp1, t_feat], [t_kp])
            tt("pool", tmp2, r_, kp, ALU.mult, [t_feat, t_kp], [t_tmp2])
            tt("pool", tmp2, tmp2, rk_bc[:], ALU.mult, [t_tmp2, t_rk], [t_tmp2])
            P.op("dve", lambda e: e.tensor_reduce(out=bonus[:], in_=h3(tmp2), axis=AX.X, op=ALU.add),
                 [t_tmp2], [t_bonus])
            bk, tb = bank()
            mm(bk[:, :], tri_f[:], rw_sig, True, True, [t_tri, t_sig], [tb])
            cp("act", Lraw, bk[:, :], [tb], [t_L])
            for ch, sel in enumerate((sel63, sel127)):
                bk, tb = bank()
                mm(bk[0:64, :], sel[:], Lraw, True, True, [t_sel, t_L], [tb])
                act(DCb[ch], bk[0:64, :], AF.Exp, [tb], [t_DC[ch]], scale=C0)
            act(Ebuf, Lraw, AF.Exp, [t_L], [t_E], scale=C0)
            tt("dve", XB["r"], r_, Ebuf, ALU.mult, [t_feat, t_E], [t_XB["r"]])
            act(Ebuf, Lraw, AF.Exp, [t_L], [t_E], scale=-C0)
            tt("pool", XB["k"], kp, Ebuf, ALU.mult, [t_kp, t_E], [t_XB["k"]])
            tt("dve", tmp1, kkn, rw_a, ALU.mult, [t_kkn, t_rwa], [t_tmp1])
            tt("pool", XB["b"], tmp1, Ebuf, ALU.mult, [t_tmp1, t_E], [t_XB["b"]])
            tt("dve", tmp2, Lraw, rw_sig, ALU.subtract, [t_L, t_sig], [t_tmp2])
            act(Ebuf, tmp2, AF.Exp, [t_tmp2], [t_E], scale=C0)
            stt(XB["a"], kkn, -1.0, Ebuf, ALU.mult, ALU.mult, [t_kkn, t_E], [t_XB["a"]])
            cp("pool", XB["v"], v_, [t_feat], [t_XB["v"]])
            for n in ("a", "b", "k", "r"):
                bk, tb = bank()
                bkb = bk[:].bitcast(BF16)
                for h in range(8):
                    tr(bkb[0:64, h * 128:(h + 1) * 128], XB[n][:, h * 64:(h + 1) * 64], ident_b[:],
                       [t_XB[n], t_id], [tb], sig=(h == 7))
                cp("act" if n in ("a", "k") else "dve", XF[n], bkb[0:64, :].rearrange("p (h t) -> p h t", h=8),
                   [tb], [t_XF[n]])
            for n in ("a", "b", "k", "v"):
                bk, tb = bank()
                mm(bk[0:64, :], shift_hi[:], XB[n], True, True, [t_sh, t_XB[n]], [tb])
                cp("act" if n in ("a", "k") else "dve", XH[n], bk[0:64, :], [tb], [t_XH[n]])

            def tok(n, ch):
                return (XB[n][0:64, :], t_XB[n]) if ch == 0 else (XH[n], t_XH[n])

            def hmm(lhs, rhs, post):
                bk, tb = bank()
                for h in range(8):
                    l, tl = lhs(h)
                    r, trr = rhs(h)
                    mm(bk[0:64, h * 64:(h + 1) * 64], l, r, True, True, [tl, trr], [tb], sig=(h == 7))
                post(bk[0:64, :].rearrange("p (h t) -> p h t", h=8), tb)

            def FM(n, ch):
                return lambda h: (XF[n][:, h, ch * 64:(ch + 1) * 64], t_XF[n])

            def TM(n, ch):
                ap, trk = tok(n, ch)
                return lambda h: (ap[:, h * 64:(h + 1) * 64], trk)

            def CBm(ch, n):
                return lambda h: (CB[ch][n][:, h, :], CB[ch]["t_" + n])

            est = {"i": 0}

            def nxt():
                est["i"] ^= 1
                return ("dve", "act")[est["i"]]

            def to_masked(ch, n, mask):
                def post(ps, tb):
                    tt("dve", CB[ch][n], ps, mask[:], ALU.mult, [tb, t_msk], [CB[ch]["t_" + n]])
                return post

            def to_plain(ch, n):
                def post(ps, tb):
                    cp(nxt(), CB[ch][n], ps, [tb], [CB[ch]["t_" + n]])
                return post

            for ch in range(2):
                hmm(FM("b", ch), FM("a", ch), to_masked(ch, "N", m_su))
                hmm(FM("a", ch), FM("b", ch), to_masked(ch, "NT", m_sl))
                hmm(FM("b", ch), FM("r", ch), to_masked(ch, "Arb", m_ui))
                hmm(FM("k", ch), FM("r", ch), to_masked(ch, "Ark", m_ui))
                hmm(FM("a", ch), FM("k", ch), to_masked(ch, "AakT", m_sl))
            for ch in range(2):
                tt("pool", CB[ch]["T"], CB[ch]["N"], id8[:], ALU.add, [CB[ch]["t_N"], t_id8], [CB[ch]["t_T"]])
            for ch in range(2):
                hmm(CBm(ch, "NT"), CBm(ch, "N"), to_plain(ch, "M"))
                hmm(CBm(ch, "N"), CBm(ch, "NT"), to_plain(ch, "MT"))
            cur = ("M", "MT", "T")
            alt = ("M2", "MT2", "T2")
            for jlev in range(1, 6):
                Mn, MTn, Tn = cur
                Mo, MTo, To = alt
                for ch in range(2):
                    def post_T(ps, tb, ch=ch, Tn=Tn, To=To):
                        tt("dve", CB[ch][To], ps, CB[ch][Tn], ALU.add, [tb, CB[ch]["t_" + Tn]],
                           [CB[ch]["t_" + To]])
                    hmm(CBm(ch, MTn), CBm(ch, Tn), post_T)
                    if jlev < 5:
                        hmm(CBm(ch, MTn), CBm(ch, Mn), to_plain(ch, Mo))
                        hmm(CBm(ch, Mn), CBm(ch, MTn), to_plain(ch, MTo))
                cur, alt = (Mo, MTo, To), (Mn, MTn, Tn)
            Tfin = cur[2]
            for ch in range(2):
                hmm(CBm(ch, Tfin), TM("a", ch), to_plain(ch, "P1T"))
                hmm(CBm(ch, Tfin), CBm(ch, "AakT"), to_plain(ch, "P2T"))
            t13 = tmp1[0:64, :].rearrange("p (h t) -> p h t", h=8)
            t23 = tmp2[0:64, :].rearrange("p (h t) -> p h t", h=8)
            for ch in range(2):
                dc3 = DCb[ch].rearrange("p (h t) -> p h t", h=8)

                def post_Q1(ps, tb, ch=ch):
                    tt("dve", CB[ch]["Q1"], ps, XF["r"][:, :, ch * 64:(ch + 1) * 64], ALU.add,
                       [tb, t_XF["r"]], [CB[ch]["t_Q1"]])
                if own:
                    hmm(CBm(ch, "P1T"), CBm(ch, "Arb"), post_Q1)

                def post_G1(ps, tb, ch=ch, dc3=dc3):
                    tt("dve", t13, ps, id8[:], ALU.add, [tb, t_id8], [t_tmp1])
                    tt("pool", CB[ch]["G1"], t13, dc3, ALU.mult, [t_tmp1, t_DC[ch]], [CB[ch]["t_G1"]])
                hmm(CBm(ch, "P1T"), TM("b", ch), post_G1)

                def post_Q2(ps, tb, ch=ch):
                    tt("dve", CB[ch]["Q2"], ps, CB[ch]["Ark"], ALU.add, [tb, CB[ch]["t_Ark"]], [CB[ch]["t_Q2"]])
                if own:
                    hmm(CBm(ch, "P2T"), CBm(ch, "Arb"), post_Q2)

                def post_G2(ps, tb, ch=ch, dc3=dc3):
                    kap, ktrk = tok("k", ch)
                    tt("dve", t23, ps, kap.rearrange("p (h t) -> p h t", h=8), ALU.add, [tb, ktrk], [t_tmp2])
                    tt("pool", CB[ch]["G2"], t23, dc3, ALU.mult, [t_tmp2, t_DC[ch]], [CB[ch]["t_G2"]])
                hmm(CBm(ch, "P2T"), TM("b", ch), post_G2)
            for ch in range(2):
                vap, vtrk = tok("v", ch)
                if own:
                    bky, tby = bank()
                    for h in range(8):
                        hs = slice(h * 64, (h + 1) * 64)
                        mm(bky[0:64, hs], CB[ch]["Q1"][:, h, :], stTb[:, hs], True, False, [CB[ch]["t_Q1"], t_stb], [tby])
                        mm(bky[0:64, hs], CB[ch]["Q2"][:, h, :], vap[:, hs], False, True, [CB[ch]["t_Q2"], vtrk], [tby], sig=(h == 7))
                bks, tbs = bank()
                for h in range(8):
                    hs = slice(h * 64, (h + 1) * 64)
                    mm(bks[0:64, hs], CB[ch]["G1"][:, h, :], stTb[:, hs], True, False, [CB[ch]["t_G1"], t_stb], [tbs])
                    mm(bks[0:64, hs], CB[ch]["G2"][:, h, :], vap[:, hs], False, True, [CB[ch]["t_G2"], vtrk], [tbs], sig=(h == 7))
                if own:
                    cp("act", yc[ch], bky[0:64, :], [tby], [t_yc[ch]])
                cp("dve", stTb[:], bks[0:64, :], [tbs], [t_stb])
            if not own:
                return
            bk, tb = bank()
            mm(bk[:, :], ident_f[0:64, :], yc[0], True, False, [t_id, t_yc[0]], [tb])
            mm(bk[:, :], sel_lo_hi[:], yc[1], False, True, [t_selhi, t_yc[1]], [tb])
            cp("act", y_f, bk[:, :], [tb], [t_y])
            P.op("dve", lambda e: e.tensor_reduce(out=sm[:, 32:40], in_=h3(y_f), axis=AX.X, op=ALU.add),
                 [t_y], [t_sm])
            ts("dve", sm[:, 32:40], sm[:, 32:40], 1.0 / 64, ALU.mult, [t_sm], [t_sm])
            tt("dve", h3(y_f), h3(y_f), sm[:, 32:40].unsqueeze(2).to_broadcast([128, 8, 64]), ALU.subtract,
               [t_y, t_sm], [t_y])
            act(tmp1, y_f, AF.Square, [t_y], [t_tmp1])
            P.op("dve", lambda e: e.tensor_reduce(out=sm[:, 40:48], in_=h3(tmp1), axis=AX.X, op=ALU.add),
                 [t_tmp1], [t_sm])
            rsqrt_inplace(sm[:, 40:48], t_sm, 1.0 / 64, 64e-5)
            tt("dve", h3(y_f), h3(y_f), sm[:, 40:48].unsqueeze(2).to_broadcast([128, 8, 64]), ALU.mult,
               [t_y, t_sm], [t_y])
            tt("pool", y_f, y_f, lng_bc[:], ALU.mult, [t_y, t_lng], [t_y])
            tt("pool", y_f, y_f, lnb_bc[:], ALU.add, [t_y, t_lnb], [t_y])
            tt("dve", h3(tmp2), h3(v_), bonus[:].unsqueeze(2).to_broadcast([128, 8, 64]), ALU.mult,
               [t_feat, t_bonus], [t_tmp2])
            tt("dve", y_f, y_f, tmp2, ALU.add, [t_y, t_tmp2], [t_y])
            j = t % 4
            tt("dve", mixr[:, j, :], y_f, rw_g, ALU.mult, [t_y, t_rwg], [t_mixr[j]])
            if dbg:
                tt("dve", tmp1, y_f, rw_g, ALU.mult, [t_y, t_rwg], [t_tmp1])
                P.dma("sp", dbgo["d_rw"][(t - OWN0) * 128:(t - OWN0 + 1) * 128, :], tmp1, t_tmp1[0], False)

        def attention(B):
            nk = 4 * B + 4
            LA = 2
            for hp in range(4):
                H2 = (2 * hp, 2 * hp + 1)
                bko = [banks[6], banks[7]]
                tbo = [btrk[6], btrk[7]]

                def qk(i, kt):
                    h = H2[i]
                    bks, tbs = bank()
                    ks = slice(kt * 128, (kt + 1) * 128)
                    mm(bks[:, :], ckvT_all[:, ks], qlatT[:, h, :], True, False, [t_kv[kt], t_qlat], [tbs])
                    mm(bks[:, :], krT_all[:, ks], qrT[:, h, :], False, True, [t_kv[kt], t_qrT], [tbs])
                    return bks, tbs
                pend = [[], []]
                for k_ in range(min(LA, nk)):
                    for i in range(2):
                        pend[i].append(qk(i, k_))
                for kt in range(nk):
                    m = kt - 4 * B
                    cur = [pend[i].pop(0) for i in range(2)]
                    if kt + LA < nk:
                        for i in range(2):
                            pend[i].append(qk(i, kt + LA))
                    pts = []
                    for i in range(2):
                        h = H2[i]
                        bks, tbs = cur[i]
                        pt, tpt = pT2[i][kt % 3], t_pT2[i][kt % 3]
                        pts.append((pt, tpt))
                        act(pt, bks[:, :], AF.Exp, [tbs, t_kv[kt], t_nb], [tpt], scale=sc_all[:, kt, h:h + 1],
                            bias=kb_all[:, kt:kt + 1])
                        if m >= 0:
                            P.op("pool", lambda e, pt=pt, m=m: e.affine_select(
                                out=pt, in_=pt, pattern=[[1, 512]], compare_op=ALU.is_ge, fill=0.0,
                                base=-m * 128, channel_multiplier=-1), [tpt], [tpt])
                    for i in range(2):
                        pt, tpt = pts[i]
                        mm(bko[i][:, :], ckv_tok[:, kt, :], pt, kt == 0, kt == nk - 1, [t_kv[kt], tpt], [tbo[i]],
                           sig=(i == 1 and kt + LA >= nk))
                        if kt == 0:
                            cp("dve", den2[i], pt, [tpt], [t_den2[i]])
                        else:
                            tt("dve", den2[i], den2[i], pt, ALU.add, [t_den2[i], tpt], [t_den2[i]])
                for i in range(2):
                    h = H2[i]
                    cp("act", oT2[i], bko[i][:, :], [tbo[i]], [t_oT2[i]])
                    bkf, tbf = bank()
                    for j in range(4):
                        mm(bkf[:, j * 66:j * 66 + 64], oT2[i][:, j * 128:(j + 1) * 128], Wuv[:, h * 64:(h + 1) * 64],
                           True, True, [t_oT2[i], t_Wuv], [tbf], sig=False)
                        mm(bkf[:, j * 66 + 64:j * 66 + 65], den2[i][:, j * 128:(j + 1) * 128], ones_c[:, 0:1], True, True,
                           [t_den2[i], t_ones], [tbf], sig=(j == 3))
                    acc3 = bkf[:, 0:264].rearrange("p (j d) -> p j d", j=4)
                    rd = rden[:, i * 4:(i + 1) * 4]
                    P.op("dve", lambda e, acc3=acc3, rd=rd: e.reciprocal(out=rd.unsqueeze(2), in_=acc3[:, :, 64:65]),
                         [tbf], [t_rden])
                    tt("dve", attn[:, :, h * 64:(h + 1) * 64], acc3[:, :, 0:64],
                       rd.unsqueeze(2).to_broadcast([128, 4, 64]), ALU.mult, [tbf, t_rden], [t_attn])
            for j in range(4):
                act(junk[:, 0:512], attn[:, j, :], AF.Square, [t_attn], [t_junk, t_sm], accum=sm[:, 4 + j:5 + j])
            rsqrt_inplace(sm[:, 4:8], t_sm, 1.0 / 512, 1e-6)
            for j in range(4):
                t = 4 * B + j
                ts("dve", mixm[:, j, :], attn[:, j, :], sm[:, 4 + j:5 + j], ALU.mult, [t_attn, t_sm], [t_mixm])
                if dbg:
                    ts("dve", tmp1, attn[:, j, :], sm[:, 4 + j:5 + j], ALU.mult, [t_attn, t_sm], [t_tmp1])
                    P.dma("sp", dbgo["d_mla"][(t - OWN0) * 128:(t - OWN0 + 1) * 128, :], tmp1, t_tmp1[0], False)
            for half in range(2):
                for c in range(8):
                    wst, t_wst = (wst0, t_wst0) if c % 2 == 0 else (wst1, t_wst1)
                    P.dma("sp", wst, dr["w_o"][c * 128:(c + 1) * 128, half * 512:(half + 1) * 512],
                          t_wst[0], True)
                    if c < 4:
                        ts("dve", Woh[:, c, :], wst, gmo_c[:, c:c + 1], ALU.mult, [t_wst, t_gmo], [t_Woh])
                    else:
                        cp("dve", Woh[:, c, :], wst, [t_wst], [t_Woh])
                for j in range(4):
                    t = 4 * B + j
                    if half == 0:
                        pass
                    bk, tb = bank()
                    bkb = bk[:].bitcast(BF16)
                    for c in range(8):
                        src_ = mixm[:, j, c * 128:(c + 1) * 128] if c < 4 else mixr[:, j, (c - 4) * 128:(c - 3) * 128]
                        tr(bkb[:, c * 128:(c + 1) * 128], src_, ident_b[:], [t_mixm, t_mixr[j], t_id], [tb], sig=(c == 7))
                    cp("act", mixT_, bkb[:, :], [tb], [t_mixT])
                    P.dma("pool", xr[:, 0:512], x[t * 128:(t + 1) * 128, half * 512:(half + 1) * 512], t_xr[0], True)
                    bk, tb = bank()
                    for c in range(8):
                        mm(bk[:, :], mixT[:, c, :], Woh[:, c, :], c == 0, c == 7, [t_mixT, t_Woh], [tb])
                    tt("dve", xm[:, 0:512], bk[:, :], xr[:, 0:512], ALU.add, [tb, t_xr[0]], [t_xm[0]])
                    P.dma("sp", xmid[(t - OWN0) * 128:(t - OWN0 + 1) * 128, half * 512:(half + 1) * 512], xm[:, 0:512], t_xm[0], False)
                    if dbg:
                        P.dma("sp", dbgo["d_xmid"][(t - OWN0) * 128:(t - OWN0 + 1) * 128, half * 512:(half + 1) * 512], xm[:, 0:512],
                              t_xm[0], False)

        def moe_phase():
            TB = min(2048, SO)
            TBT = TB // 128
            NB = SO // TB
            NG = TB // 512
            yacc = sb("yacc", [128, TBT, D]); t_yacc = [Trk() for _ in range(TBT)]
            xT = sb("xT", [128, 8, TB], BF16); t_xT = [Trk() for _ in range(TBT)]
            hn_f = sb("hn_f", [128, D]); t_hnf = Trk()
            hn_b = sb("hn_b", [128, D], BF16); t_hnb = Trk()
            hnT_f = sb("hnT_f", [128, 8, 128]); t_hnT = Trk()
            mjunk = sb("mjunk", [128, D], BF16); t_mj = Trk()
            gffn_bc = sb("gffn_bc", [128, D]); t_gfb = Trk()
            P.dma("sp", gffn_bc[:], dr["g_ffn"].partition_broadcast(128), t_gfb, True)
            bgb = sb("bgb", [128, 36]); t_bgb = Trk()
            P.dma("sp", bgb[:, 0:4], dr["b_group"].partition_broadcast(128), t_bgb, True)
            P.dma("sp", bgb[:, 4:36], dr["b_expert"].partition_broadcast(128), t_bgb, True)
            Wr = sb("Wr", [128, 8, 36]); t_Wr = Trk()
            P.dma("sp", Wr[:, :, 0:4], dr["w_group"].rearrange("(c p) g -> p c g", p=128), t_Wr, True)
            P.dma("sp", Wr[:, :, 4:36], dr["w_expert"].rearrange("(c p) g -> p c g", p=128), t_Wr, True)
            lg = sb("lg", [128, 36]); t_lg = Trk()
            ms = sb("ms", [128, 32]); t_ms = Trk()
            r1 = sb("r1", [128, 32]); r2 = sb("r2", [128, 32]); r3 = sb("r3", [128, 32]); r4 = sb("r4", [128, 32])
            t_r = Trk()
            comb = sb("comb", [128, TBT, NE]); t_comb = [Trk() for _ in range(TBT)]
            wsg = [sb("wsg%d" % i, [128, 8, DE]) for i in range(2)]
            wsu = [sb("wsu%d" % i, [128, 8, DE]) for i in range(2)]
            wsd = [sb("wsd%d" % i, [128, 2, D]) for i in range(2)]
            t_wsg = [Trk(), Trk()]; t_wsu = [Trk(), Trk()]; t_wsd = [Trk(), Trk()]
            wbg = [sb("wbg%d" % i, [128, 8, DE], BF16) for i in range(2)]
            wbu = [sb("wbu%d" % i, [128, 8, DE], BF16) for i in range(2)]
            wbd = [sb("wbd%d" % i, [128, 2, D], BF16) for i in range(2)]
            t_wbg = [Trk(), Trk()]; t_wbu = [Trk(), Trk()]; t_wbd = [Trk(), Trk()]
            sg = [sb("sg%d" % i, [128, 512]) for i in range(2)]; t_sg = [Trk(), Trk()]
            hTb = [sb("hTb%d" % i, [128, 2, 512], BF16) for i in range(2)]; t_hTb = [Trk(), Trk()]
            flat = lambda ap: ap.rearrange("p a b -> p (a b)")
            wq = {"i": 0}

            def load_expert(e):
                i = e % 2
                q1 = "sp" if wq["i"] % 2 == 0 else "pool"
                q2 = "pool" if wq["i"] % 2 == 0 else "sp"
                wq["i"] += 1
                P.dma(q1, wsg[i][:], dr["w_gate"][e].rearrange("(c p) f -> p c f", p=128), t_wsg[i], True)
                P.dma(q2, wsu[i][:], dr["w_up"][e].rearrange("(c p) f -> p c f", p=128), t_wsu[i], True)
                P.dma(q1, wsd[i][:], dr["w_down"][e].rearrange("(c p) n -> p c n", p=128), t_wsd[i], True)
                cp("pool", flat(wbg[i][:]), flat(wsg[i][:]), [t_wsg[i]], [t_wbg[i]])
                cp("pool", flat(wbu[i][:]), flat(wsu[i][:]), [t_wsu[i]], [t_wbu[i]])
                cp("act", flat(wbd[i][:]), flat(wsd[i][:]), [t_wsd[i]], [t_wbd[i]])

            for blk in range(NB):
                for ti in range(TBT):
                    tok0 = blk * TB + ti * 128
                    P.dma("sp" if ti % 2 == 0 else "pool", yacc[:, ti, :], xmid[tok0:tok0 + 128, :], t_yacc[ti], True)
                    act(mjunk[:], yacc[:, ti, :], AF.Square, [t_yacc[ti]], [t_mj, t_ms], accum=ms[:, 0:1])
                    rsqrt_inplace(ms[:, 0:1], t_ms, 1.0 / D, 1e-6)
                    ts("dve", hn_f[:], yacc[:, ti, :], ms[:, 0:1], ALU.mult, [t_yacc[ti], t_ms], [t_hnf])
                    tt("pool", hn_f[:], hn_f[:], gffn_bc[:], ALU.mult, [t_hnf, t_gfb], [t_hnf])
                    cp("act", hn_b[:], hn_f[:], [t_hnf], [t_hnb])
                    bk, tb = bank()
                    bkb = bk[:].bitcast(BF16)
                    for c in range(8):
                        tr(bkb[:, c * 128:(c + 1) * 128], hn_b[:, c * 128:(c + 1) * 128], ident_b[:], [t_hnb, t_id], [tb], sig=(c == 7))
                    cp("act", xT[:, :, ti * 128:(ti + 1) * 128], bkb[:, :].rearrange("p (c t) -> p c t", c=8),
                       [tb], [t_xT[ti]])
                    for hh in range(2):
                        bk, tb = bank()
                        for c4 in range(4):
                            c = hh * 4 + c4
                            tr(bk[:, c4 * 128:(c4 + 1) * 128], hn_f[:, c * 128:(c + 1) * 128], ident_f[:],
                               [t_hnf, t_id], [tb], sig=(c4 == 3))
                        cp("dve", hnT_f[:, hh * 4:(hh + 1) * 4, :], bk[:, :].rearrange("p (c t) -> p c t", c=4),
                           [tb], [t_hnT])
                    bk, tb = bank()
                    for c in range(8):
                        mm(bk[:, 0:36], hnT_f[:, c, :], Wr[:, c, :], c == 0, c == 7, [t_hnT, t_Wr], [tb])
                    tt("dve", lg[:], bk[:, 0:36], bgb[:], ALU.add, [tb, t_bgb], [t_lg])
                    P.op("dve", lambda e: e.tensor_reduce(out=ms[:, 1:2], in_=lg[:, 0:4], axis=AX.X, op=ALU.max),
                         [t_lg], [t_ms])
                    ts("dve", r1[:, 0:4], lg[:, 0:4], ms[:, 1:2], ALU.is_equal, [t_lg, t_ms], [t_r])
                    ts("dve", ms[:, 2:3], ms[:, 1:2], -1.0, ALU.mult, [t_ms], [t_ms])
                    act(r2[:, 0:4], lg[:, 0:4], AF.Exp, [t_lg, t_ms], [t_r, t_ms], bias=ms[:, 2:3], accum=ms[:, 3:4])
                    P.op("dve", lambda e: e.reciprocal(out=ms[:, 3:4], in_=ms[:, 3:4]), [t_ms], [t_ms])
                    ts("dve", r1[:, 4:8], r1[:, 0:4], -1.0, ALU.add, [t_r], [t_r], s2=1e30, op1=ALU.mult)
                    tt("dve", r3[:].rearrange("p (g e) -> p g e", g=4), lg[:, 4:36].rearrange("p (g e) -> p g e", g=4),
                       r1[:, 4:8].unsqueeze(2).to_broadcast([128, 4, 8]), ALU.add, [t_lg, t_r], [t_r])
                    P.op("dve", lambda e: e.tensor_reduce(out=ms[:, 4:5], in_=r3[:], axis=AX.X, op=ALU.max),
                         [t_r], [t_ms])
                    ts("dve", r2[:], r3[:], ms[:, 4:5], ALU.is_equal, [t_r, t_ms], [t_r])
                    stt(r4[:], r2[:], -1e30, r3[:], ALU.mult, ALU.add, [t_r], [t_r])
                    P.op("dve", lambda e: e.tensor_reduce(out=ms[:, 5:6], in_=r4[:], axis=AX.X, op=ALU.max),
                         [t_r], [t_ms])
                    ts("dve", r3[:], r4[:], ms[:, 5:6], ALU.is_equal, [t_r, t_ms], [t_r])
                    tt("dve", ms[:, 6:7], ms[:, 5:6], ms[:, 4:5], ALU.subtract, [t_ms], [t_ms])
                    act(ms[:, 6:7], ms[:, 6:7], AF.Exp, [t_ms], [t_ms])
                    ts("dve", ms[:, 7:8], ms[:, 6:7], 1.0, ALU.add, [t_ms], [t_ms])
                    P.op("dve", lambda e: e.reciprocal(out=ms[:, 7:8], in_=ms[:, 7:8]), [t_ms], [t_ms])
                    tt("dve", ms[:, 8:9], ms[:, 6:7], ms[:, 7:8], ALU.mult, [t_ms], [t_ms])
                    tt("dve", ms[:, 7:8], ms[:, 7:8], ms[:, 3:4], ALU.mult, [t_ms], [t_ms])
                    tt("dve", ms[:, 8:9], ms[:, 8:9], ms[:, 3:4], ALU.mult, [t_ms], [t_ms])
                    ts("dve", r4[:], r2[:], ms[:, 7:8], ALU.mult, [t_r, t_ms], [t_r])
                    stt(comb[:, ti, :], r3[:], ms[:, 8:9], r4[:], ALU.mult, ALU.add, [t_r, t_ms], [t_comb[ti]])
                for e in range(NE):
                    i = e % 2
                    load_expert(e)
                    for grp in range(NG):
                        gs = slice(grp * 512, (grp + 1) * 512)
                        xtr = [t_xT[grp * 4 + j] for j in range(4)]
                        hb_, thb_ = hTb[grp % 2], t_hTb[grp % 2]
                        for fc in range(2):
                            bkg, tbg = bank()
                            for c in range(8):
                                mm(bkg[:, :], wbg[i][:, c, fc * 128:(fc + 1) * 128], xT[:, c, gs], c == 0, c == 7,
                                   [t_wbg[i], xtr], [tbg])
                            bku, tbu = bank()
                            for c in range(8):
                                mm(bku[:, :], wbu[i][:, c, fc * 128:(fc + 1) * 128], xT[:, c, gs], c == 0, c == 7,
                                   [t_wbu[i], xtr], [tbu])
                            act(sg[fc][:], bkg[:, :], AF.Silu, [tbg], [t_sg[fc]])
                            tt("dve", hb_[:, fc, :], bku[:, :], sg[fc][:], ALU.mult, [tbu, t_sg[fc]], [thb_])
                        for j in range(4):
                            ti = grp * 4 + j
                            for half in range(2):
                                bk, tb = bank()
                                for fc in range(2):
                                    mm(bk[:, :], hb_[:, fc, j * 128:(j + 1) * 128],
                                       wbd[i][:, fc, half * 512:(half + 1) * 512], fc == 0, fc == 1,
                                       [thb_, t_wbd[i]], [tb])
                                ysl = yacc[:, ti, half * 512:(half + 1) * 512]
                                stt(ysl, bk[:, :], comb[:, ti, e:e + 1], ysl, ALU.mult, ALU.add,
                                    [tb, t_comb[ti], t_yacc[ti]], [t_yacc[ti]])
                for ti in range(TBT):
                    tok0 = blk * TB + ti * 128
                    P.dma("sp" if ti % 2 == 0 else "pool", out[tok0:tok0 + 128, :], yacc[:, ti, :], t_yacc[ti], False)

        P.barrier()
        for t in range(NT):
            phase1(t)
            rwkv(t)
            if t % 4 == 3 and t >= OWN0:
                for j in range(4):
                    qside(j)
                attention(t // 4)
        P.barrier()
        mes.close()
        cures["es"] = es
        moe_phase()
        P.final_wait("sp")
        with nc.Block() as block:
            P.emit(block)
    return nc


_CACHE = {}


def kernel(**inputs):
    NT = 64
    if NT not in _CACHE:
        _CACHE[NT] = build(NT)
    nc = _CACHE[NT]
    x = np.asarray(inputs["x"], dtype=np.float32)
    pos = np.asarray(inputs["positions"], dtype=np.int32)
    w = {}
    for n, shp in WNAMES:
        w[n] = np.ascontiguousarray(np.asarray(inputs[n], dtype=np.float32).reshape(shp))
    H = 4096
    in_maps = []
    for c in range(8):
        b, p = c // 2, c % 2
        kb = np.full((128, NT), -8.0, np.float32)
        if p == 1:
            xc = x[b]
            pc = pos[b]
        else:
            xc = np.concatenate([np.zeros((H, D), np.float32), x[b, :H]], axis=0)
            pc = np.concatenate([np.zeros((H,), np.int32), pos[b, :H]], axis=0)
            kb[:, :NT // 2] = -30000.0
        m = {"x": np.ascontiguousarray(xc), "pos": np.ascontiguousarray(pc), "kbias": kb}
        m.update(w)
        in_maps.append(m)
    res = run_bass_kernel_spmd(nc, in_maps, core_ids=list(range(8)))
    out = np.empty((4, 2 * H, D), np.float32)
    for c in range(8):
        b, p = c // 2, c % 2
        out[b, p * H:(p + 1) * H] = res.results[c]["out"]
    return out
```

```python
import math
from contextlib import ExitStack

import numpy as np
import concourse.bass as bass
import concourse.mybir as mybir
from concourse.bass_utils import run_bass_kernel_spmd

F32 = mybir.dt.float32
BF16 = mybir.dt.bfloat16
I32 = mybir.dt.int32
AF = mybir.ActivationFunctionType
ALU = mybir.AluOpType
AX = mybir.AxisListType

D = 1024
NH = 8
QK = 96
NOPE = 64
ROPE = 32
INW = 2208
NE = 32
DE = 256
C0 = -math.exp(-0.5)


class Trk:
    __slots__ = ("w", "r", "dsem", "dcnt", "q")

    def __init__(self):
        self.w = None
        self.r = {}
        self.dsem = None
        self.dcnt = 0
        self.q = None


class Prog:
    ENG = ("pe", "act", "dve", "pool", "sp")

    def __init__(self, nc):
        self.nc = nc
        self.streams = {e: [] for e in self.ENG}
        self.cnt = {e: 0 for e in self.ENG}
        self.sems = {}
        for e in ("pe", "act", "dve", "pool"):
            self.sems[e] = nc.alloc_semaphore(name="s_" + e)
        self.seen = {e: {} for e in self.ENG}
        self.ndsem = 0
        self.dtrk = []
        self.pending = {e: {} for e in self.ENG}

    def _need(self, eng, waits, key, val):
        if val <= 0 or self.seen[eng].get(key, 0) >= val:
            return
        if key == eng and val > self.cnt[eng]:
            return
        if waits.get(key, 0) < val:
            waits[key] = val

    def _deps(self, eng, reads, writes):
        waits = {}
        if self.pending[eng]:
            for k, v in self.pending[eng].items():
                self._need(eng, waits, k, v)
            self.pending[eng] = {}
        for t in reads:
            if t.w is not None:
                self._need(eng, waits, t.w[0], t.w[1])
        for t in writes:
            if t.w is not None:
                self._need(eng, waits, t.w[0], t.w[1])
            for k, v in t.r.items():
                self._need(eng, waits, k, v)
        for k, v in waits.items():
            self.seen[eng][k] = v
        return waits

    @staticmethod
    def _flat(ts):
        o = []
        for t in ts:
            if isinstance(t, (list, tuple)):
                o.extend(Prog._flat(t))
            else:
                o.append(t)
        return o

    def op(self, eng, fn, reads=(), writes=(), sig=True):
        reads = self._flat(reads)
        writes = self._flat(writes)
        waits = self._deps(eng, reads, writes)
        if sig:
            self.cnt[eng] += 1
            val = self.cnt[eng]
        else:
            val = self.cnt[eng] + 1
        for t in reads:
            if t.r.get(eng, 0) < val:
                t.r[eng] = val
        for t in writes:
            t.w = (eng, val)
            t.r = {}
        self.streams[eng].append((fn, waits, (eng, 1) if sig else None))

    def dma(self, q, out, in_, sb, load, **kw):
        if sb.q is None:
            sb.q = q
        q = sb.q
        if sb.dsem is None:
            sb.dsem = self.nc.alloc_semaphore(name="d%d" % self.ndsem)
            self.ndsem += 1
            self.dtrk.append(sb)
        key = ("d", id(sb))
        self.sems[key] = sb.dsem
        ex = self._flat(kw.pop("extra", []))
        reads, writes = (list(ex), [sb]) if load else ([sb], list(ex))
        if not load:
            reads, writes = [sb] + [], []
            reads = [sb]
            reads = [sb] + list(ex)
        else:
            writes = [sb] + list(ex)
            reads = []
        waits = self._deps(q, reads, writes)
        sb.dcnt += 16
        val = sb.dcnt
        for t in writes:
            t.w = (key, val)
            t.r = {}
        for t in reads:
            if t.r.get(key, 0) < val:
                t.r[key] = val

        def fn(e, out=out, in_=in_, kw=kw):
            return e.dma_start(out=out, in_=in_, **kw)
        self.streams[q].append((fn, waits, (key, 16)))

    def barrier(self):
        waits = {}
        for e in ("pe", "act", "dve", "pool"):
            if self.cnt[e] > 0:
                waits[e] = self.cnt[e]
        for t in self.dtrk:
            waits[("d", id(t))] = t.dcnt
        for e in self.ENG:
            for k, v in waits.items():
                if self.pending[e].get(k, 0) < v:
                    self.pending[e][k] = v

    def final_wait(self, eng="sp"):
        waits = {}
        for t in self.dtrk:
            self._need(eng, waits, ("d", id(t)), t.dcnt)

        def fn(e, waits=waits):
            for k, v in waits.items():
                e.wait_ge(self.sems[k], v)
            return None
        self.streams[eng].append((fn, {}, None))

    def emit(self, block):
        engmap = {"pe": "tensor", "act": "scalar", "dve": "vector", "pool": "gpsimd", "sp": "sync"}
        for e in self.ENG:
            stream = self.streams[e]
            if not stream:
                continue

            def body(engobj, stream=stream):
                for fn, waits, inc in stream:
                    for k, v in waits.items():
                        engobj.wait_ge(self.sems[k], v)
                    ins = fn(engobj)
                    if inc is not None:
                        ins.then_inc(self.sems[inc[0]], inc[1])
            getattr(block, engmap[e])(body)


WNAMES = [
    ("g_mix", [D]), ("w_in", [D, INW]), ("g_cq", [256]), ("w_uq", [256, 768]), ("g_ckv", [128]),
    ("w_uk", [128, 512]), ("w_uv", [128, 512]), ("g_qn", [QK]), ("g_kn", [QK]), ("g_mla_out", [512]),
    ("rw_mu", [1792]), ("rw_w0", [512]), ("rw_w2", [64, 512]), ("rw_a0", [512]), ("rw_a2", [64, 512]),
    ("rw_g2", [128, 512]), ("rw_k_k", [512]), ("rw_k_a", [512]), ("rw_r_k", [512]), ("rw_lnx_g", [512]),
    ("rw_lnx_b", [512]), ("w_o", [D, D]), ("g_ffn", [D]), ("w_group", [D, 4]), ("b_group", [4]),
    ("w_expert", [D, NE]), ("b_expert", [NE]), ("w_gate", [NE, D, DE]), ("w_up", [NE, D, DE]),
    ("w_down", [NE, DE, D]),
]


def build(NT, dbg=False):
    S = NT * 128
    OWN0 = NT // 2
    SO = (NT - OWN0) * 128
    nc = bass.Bass("TRN2", target_bir_lowering=False)
    dr = {}
    dr["x"] = nc.dram_tensor("x", [S, D], F32, kind="ExternalInput").ap()
    dr["pos"] = nc.dram_tensor("pos", [S], I32, kind="ExternalInput").ap()
    for n, shp in WNAMES:
        dr[n] = nc.dram_tensor(n, shp, F32, kind="ExternalInput").ap()
    dr["kbias"] = nc.dram_tensor("kbias", [128, NT], F32, kind="ExternalInput").ap()
    out = nc.dram_tensor("out", [SO, D], F32, kind="ExternalOutput").ap()
    xmid = nc.dram_tensor("xmid", [SO, D], F32, kind="Internal").ap()
    dbgo = {}
    if dbg:
        for n, w in (("d_mla", 512), ("d_rw", 512), ("d_xmid", 1024)):
            dbgo[n] = nc.dram_tensor(n, [SO, w], F32, kind="ExternalOutput").ap()
    P = Prog(nc)
    x = dr["x"]

    with ExitStack() as es:
        mes = ExitStack()
        cures = {"es": es}

        def sb(name, shape, dt=F32):
            return cures["es"].enter_context(nc.sbuf_tensor(name, shape, dt))

        banks = [es.enter_context(nc.psum_tensor("bank%d" % i, [128, 512], F32)) for i in range(8)]
        btrk = [Trk() for _ in range(8)]
        bstate = {"i": 0}

        def bank():
            i = bstate["i"]
            bstate["i"] = (i + 1) % 6
            return banks[i], btrk[i]

        def mm(o, lhsT, rhs, start, stop, reads, writes, sig=None):
            P.op("pe", lambda e: e.matmul(o, lhsT=lhsT, rhs=rhs, start=start, stop=stop,
                                          skip_group_check=True), reads, writes, sig=(stop if sig is None else sig))

        def tr(o, in_, ident, reads, writes, sig=True):
            P.op("pe", lambda e: e.transpose(out=o, in_=in_, identity=ident), reads, writes, sig=sig)

        def cp(eng, o, i, reads, writes):
            if eng == "act":
                P.op("act", lambda e: e.activation(out=o, in_=i, func=AF.Copy), reads, writes)
            else:
                P.op(eng, lambda e: e.tensor_copy(out=o, in_=i), reads, writes)

        def tt(eng, o, a, b, op_, reads, writes):
            P.op(eng, lambda e: e.tensor_tensor(out=o, in0=a, in1=b, op=op_), reads, writes)

        def ts(eng, o, a, s1, op0, reads, writes, s2=None, op1=None):
            if op1 is None:
                P.op(eng, lambda e: e.tensor_scalar(out=o, in0=a, scalar1=s1, scalar2=None, op0=op0), reads, writes)
            else:
                P.op(eng, lambda e: e.tensor_scalar(out=o, in0=a, scalar1=s1, scalar2=s2, op0=op0, op1=op1), reads, writes)

        def stt(o, a, s, b, op0, op1, reads, writes):
            P.op("dve", lambda e: e.scalar_tensor_tensor(out=o, in0=a, scalar=s, in1=b, op0=op0, op1=op1), reads, writes)

        def act(o, i, func, reads, writes, scale=None, bias=None, accum=None):
            kw = {}
            if scale is not None:
                kw["scale"] = scale
            if bias is not None:
                kw["bias"] = bias
            if accum is not None:
                kw["accum_out"] = accum
            P.op("act", lambda e: e.activation(out=o, in_=i, func=func, **kw), reads, writes)

        def rsqrt_inplace(v, tv, mult, eps):
            ts("dve", v, v, mult, ALU.mult, [tv], [tv], s2=eps, op1=ALU.add)
            act(v, v, AF.Sqrt, [tv], [tv])
            P.op("dve", lambda e: e.reciprocal(out=v, in_=v), [tv], [tv])

        ones_f = sb("ones_f", [128, 512], BF16); t_ones = Trk()
        P.op("pool", lambda e: e.memset(ones_f[:], 1.0), [], [t_ones])
        ones_c = sb("ones_c", [128, 1])
        P.op("pool", lambda e: e.memset(ones_c[:], 1.0), [], [t_ones])
        ident_f = sb("ident_f", [128, 128]); ident_b = sb("ident_b", [128, 128], BF16); t_id = Trk()
        P.op("pool", lambda e: e.affine_select(out=ident_f[:], in_=ones_f[:, 0:128], pattern=[[-1, 128]],
                                               compare_op=ALU.is_equal, fill=0.0, base=0, channel_multiplier=1),
             [t_ones], [t_id])
        cp("dve", ident_b[:], ident_f[:], [t_id], [t_id])
        cures["es"] = mes
        tri_f = sb("tri_f", [128, 128]); t_tri = Trk()
        P.op("pool", lambda e: e.affine_select(out=tri_f[:], in_=ones_f[:, 0:128], pattern=[[1, 128]],
                                               compare_op=ALU.is_ge, fill=0.0, base=0, channel_multiplier=-1),
             [t_ones], [t_tri])
        P.op("pool", lambda e: e.memset(tri_f[0:64, 64:128], 0.0), [], [t_tri])
        sel63 = sb("sel63", [128, 64]); sel127 = sb("sel127", [128, 64]); t_sel = Trk()
        P.op("pool", lambda e: e.affine_select(out=sel63[:], in_=ones_f[:, 0:64], pattern=[[0, 64]],
                                               compare_op=ALU.is_equal, fill=0.0, base=-63, channel_multiplier=1),
             [t_ones], [t_sel])
        P.op("pool", lambda e: e.affine_select(out=sel127[:], in_=ones_f[:, 0:64], pattern=[[0, 64]],
                                               compare_op=ALU.is_equal, fill=0.0, base=-127, channel_multiplier=1),
             [t_ones], [t_sel])
        shift_hi = sb("shift_hi", [128, 64], BF16); t_sh = Trk()
        P.op("pool", lambda e: e.affine_select(out=shift_hi[:], in_=ones_f[:, 0:64], pattern=[[-1, 64]],
                                               compare_op=ALU.is_equal, fill=0.0, base=-64, channel_multiplier=1),
             [t_ones], [t_sh])
        m_su = sb("m_su", [64, 8, 64], BF16); m_ui = sb("m_ui", [64, 8, 64], BF16); m_sl = sb("m_sl", [64, 8, 64], BF16); t_msk = Trk()
        ones3 = ones_f[0:64, :].rearrange("p (h t) -> p h t", h=8)
        P.op("pool", lambda e: e.affine_select(out=m_su[:], in_=ones3, pattern=[[0, 8], [1, 64]],
                                               compare_op=ALU.is_gt, fill=0.0, base=0, channel_multiplier=-1),
             [t_ones], [t_msk])
        P.op("pool", lambda e: e.affine_select(out=m_ui[:], in_=ones3, pattern=[[0, 8], [1, 64]],
                                               compare_op=ALU.is_ge, fill=0.0, base=0, channel_multiplier=-1),
             [t_ones], [t_msk])
        P.op("pool", lambda e: e.affine_select(out=m_sl[:], in_=ones3, pattern=[[0, 8], [-1, 64]],
                                               compare_op=ALU.is_gt, fill=0.0, base=0, channel_multiplier=1),
             [t_ones], [t_msk])
        id8 = sb("id8", [64, 8, 64], BF16); t_id8 = Trk()
        P.op("pool", lambda e: e.affine_select(out=id8[:], in_=ones3, pattern=[[0, 8], [-1, 64]],
                                               compare_op=ALU.is_equal, fill=0.0, base=0, channel_multiplier=1),
             [t_ones], [t_id8])
        sel_lo_hi = sb("sel_lo_hi", [64, 128]); t_selhi = Trk()
        P.op("pool", lambda e: e.affine_select(out=sel_lo_hi[:], in_=ones_f[0:64, 0:128], pattern=[[1, 128]],
                                               compare_op=ALU.is_equal, fill=0.0, base=-64, channel_multiplier=-1),
             [t_ones], [t_selhi])
        jidx = sb("jidx", [128, 16]); invf = sb("invf", [128, 16]); t_invf = Trk()
        P.op("pool", lambda e: e.iota(jidx[:], pattern=[[1, 16]], base=0, channel_multiplier=0,
                                      allow_small_or_imprecise_dtypes=True), [], [t_invf])
        act(invf[:], jidx[:], AF.Exp, [t_invf], [t_invf], scale=-math.log(10000.0) / 16.0)

        t_vec = Trk()

        def bc_load(name, src, n):
            tile_ = sb(name, [128, n])
            trk = Trk()
            P.dma("sp", tile_[:], src.partition_broadcast(128), trk, True)
            return tile_, trk

        def col_load(name, src, ncol):
            tile_ = sb(name, [128, ncol])
            trk = Trk()
            P.dma("sp", tile_[:], src.rearrange("(c p) -> p c", p=128), trk, True,
                  allow_slow_non_contiguous=True)
            return tile_, trk

        gmix_c, t_gmix = col_load("gmix_c", dr["g_mix"], 8)
        gcq_c, t_gcq = col_load("gcq_c", dr["g_cq"], 2)
        gckv_c, t_gckv = col_load("gckv_c", dr["g_ckv"], 1)
        gmo_c, t_gmo = col_load("gmo_c", dr["g_mla_out"], 4)
        mu_bc, t_mu = bc_load("mu_bc", dr["rw_mu"][0:1536], 1536)
        mul_c = sb("mul_c", [128, 3]); t_mul = Trk()
        P.dma("sp", mul_c[0:64, 0:1], dr["rw_mu"][1536:1600].rearrange("(p o) -> p o", o=1), t_mul, True)
        P.dma("sp", mul_c[0:64, 1:2], dr["rw_mu"][1600:1664].rearrange("(p o) -> p o", o=1), t_mul, True)
        P.dma("sp", mul_c[:, 2:3], dr["rw_mu"][1664:1792].rearrange("(p o) -> p o", o=1), t_mul, True)
        w0_bc, t_w0 = bc_load("w0_bc", dr["rw_w0"], 512)
        a0_bc, t_a0 = bc_load("a0_bc", dr["rw_a0"], 512)
        kk_bc, t_kk = bc_load("kk_bc", dr["rw_k_k"], 512)
        ka_bc, t_ka = bc_load("ka_bc", dr["rw_k_a"], 512)
        rk_bc, t_rk = bc_load("rk_bc", dr["rw_r_k"], 512)
        lng_bc, t_lng = bc_load("lng_bc", dr["rw_lnx_g"], 512)
        lnb_bc, t_lnb = bc_load("lnb_bc", dr["rw_lnx_b"], 512)
        gqn_bc, t_gqn = bc_load("gqn_bc", dr["g_qn"], QK)
        gkn_bc, t_gkn = bc_load("gkn_bc", dr["g_kn"], QK)
        gqk_bc = sb("gqk_bc", [128, 64]); t_gqk = Trk()
        tt("dve", gqk_bc[:], gqn_bc[:, 0:64], gkn_bc[:, 0:64], ALU.mult, [t_gqn, t_gkn], [t_gqk])

        NPG = 31
        AR = sb("arena", [128, NPG * 512])
        ARb = AR[:].bitcast(BF16)
        pg_t = [Trk() for _ in range(NPG)]

        def af(p0, ncols, rows=128):
            npg = (ncols + 511) // 512
            return AR[0:rows, p0 * 512:p0 * 512 + ncols], pg_t[p0:p0 + npg]

        def ab(p0, off, ncols, rows=128):
            c0 = p0 * 1024 + off
            p1 = (c0 + ncols - 1) // 1024
            return ARb[0:rows, c0:c0 + ncols], pg_t[c0 // 1024:p1 + 1]

        stage = [AR[:, 0:2208], AR[:, 9 * 512:9 * 512 + 2208]]
        t_stage = [pg_t[0:5], pg_t[9:14]]
        sstate = {"i": 0}

        def load_scaled(dst, dst_trk, src_ap, ncols, rows=128, scale_col=None, scale_trk=None, q="sp"):
            i = sstate["i"]
            sstate["i"] = 1 - i
            st, tst = stage[i], t_stage[i]
            P.dma(q, st[0:rows, 0:ncols], src_ap, tst[0], True, extra=tst[1:])
            if scale_col is None:
                cp("dve", dst, st[0:rows, 0:ncols], [tst], [dst_trk])
            else:
                ts("dve", dst, st[0:rows, 0:ncols], scale_col, ALU.mult, [tst, scale_trk], [dst_trk])

        Win = sb("Win", [128, 8, INW], BF16); t_Win = Trk()
        for c in range(8):
            load_scaled(Win[:, c, :], t_Win, dr["w_in"][c * 128:(c + 1) * 128, :], INW,
                        scale_col=gmix_c[:, c:c + 1], scale_trk=t_gmix, q=("sp" if c % 2 == 0 else "pool"))
        Wuq = sb("Wuq", [128, 2, 768], BF16); t_Wuq = Trk()
        for c in range(2):
            load_scaled(Wuq[:, c, :], t_Wuq, dr["w_uq"][c * 128:(c + 1) * 128, :], 768,
                        scale_col=gcq_c[:, c:c + 1], scale_trk=t_gcq)
        Wuk = sb("Wuk", [128, 512], BF16); t_Wuk = Trk()
        load_scaled(Wuk[:], t_Wuk, dr["w_uk"][:, :], 512, scale_col=gckv_c[:, 0:1], scale_trk=t_gckv)
        Wuv = sb("Wuv", [128, 512], BF16); t_Wuv = Trk()
        load_scaled(Wuv[:], t_Wuv, dr["w_uv"][:, :], 512, scale_col=gckv_c[:, 0:1], scale_trk=t_gckv)
        W2 = sb("W2", [64, 512], BF16); t_W2 = Trk()
        load_scaled(W2[:], t_W2, dr["rw_w2"][:, :], 512, rows=64)
        A2 = sb("A2", [64, 512], BF16); t_A2 = Trk()
        load_scaled(A2[:], t_A2, dr["rw_a2"][:, :], 512, rows=64)
        G2 = sb("G2", [128, 512], BF16); t_G2 = Trk()
        load_scaled(G2[:], t_G2, dr["rw_g2"][:, :], 512)
        WukT = sb("WukT", [64, 8, 128], BF16); t_WukT = Trk()
        bk, tb = bank()
        bkb = bk[:].bitcast(BF16)
        for h in range(8):
            tr(bkb[0:64, h * 128:(h + 1) * 128], Wuk[:, h * 64:(h + 1) * 64], ident_b[:], [t_Wuk, t_id], [tb], sig=(h == 7))
        cp("dve", WukT[:].rearrange("p h r -> p (h r)"), bkb[0:64, :], [tb], [t_WukT])

        ckvT_all = sb("ckvT_all", [128, S], BF16)
        krT_all = sb("krT_all", [128, S], BF16)
        t_krz = Trk()
        P.op("pool", lambda e: e.memset(krT_all[:, :], 0.0), [], [t_krz])
        ckv_tok = sb("ckv_tok", [128, NT, 128], BF16)
        sc_all = sb("sc_all", [128, NT, 8])
        t_kv = [Trk() for _ in range(NT)]
        for t_ in t_kv:
            t_.w = t_krz.w
        kb_all = sb("kb_all", [128, NT]); t_nb = Trk()
        P.dma("sp", kb_all[:], dr["kbias"][:, :], t_nb, True)
        stTb = sb("stTb", [64, 512], BF16); t_stb = Trk()
        P.op("pool", lambda e: e.memset(stTb[:], 0.0), [], [t_stb])
        hT1 = sb("hT1", [128, 8, 129], BF16); t_hT1 = Trk()
        P.op("pool", lambda e: e.memset(hT1[:].rearrange("p c t -> p (c t)"), 0.0), [], [t_hT1])

        sm = sb("sm", [128, 64]); t_sm = Trk()
        t_smx, t_smkv, t_smks, t_smcq, t_smmla, t_smq, t_smkk, t_smgn = [Trk() for _ in range(8)]
        mla_f = sb("mla_f", [128, 416]); t_mla = Trk()
        feat = sb("feat", [128, 1536]); t_feat = Trk()
        lor = sb("lor", [128, 3, 128]); t_lor = Trk()
        lorb = sb("lorb", [128, 3, 128], BF16); t_lorb = Trk()
        posi = sb("posi", [128, 1], I32); posf = sb("posf", [128, 1]); t_pos = Trk()
        ang = sb("ang", [128, 32]); angi = sb("angi", [128, 32], I32); t_ang = Trk()
        cs4 = sb("cs4", [128, 4, 32]); t_cs4 = [Trk() for _ in range(4)]
        kr = sb("kr", [128, 64]); t_kr = Trk()
        krb = sb("krb", [128, 32], BF16); t_krb = Trk()
        cqn4 = sb("cqn4", [128, 4, 256], BF16); t_cqn4 = [Trk() for _ in range(4)]
        bonus = sb("bonus", [128, 8]); t_bonus = Trk()
        rden = sb("rden", [128, 8]); t_rden = Trk()
        mixr = sb("mixr", [128, 4, 512], BF16); t_mixr = [Trk() for _ in range(4)]

        rw_sig, t_sig = af(0, 512)
        rw_a, t_rwa = af(1, 512)
        rw_g, t_rwg = af(2, 512)
        cur_f, t_cur = af(3, 512); kkn, t_kkn = cur_f, t_cur
        dif_f, t_dif = af(4, 512); kp, t_kp = dif_f, t_dif
        tmp1, t_tmp1 = af(5, 512)
        tmp2, t_tmp2 = af(6, 512)
        Lraw, t_L = af(7, 512); y_f, t_y = Lraw, t_L
        Ebuf, t_E = af(8, 512)
        DCb = []; t_DC = []
        for i in range(2):
            a_, t_ = af(9 + i, 512, rows=64)
            DCb.append(a_); t_DC.append(t_)
        XB = {}; t_XB = {}
        for i, n in enumerate(("a", "b", "k", "r", "v")):
            XB[n], t_XB[n] = ab(11, i * 512, 512)
        XH = {}; t_XH = {}
        for i, n in enumerate(("a", "b", "k", "v")):
            XH[n], t_XH[n] = ab(14, i * 512, 512, rows=64)
        XF = {}; t_XF = {}
        for i, n in enumerate(("a", "b", "k", "r")):
            a_, t_ = ab(16 + i, 0, 1024, rows=64)
            XF[n] = a_.rearrange("p (h t) -> p h t", h=8); t_XF[n] = t_
        yc = [tmp1[0:64, :], tmp2[0:64, :]]; t_yc = [t_tmp1, t_tmp2]
        xt_, t_xt_ = af(20, 1024)
        junk, t_junk = ab(22, 0, 1024)
        hb, t_hb = ab(23, 0, 1024)
        sq, t_sq = af(24, 768)
        ckvn_, t_ckvn_ = ab(26, 0, 128)

        CBNAMES = ("N", "NT", "Arb", "Ark", "AakT", "M", "MT", "M2", "MT2", "T", "T2")
        CBALIAS = {"P1T": "N", "P2T": "NT", "Q1": "M", "Q2": "MT", "G1": "M2", "G2": "MT2"}
        CB = []
        for ch in range(2):
            d = {}
            for i, n in enumerate(CBNAMES):
                idx = ch * 11 + i
                a_, t_ = ab(20 + idx // 2, (idx % 2) * 512, 512, rows=64)
                d[n] = a_.rearrange("p (h t) -> p h t", h=8)
                d["t_" + n] = t_
            for n, o in CBALIAS.items():
                d[n] = d[o]
                d["t_" + n] = d["t_" + o]
            CB.append(d)

        cqT_, t_cqT = ab(0, 0, 256)
        cqT = cqT_.rearrange("p (c t) -> p c t", c=2)
        q_f_, t_q = af(1, 768)
        q_f = q_f_.rearrange("p (h d) -> p h d", h=8)
        sqq, t_sqq = af(3, 768)
        qnb_, t_qnb = ab(5, 0, 512); qnb = qnb_.rearrange("p (h d) -> p h d", h=8)
        qr1_, t_qr1 = af(6, 256); qr1 = qr1_.rearrange("p (h d) -> p h d", h=8)
        qr2_, t_qr2 = af(7, 256); qr2 = qr2_.rearrange("p (h d) -> p h d", h=8)
        qrb_, t_qrb = ab(8, 0, 256); qrb = qrb_.rearrange("p (h d) -> p h d", h=8)
        qnT_, t_qnT = ab(9, 0, 1024, rows=64); qnT = qnT_.rearrange("p (h t) -> p h t", h=8)
        qlatT_, t_qlat = ab(10, 0, 4096); qlatT = qlatT_.rearrange("p (h t) -> p h t", h=8)
        qrT_, t_qrT = ab(14, 0, 4096); qrT = qrT_.rearrange("p (h t) -> p h t", h=8)
        pT = []; t_pT = []
        for i in range(3):
            a_, t_ = ab(18 + i, 0, 512)
            pT.append(a_); t_pT.append(t_)
        den, t_den = af(21, 512)
        oT, t_oT = ab(22, 0, 512)
        pT2 = [[], []]; t_pT2 = [[], []]
        for i_, pgs in enumerate(((18, 19, 20), (0, 1, 2))):
            for p_ in pgs:
                a_, t_ = ab(p_, 0, 512)
                pT2[i_].append(a_); t_pT2[i_].append(t_)
        den3_, t_den3 = af(3, 512)
        den2 = [den, den3_]; t_den2 = [t_den, t_den3]
        oT4_, t_oT4 = ab(4, 0, 512)
        oT2 = [oT, oT4_]; t_oT2 = [t_oT, t_oT4]
        attn_, t_attn = af(23, 2048); attn = attn_.rearrange("p (j d) -> p j d", j=4)
        xr, t_xr = af(0, 1024)
        xm, t_xm = af(2, 1024)
        Woh_, t_Woh = ab(4, 0, 4096); Woh = Woh_.rearrange("p (c n) -> p c n", c=8)
        wst0, t_wst0 = af(8, 512)
        wst1, t_wst1 = af(29, 512)
        mixT_, t_mixT = ab(9, 0, 1024); mixT = mixT_.rearrange("p (c t) -> p c t", c=8)
        mixm_, t_mixm = ab(27, 0, 2048); mixm = mixm_.rearrange("p (j d) -> p j d", j=4)

        h3 = lambda ap: ap.rearrange("p (h d) -> p h d", h=8)

        def phase1(t):
            j = t % 4
            own = t >= OWN0
            xb, txb = xt_, t_xt_
            P.dma("sp", xb, x[t * 128:(t + 1) * 128, :], txb[0], True, extra=txb[1:])
            P.dma("sp", posi[:], dr["pos"][t * 128:(t + 1) * 128].rearrange("(p o) -> p o", o=1), t_pos, True)
            cs = cs4[:, j, :]
            t_cs = t_cs4[j]
            cp("dve", posf[:], posi[:], [t_pos], [t_pos])
            ts("dve", ang[:, 16:32], invf[:], posf[:, 0:1], ALU.mult, [t_pos, t_invf], [t_ang],
               s2=1.0 / (2 * math.pi), op1=ALU.mult)
            ts("dve", ang[:, 0:16], ang[:, 16:32], 0.25, ALU.add, [t_ang], [t_ang])
            cp("dve", angi[:], ang[:], [t_ang], [t_ang])
            cp("dve", cs, angi[:], [t_ang], [t_cs])
            tt("dve", ang[:], ang[:], cs, ALU.subtract, [t_ang, t_cs], [t_ang])
            act(cs, ang[:], AF.Sin, [t_ang], [t_cs], scale=6.2831845)
            act(junk, xb, AF.Square, [txb], [t_junk, t_smx], accum=sm[:, 0:1])
            rsqrt_inplace(sm[:, 0:1], t_smx, 1.0 / D, 1e-6)
            ts("dve", hb, xb, sm[:, 0:1], ALU.mult, [txb, t_smx], [t_hb])
            hcur, thc = hT1, t_hT1
            bk, tb = bank()
            bkb = bk[:].bitcast(BF16)
            for c in range(8):
                tr(bkb[:, c * 128:(c + 1) * 128], hb[:, c * 128:(c + 1) * 128], ident_b[:], [t_hb, t_id], [tb], sig=(c == 7))
            cp("dve", hcur[:, :, 0:1], hcur[:, :, 128:129], [thc], [thc])
            cp("act", hcur[:, :, 1:129], bkb[:, :].rearrange("p (c t) -> p c t", c=8), [tb], [thc])
            bk, tb = bank()
            for c in range(8):
                mm(bk[:, 0:416], hcur[:, c, 1:129], Win[:, c, 0:416], c == 0, c == 7, [thc, t_Win], [tb])
            cp("act", mla_f[:], bk[:, 0:416], [tb], [t_mla])
            for g in range(3):
                if g == 0 and not own:
                    continue
                c0 = 416 + g * 512
                bkc, tbc = bank()
                for c in range(8):
                    mm(bkc[:, :], hcur[:, c, 1:129], Win[:, c, c0:c0 + 512], c == 0, c == 7, [thc, t_Win], [tbc])
                bkp, tbp = bank()
                for c in range(8):
                    mm(bkp[:, :], hcur[:, c, 0:128], Win[:, c, c0:c0 + 512], c == 0, c == 7, [thc, t_Win], [tbp])
                cp("act", cur_f, bkc[:, :], [tbc], [t_cur])
                tt("dve", dif_f, bkp[:, :], cur_f, ALU.subtract, [tbp, t_cur], [t_dif])
                tt("pool", dif_f, dif_f, mu_bc[:, g * 512:(g + 1) * 512], ALU.mult, [t_dif, t_mu], [t_dif])
                tt("pool", feat[:, g * 512:(g + 1) * 512], dif_f, cur_f, ALU.add, [t_dif, t_cur], [t_feat])
            for li, (c0, n) in enumerate(((1952, 64), (2016, 64), (2080, 128))):
                if li == 2 and not own:
                    continue
                bkc, tbc = bank()
                for c in range(8):
                    mm(bkc[0:n, 0:128], Win[:, c, c0:c0 + n], hcur[:, c, 1:129], c == 0, c == 7, [thc, t_Win], [tbc])
                for c in range(8):
                    mm(bkc[0:n, 128:256], Win[:, c, c0:c0 + n], hcur[:, c, 0:128], c == 0, c == 7, [thc, t_Win], [tbc])
                cp("act", lor[0:n, li, :], bkc[0:n, 0:128], [tbc], [t_lor])
                tt("dve", dif_f[0:n, 0:128], bkc[0:n, 128:256], lor[0:n, li, :], ALU.subtract, [tbc, t_lor], [t_dif])
                stt(lor[0:n, li, :], dif_f[0:n, 0:128], mul_c[0:n, li:li + 1], lor[0:n, li, :], ALU.mult, ALU.add,
                    [t_dif, t_mul, t_lor], [t_lor])
            act(lorb[0:64, 0, :], lor[0:64, 0, :], AF.Tanh, [t_lor], [t_lorb])
            cp("dve", lorb[0:64, 1, :], lor[0:64, 1, :], [t_lor], [t_lorb])
            if own:
                act(lorb[:, 2, :], lor[:, 2, :], AF.Sigmoid, [t_lor], [t_lorb])
            bk, tb = bank()
            mm(bk[:, :], lorb[0:64, 0, :], W2[:, :], True, True, [t_lorb, t_W2], [tb])
            tt("dve", rw_sig, bk[:, :], w0_bc[:], ALU.add, [tb, t_w0], [t_sig])
            act(rw_sig, rw_sig, AF.Sigmoid, [t_sig], [t_sig])
            bk, tb = bank()
            mm(bk[:, :], lorb[0:64, 1, :], A2[:, :], True, True, [t_lorb, t_A2], [tb])
            tt("dve", rw_a, bk[:, :], a0_bc[:], ALU.add, [tb, t_a0], [t_rwa])
            act(rw_a, rw_a, AF.Sigmoid, [t_rwa], [t_rwa])
            if own:
                bk, tb = bank()
                mm(bk[:, :], lorb[:, 2, :], G2[:, :], True, True, [t_lorb, t_G2], [tb])
                cp("act", rw_g, bk[:, :], [tb], [t_rwg])

            tk = t_kv[t]
            act(junk[:, 0:128], mla_f[:, 256:384], AF.Square, [t_mla], [t_junk, t_smkv], accum=sm[:, 1:2])
            rsqrt_inplace(sm[:, 1:2], t_smkv, 1.0 / 128, 1e-6)
            ts("dve", ckv_tok[:, t, :], mla_f[:, 256:384], sm[:, 1:2], ALU.mult, [t_mla, t_smkv], [tk])
            bk, tb = bank()
            bkb = bk[:].bitcast(BF16)
            tr(bkb[:, 0:128], ckv_tok[:, t, :], ident_b[:], [tk, t_id], [tb])
            cp("dve", ckvT_all[:, t * 128:(t + 1) * 128], bkb[:, 0:128], [tb], [tk])
            bk, tb = bank()
            mm(bk[:, :], ckvT_all[:, t * 128:(t + 1) * 128], Wuk[:, :], True, True, [tk, t_Wuk], [tb])
            act(sq[:, 0:512], bk[:, :], AF.Square, [tb], [t_sq])
            P.op("dve", lambda e: e.tensor_reduce(out=sm[:, 8:16], in_=h3(sq[:, 0:512]), axis=AX.X, op=ALU.add),
                 [t_sq], [t_smks])
            act(junk[:, 0:32], mla_f[:, 384:416], AF.Square, [t_mla], [t_junk, t_smks], accum=sm[:, 2:3])
            ts("dve", sm[:, 8:16], sm[:, 8:16], sm[:, 2:3], ALU.add, [t_smks], [t_smks])
            rsqrt_inplace(sm[:, 8:16], t_smks, 1.0 / QK, 1e-6)
            ts("dve", sc_all[:, t, :], sm[:, 8:16], QK ** -0.5, ALU.mult, [t_smks], [tk])
            tt("dve", kr[:, 0:32], mla_f[:, 384:416], gkn_bc[:, 64:96], ALU.mult, [t_mla, t_gkn], [t_kr])
            tt("dve", kr[:, 32:48], kr[:, 0:16], cs[:, 0:16], ALU.mult, [t_kr, t_cs], [t_kr])
            tt("dve", kr[:, 48:64], kr[:, 16:32], cs[:, 16:32], ALU.mult, [t_kr, t_cs], [t_kr])
            tt("dve", krb[:, 0:16], kr[:, 32:48], kr[:, 48:64], ALU.subtract, [t_kr], [t_krb])
            tt("dve", kr[:, 32:48], kr[:, 16:32], cs[:, 0:16], ALU.mult, [t_kr, t_cs], [t_kr])
            tt("dve", kr[:, 48:64], kr[:, 0:16], cs[:, 16:32], ALU.mult, [t_kr, t_cs], [t_kr])
            tt("dve", krb[:, 16:32], kr[:, 32:48], kr[:, 48:64], ALU.add, [t_kr], [t_krb])
            bk, tb = bank()
            bkb = bk[:].bitcast(BF16)
            tr(bkb[0:32, 0:128], krb[:], ident_b[:], [t_krb, t_id], [tb])
            cp("dve", krT_all[0:32, t * 128:(t + 1) * 128], bkb[0:32, 0:128], [tb], [tk])
            if t >= OWN0:
                act(junk[:, 0:256], mla_f[:, 0:256], AF.Square, [t_mla], [t_junk, t_smcq], accum=sm[:, 3:4])
                rsqrt_inplace(sm[:, 3:4], t_smcq, 1.0 / 256, 1e-6)
                ts("dve", cqn4[:, j, :], mla_f[:, 0:256], sm[:, 3:4], ALU.mult, [t_mla, t_smcq], [t_cqn4[j]])

        def qside(j):
            cs = cs4[:, j, :]
            t_cs = t_cs4[j]
            bk, tb = bank()
            bkb = bk[:].bitcast(BF16)
            for c in range(2):
                tr(bkb[:, c * 128:(c + 1) * 128], cqn4[:, j, c * 128:(c + 1) * 128], ident_b[:], [t_cqn4[j], t_id], [tb], sig=(c == 1))
            cp("dve", cqT_, bkb[:, 0:256], [tb], [t_cqT])
            bk1, tb1 = bank()
            for c in range(2):
                mm(bk1[:, :], cqT[:, c, :], Wuq[:, c, 0:512], c == 0, c == 1, [t_cqT, t_Wuq], [tb1])
            bk2, tb2 = bank()
            for c in range(2):
                mm(bk2[:, 0:256], cqT[:, c, :], Wuq[:, c, 512:768], c == 0, c == 1, [t_cqT, t_Wuq], [tb2])
            cp("act", q_f_[:, 0:512], bk1[:, :], [tb1], [t_q])
            cp("act", q_f_[:, 512:768], bk2[:, 0:256], [tb2], [t_q])
            act(sqq, q_f_, AF.Square, [t_q], [t_sqq])
            P.op("dve", lambda e: e.tensor_reduce(out=sm[:, 16:24], in_=sqq.rearrange("p (h d) -> p h d", h=8),
                                                  axis=AX.X, op=ALU.add), [t_sqq], [t_sm])
            rsqrt_inplace(sm[:, 16:24], t_sm, 1.0 / QK, 1e-6)
            rq_b64 = sm[:, 16:24].unsqueeze(2).to_broadcast([128, 8, 64])
            rq_b32 = sm[:, 16:24].unsqueeze(2).to_broadcast([128, 8, 32])
            tt("dve", q_f[:, :, 0:64], q_f[:, :, 0:64], rq_b64, ALU.mult, [t_q, t_sm], [t_q])
            tt("dve", qnb, q_f[:, :, 0:64], gqk_bc[:].unsqueeze(1).to_broadcast([128, 8, 64]), ALU.mult,
               [t_q, t_gqk], [t_qnb])
            tt("dve", q_f[:, :, 64:96], q_f[:, :, 64:96], rq_b32, ALU.mult, [t_q, t_sm], [t_q])
            tt("dve", q_f[:, :, 64:96], q_f[:, :, 64:96], gqn_bc[:, 64:96].unsqueeze(1).to_broadcast([128, 8, 32]),
               ALU.mult, [t_q, t_gqn], [t_q])
            cosb = cs[:, 0:16].unsqueeze(1).to_broadcast([128, 8, 16])
            sinb = cs[:, 16:32].unsqueeze(1).to_broadcast([128, 8, 16])
            tt("dve", qr1[:, :, 0:16], q_f[:, :, 64:80], cosb, ALU.mult, [t_q, t_cs], [t_qr1])
            tt("dve", qr1[:, :, 16:32], q_f[:, :, 80:96], sinb, ALU.mult, [t_q, t_cs], [t_qr1])
            tt("dve", qr2[:, :, 0:16], q_f[:, :, 80:96], cosb, ALU.mult, [t_q, t_cs], [t_qr2])
            tt("dve", qr2[:, :, 16:32], q_f[:, :, 64:80], sinb, ALU.mult, [t_q, t_cs], [t_qr2])
            tt("dve", qrb[:, :, 0:16], qr1[:, :, 0:16], qr1[:, :, 16:32], ALU.subtract, [t_qr1], [t_qrb])
            tt("dve", qrb[:, :, 16:32], qr2[:, :, 0:16], qr2[:, :, 16:32], ALU.add, [t_qr2], [t_qrb])
            bk, tb = bank()
            bkb = bk[:].bitcast(BF16)
            for h in range(8):
                tr(bkb[0:64, h * 128:(h + 1) * 128], qnb[:, h, :], ident_b[:], [t_qnb, t_id], [tb], sig=(h == 7))
            cp("dve", qnT_, bkb[0:64, :], [tb], [t_qnT])
            bk, tb = bank()
            bkb = bk[:].bitcast(BF16)
            for h in range(8):
                tr(bkb[0:32, h * 128:(h + 1) * 128], qrb[:, h, :], ident_b[:], [t_qrb, t_id], [tb], sig=(h == 7))
            if j == 0:
                P.op("pool", lambda e: e.memset(qrT_[:, :], 0.0), [], [t_qrT])
            cp("act", qrT[0:32, :, j * 128:(j + 1) * 128], bkb[0:32, :].rearrange("p (h t) -> p h t", h=8),
               [tb], [t_qrT])
            for hh in range(2):
                bk, tb = bank()
                for h4 in range(4):
                    h = hh * 4 + h4
                    mm(bk[:, h4 * 128:(h4 + 1) * 128], WukT[:, h, :], qnT[:, h, :], True, True,
                       [t_WukT, t_qnT], [tb], sig=(h4 == 3))
                cp("act", qlatT[:, hh * 4:(hh + 1) * 4, j * 128:(j + 1) * 128],
                   bk[:, :].rearrange("p (h t) -> p h t", h=4), [tb], [t_qlat])

        def rwkv(t):
            own = t >= OWN0
            r_ = feat[:, 0:512]; k_ = feat[:, 512:1024]; v_ = feat[:, 1024:1536]
            tt("dve", kkn, k_, kk_bc[:], ALU.mult, [t_feat, t_kk], [t_kkn])
            act(tmp1, kkn, AF.Square, [t_kkn], [t_tmp1])
            P.op("dve", lambda e: e.tensor_reduce(out=sm[:, 24:32], in_=h3(tmp1), axis=AX.X, op=ALU.add),
                 [t_tmp1], [t_smkk])
            rsqrt_inplace(sm[:, 24:32], t_smkk, 1.0, 1e-12)
            tt("dve", h3(kkn), h3(kkn), sm[:, 24:32].unsqueeze(2).to_broadcast([128, 8, 64]), ALU.mult,
               [t_kkn, t_smkk], [t_kkn])
            stt(tmp1, rw_a, -1.0, ka_bc[:], ALU.add, ALU.mult, [t_rwa, t_ka], [t_tmp1])
            stt(kp, tmp1, 1.0, k_, ALU.add, ALU.mult, [t_tmp1, t_feat], [t_kp])
            if own:
                tt("pool", tmp2, r_, kp, ALU.mult, [t_feat, t_kp], [t_tmp2])
                tt("pool", tmp2, tmp2, rk_bc[:], ALU.mult, [t_tmp2, t_rk], [t_tmp2])
                P.op("dve", lambda e: e.tensor_reduce(out=bonus[:], in_=h3(tmp2), axis=AX.X, op=ALU.add),
                     [t_tmp2], [t_bonus])
            bk, tb = bank()
            mm(bk[:, :], tri_f[:], rw_sig, True, True, [t_tri, t_sig], [tb])
            cp("act", Lraw, bk[:, :], [tb], [t_L])
            for ch, sel in enumerate((sel63, sel127)):
                bk, tb = bank()
                mm(bk[0:64, :], sel[:], Lraw, True, True, [t_sel, t_L], [tb])
                act(DCb[ch], bk[0:64, :], AF.Exp, [tb], [t_DC[ch]], scale=C0)
            if own:
                act(Ebuf, Lraw, AF.Exp, [t_L], [t_E], scale=C0)
                tt("dve", XB["r"], r_, Ebuf, ALU.mult, [t_feat, t_E], [t_XB["r"]])
            act(Ebuf, Lraw, AF.Exp, [t_L], [t_E], scale=-C0)
            tt("pool", XB["k"], kp, Ebuf, ALU.mult, [t_kp, t_E], [t_XB["k"]])
            tt("dve", tmp1, kkn, rw_a, ALU.mult, [t_kkn, t_rwa], [t_tmp1])
            tt("pool", XB["b"], tmp1, Ebuf, ALU.mult, [t_tmp1, t_E], [t_XB["b"]])
            tt("dve", tmp2, Lraw, rw_sig, ALU.subtract, [t_L, t_sig], [t_tmp2])
            act(Ebuf, tmp2, AF.Exp, [t_tmp2], [t_E], scale=C0)
            stt(XB["a"], kkn, -1.0, Ebuf, ALU.mult, ALU.mult, [t_kkn, t_E], [t_XB["a"]])
            cp("pool", XB["v"], v_, [t_feat], [t_XB["v"]])
            for n in (("a", "b", "k", "r") if own else ("a", "b", "k")):
                bk, tb = bank()
                bkb = bk[:].bitcast(BF16)
                for h in range(8):
                    tr(bkb[0:64, h * 128:(h + 1) * 128], XB[n][:, h * 64:(h + 1) * 64], ident_b[:],
                       [t_XB[n], t_id], [tb], sig=(h == 7))
                cp("act" if n in ("a", "k") else "dve", XF[n], bkb[0:64, :].rearrange("p (h t) -> p h t", h=8),
                   [tb], [t_XF[n]])
            for n in ("a", "b", "k", "v"):
                bk, tb = bank()
                mm(bk[0:64, :], shift_hi[:], XB[n], True, True, [t_sh, t_XB[n]], [tb])
                cp("act" if n in ("a", "k") else "dve", XH[n], bk[0:64, :], [tb], [t_XH[n]])

            def tok(n, ch):
                return (XB[n][0:64, :], t_XB[n]) if ch == 0 else (XH[n], t_XH[n])

            def hmm(lhs, rhs, post):
                bk, tb = bank()
                for h in range(8):
                    l, tl = lhs(h)
                    r, trr = rhs(h)
                    mm(bk[0:64, h * 64:(h + 1) * 64], l, r, True, True, [tl, trr], [tb], sig=(h == 7))
                post(bk[0:64, :].rearrange("p (h t) -> p h t", h=8), tb)

            def FM(n, ch):
                return lambda h: (XF[n][:, h, ch * 64:(ch + 1) * 64], t_XF[n])

            def TM(n, ch):
                ap, trk = tok(n, ch)
                return lambda h: (ap[:, h * 64:(h + 1) * 64], trk)

            def CBm(ch, n):
                return lambda h: (CB[ch][n][:, h, :], CB[ch]["t_" + n])

            est = {"i": 0}

            def nxt():
                est["i"] ^= 1
                return ("dve", "act")[est["i"]]

            def to_masked(ch, n, mask):
                def post(ps, tb):
                    tt("dve", CB[ch][n], ps, mask[:], ALU.mult, [tb, t_msk], [CB[ch]["t_" + n]])
                return post

            def to_plain(ch, n):
                def post(ps, tb):
                    cp(nxt(), CB[ch][n], ps, [tb], [CB[ch]["t_" + n]])
                return post

            for ch in range(2):
                hmm(FM("b", ch), FM("a", ch), to_masked(ch, "N", m_su))
                hmm(FM("a", ch), FM("b", ch), to_masked(ch, "NT", m_sl))
                if own:
                    hmm(FM("b", ch), FM("r", ch), to_masked(ch, "Arb", m_ui))
                    hmm(FM("k", ch), FM("r", ch), to_masked(ch, "Ark", m_ui))
                hmm(FM("a", ch), FM("k", ch), to_masked(ch, "AakT", m_sl))
            for ch in range(2):
                tt("pool", CB[ch]["T"], CB[ch]["N"], id8[:], ALU.add, [CB[ch]["t_N"], t_id8], [CB[ch]["t_T"]])
            for ch in range(2):
                hmm(CBm(ch, "NT"), CBm(ch, "N"), to_plain(ch, "M"))
                hmm(CBm(ch, "N"), CBm(ch, "NT"), to_plain(ch, "MT"))
            cur = ("M", "MT", "T")
            alt = ("M2", "MT2", "T2")
            for jlev in range(1, 6):
                Mn, MTn, Tn = cur
                Mo, MTo, To = alt
                for ch in range(2):
                    def post_T(ps, tb, ch=ch, Tn=Tn, To=To):
                        tt("dve", CB[ch][To], ps, CB[ch][Tn], ALU.add, [tb, CB[ch]["t_" + Tn]],
                           [CB[ch]["t_" + To]])
                    hmm(CBm(ch, MTn), CBm(ch, Tn), post_T)
                    if jlev < 5:
                        hmm(CBm(ch, MTn), CBm(ch, Mn), to_plain(ch, Mo))
                        hmm(CBm(ch, Mn), CBm(ch, MTn), to_plain(ch, MTo))
                cur, alt = (Mo, MTo, To), (Mn, MTn, Tn)
            Tfin = cur[2]
            for ch in range(2):
                hmm(CBm(ch, Tfin), TM("a", ch), to_plain(ch, "P1T"))
                hmm(CBm(ch, Tfin), CBm(ch, "AakT"), to_plain(ch, "P2T"))
            t13 = tmp1[0:64, :].rearrange("p (h t) -> p h t", h=8)
            t23 = tmp2[0:64, :].rearrange("p (h t) -> p h t", h=8)
            for ch in range(2):
                dc3 = DCb[ch].rearrange("p (h t) -> p h t", h=8)

                def post_Q1(ps, tb, ch=ch):
                    tt("dve", CB[ch]["Q1"], ps, XF["r"][:, :, ch * 64:(ch + 1) * 64], ALU.add,
                       [tb, t_XF["r"]], [CB[ch]["t_Q1"]])
                if own:
                    hmm(CBm(ch, "P1T"), CBm(ch, "Arb"), post_Q1)

                def post_G1(ps, tb, ch=ch, dc3=dc3):
                    tt("dve", t13, ps, id8[:], ALU.add, [tb, t_id8], [t_tmp1])
                    tt("pool", CB[ch]["G1"], t13, dc3, ALU.mult, [t_tmp1, t_DC[ch]], [CB[ch]["t_G1"]])
                hmm(CBm(ch, "P1T"), TM("b", ch), post_G1)

                def post_Q2(ps, tb, ch=ch):
                    tt("dve", CB[ch]["Q2"], ps, CB[ch]["Ark"], ALU.add, [tb, CB[ch]["t_Ark"]], [CB[ch]["t_Q2"]])
                if own:
                    hmm(CBm(ch, "P2T"), CBm(ch, "Arb"), post_Q2)

                def post_G2(ps, tb, ch=ch, dc3=dc3):
                    kap, ktrk = tok("k", ch)
                    tt("dve", t23, ps, kap.rearrange("p (h t) -> p h t", h=8), ALU.add, [tb, ktrk], [t_tmp2])
                    tt("pool", CB[ch]["G2"], t23, dc3, ALU.mult, [t_tmp2, t_DC[ch]], [CB[ch]["t_G2"]])
                hmm(CBm(ch, "P2T"), TM("b", ch), post_G2)
            for ch in range(2):
                vap, vtrk = tok("v", ch)
                if own:
                    bky, tby = bank()
                    for h in range(8):
                        hs = slice(h * 64, (h + 1) * 64)
                        mm(bky[0:64, hs], CB[ch]["Q1"][:, h, :], stTb[:, hs], True, False, [CB[ch]["t_Q1"], t_stb], [tby])
                        mm(bky[0:64, hs], CB[ch]["Q2"][:, h, :], vap[:, hs], False, True, [CB[ch]["t_Q2"], vtrk], [tby], sig=(h == 7))
                bks, tbs = bank()
                for h in range(8):
                    hs = slice(h * 64, (h + 1) * 64)
                    mm(bks[0:64, hs], CB[ch]["G1"][:, h, :], stTb[:, hs], True, False, [CB[ch]["t_G1"], t_stb], [tbs])
                    mm(bks[0:64, hs], CB[ch]["G2"][:, h, :], vap[:, hs], False, True, [CB[ch]["t_G2"], vtrk], [tbs], sig=(h == 7))
                if own:
                    cp("act", yc[ch], bky[0:64, :], [tby], [t_yc[ch]])
                cp("dve", stTb[:], bks[0:64, :], [tbs], [t_stb])
            if not own:
                return
            bk, tb = bank()
            mm(bk[:, :], ident_f[0:64, :], yc[0], True, False, [t_id, t_yc[0]], [tb])
            mm(bk[:, :], sel_lo_hi[:], yc[1], False, True, [t_selhi, t_yc[1]], [tb])
            cp("act", y_f, bk[:, :], [tb], [t_y])
            P.op("dve", lambda e: e.tensor_reduce(out=sm[:, 32:40], in_=h3(y_f), axis=AX.X, op=ALU.add),
                 [t_y], [t_sm])
            ts("dve", sm[:, 32:40], sm[:, 32:40], 1.0 / 64, ALU.mult, [t_sm], [t_sm])
            tt("dve", h3(y_f), h3(y_f), sm[:, 32:40].unsqueeze(2).to_broadcast([128, 8, 64]), ALU.subtract,
               [t_y, t_sm], [t_y])
            act(tmp1, y_f, AF.Square, [t_y], [t_tmp1])
            P.op("dve", lambda e: e.tensor_reduce(out=sm[:, 40:48], in_=h3(tmp1), axis=AX.X, op=ALU.add),
                 [t_tmp1], [t_sm])
            rsqrt_inplace(sm[:, 40:48], t_sm, 1.0 / 64, 64e-5)
            tt("dve", h3(y_f), h3(y_f), sm[:, 40:48].unsqueeze(2).to_broadcast([128, 8, 64]), ALU.mult,
               [t_y, t_sm], [t_y])
            tt("pool", y_f, y_f, lng_bc[:], ALU.mult, [t_y, t_lng], [t_y])
            tt("pool", y_f, y_f, lnb_bc[:], ALU.add, [t_y, t_lnb], [t_y])
            tt("dve", h3(tmp2), h3(v_), bonus[:].unsqueeze(2).to_broadcast([128, 8, 64]), ALU.mult,
               [t_feat, t_bonus], [t_tmp2])
            tt("dve", y_f, y_f, tmp2, ALU.add, [t_y, t_tmp2], [t_y])
            j = t % 4
            tt("dve", mixr[:, j, :], y_f, rw_g, ALU.mult, [t_y, t_rwg], [t_mixr[j]])
            if dbg:
                tt("dve", tmp1, y_f, rw_g, ALU.mult, [t_y, t_rwg], [t_tmp1])
                P.dma("sp", dbgo["d_rw"][(t - OWN0) * 128:(t - OWN0 + 1) * 128, :], tmp1, t_tmp1[0], False)

        def attention(B):
            nk = 4 * B + 4
            LA = 2
            for hp in range(4):
                H2 = (2 * hp, 2 * hp + 1)
                bko = [banks[6], banks[7]]
                tbo = [btrk[6], btrk[7]]

                def qk(i, kt):
                    h = H2[i]
                    bks, tbs = bank()
                    ks = slice(kt * 128, (kt + 1) * 128)
                    mm(bks[:, :], ckvT_all[:, ks], qlatT[:, h, :], True, False, [t_kv[kt], t_qlat], [tbs])
                    mm(bks[:, :], krT_all[:, ks], qrT[:, h, :], False, True, [t_kv[kt], t_qrT], [tbs])
                    return bks, tbs
                pend = [[], []]
                for k_ in range(min(LA, nk)):
                    for i in range(2):
                        pend[i].append(qk(i, k_))
                for kt in range(nk):
                    m = kt - 4 * B
                    cur = [pend[i].pop(0) for i in range(2)]
                    if kt + LA < nk:
                        for i in range(2):
                            pend[i].append(qk(i, kt + LA))
                    pts = []
                    for i in range(2):
                        h = H2[i]
                        bks, tbs = cur[i]
                        pt, tpt = pT2[i][kt % 3], t_pT2[i][kt % 3]
                        pts.append((pt, tpt))
                        act(pt, bks[:, :], AF.Exp, [tbs, t_kv[kt], t_nb], [tpt], scale=sc_all[:, kt, h:h + 1],
                            bias=kb_all[:, kt:kt + 1])
                        if m >= 0:
                            P.op("pool", lambda e, pt=pt, m=m: e.affine_select(
                                out=pt, in_=pt, pattern=[[1, 512]], compare_op=ALU.is_ge, fill=0.0,
                                base=-m * 128, channel_multiplier=-1), [tpt], [tpt])
                    for i in range(2):
                        pt, tpt = pts[i]
                        mm(bko[i][:, :], ckv_tok[:, kt, :], pt, kt == 0, kt == nk - 1, [t_kv[kt], tpt], [tbo[i]],
                           sig=(i == 1 and kt + LA >= nk))
                        if kt == 0:
                            cp("dve", den2[i], pt, [tpt], [t_den2[i]])
                        else:
                            tt("dve", den2[i], den2[i], pt, ALU.add, [t_den2[i], tpt], [t_den2[i]])
                for i in range(2):
                    h = H2[i]
                    cp("act", oT2[i], bko[i][:, :], [tbo[i]], [t_oT2[i]])
                    bkf, tbf = bank()
                    for j in range(4):
                        mm(bkf[:, j * 66:j * 66 + 64], oT2[i][:, j * 128:(j + 1) * 128], Wuv[:, h * 64:(h + 1) * 64],
                           True, True, [t_oT2[i], t_Wuv], [tbf], sig=False)
                        mm(bkf[:, j * 66 + 64:j * 66 + 65], den2[i][:, j * 128:(j + 1) * 128], ones_c[:, 0:1], True, True,
                           [t_den2[i], t_ones], [tbf], sig=(j == 3))
                    acc3 = bkf[:, 0:264].rearrange("p (j d) -> p j d", j=4)
                    rd = rden[:, i * 4:(i + 1) * 4]
                    P.op("dve", lambda e, acc3=acc3, rd=rd: e.reciprocal(out=rd.unsqueeze(2), in_=acc3[:, :, 64:65]),
                         [tbf], [t_rden])
                    tt("dve", attn[:, :, h * 64:(h + 1) * 64], acc3[:, :, 0:64],
                       rd.unsqueeze(2).to_broadcast([128, 4, 64]), ALU.mult, [tbf, t_rden], [t_attn])
            for j in range(4):
                act(junk[:, 0:512], attn[:, j, :], AF.Square, [t_attn], [t_junk, t_sm], accum=sm[:, 4 + j:5 + j])
            rsqrt_inplace(sm[:, 4:8], t_sm, 1.0 / 512, 1e-6)
            for j in range(4):
                t = 4 * B + j
                ts("dve", mixm[:, j, :], attn[:, j, :], sm[:, 4 + j:5 + j], ALU.mult, [t_attn, t_sm], [t_mixm])
                if dbg:
                    ts("dve", tmp1, attn[:, j, :], sm[:, 4 + j:5 + j], ALU.mult, [t_attn, t_sm], [t_tmp1])
                    P.dma("sp", dbgo["d_mla"][(t - OWN0) * 128:(t - OWN0 + 1) * 128, :], tmp1, t_tmp1[0], False)
            for half in range(2):
                for c in range(8):
                    wst, t_wst = (wst0, t_wst0) if c % 2 == 0 else (wst1, t_wst1)
                    P.dma("sp", wst, dr["w_o"][c * 128:(c + 1) * 128, half * 512:(half + 1) * 512],
                          t_wst[0], True)
                    if c < 4:
                        ts("dve", Woh[:, c, :], wst, gmo_c[:, c:c + 1], ALU.mult, [t_wst, t_gmo], [t_Woh])
                    else:
                        cp("dve", Woh[:, c, :], wst, [t_wst], [t_Woh])
                for j in range(4):
                    t = 4 * B + j
                    if half == 0:
                        pass
                    bk, tb = bank()
                    bkb = bk[:].bitcast(BF16)
                    for c in range(8):
                        src_ = mixm[:, j, c * 128:(c + 1) * 128] if c < 4 else mixr[:, j, (c - 4) * 128:(c - 3) * 128]
                        tr(bkb[:, c * 128:(c + 1) * 128], src_, ident_b[:], [t_mixm, t_mixr[j], t_id], [tb], sig=(c == 7))
                    cp("act", mixT_, bkb[:, :], [tb], [t_mixT])
                    P.dma("pool", xr[:, 0:512], x[t * 128:(t + 1) * 128, half * 512:(half + 1) * 512], t_xr[0], True)
                    bk, tb = bank()
                    for c in range(8):
                        mm(bk[:, :], mixT[:, c, :], Woh[:, c, :], c == 0, c == 7, [t_mixT, t_Woh], [tb])
                    tt("dve", xm[:, 0:512], bk[:, :], xr[:, 0:512], ALU.add, [tb, t_xr[0]], [t_xm[0]])
                    P.dma("sp", xmid[(t - OWN0) * 128:(t - OWN0 + 1) * 128, half * 512:(half + 1) * 512], xm[:, 0:512], t_xm[0], False)
                    if dbg:
                        P.dma("sp", dbgo["d_xmid"][(t - OWN0) * 128:(t - OWN0 + 1) * 128, half * 512:(half + 1) * 512], xm[:, 0:512],
                              t_xm[0], False)

        def moe_phase():
            TB = min(2048, SO)
            TBT = TB // 128
            NB = SO // TB
            NG = TB // 512
            yacc = sb("yacc", [128, TBT, D]); t_yacc = [Trk() for _ in range(TBT)]
            xT = sb("xT", [128, 8, TB], BF16); t_xT = [Trk() for _ in range(TBT)]
            hn_f = sb("hn_f", [128, D]); t_hnf = Trk()
            hn_b = sb("hn_b", [128, D], BF16); t_hnb = Trk()
            hnT_f = sb("hnT_f", [128, 8, 128]); t_hnT = Trk()
            mjunk = sb("mjunk", [128, D], BF16); t_mj = Trk()
            gffn_bc = sb("gffn_bc", [128, D]); t_gfb = Trk()
            P.dma("sp", gffn_bc[:], dr["g_ffn"].partition_broadcast(128), t_gfb, True)
            bgb = sb("bgb", [128, 36]); t_bgb = Trk()
            P.dma("sp", bgb[:, 0:4], dr["b_group"].partition_broadcast(128), t_bgb, True)
            P.dma("sp", bgb[:, 4:36], dr["b_expert"].partition_broadcast(128), t_bgb, True)
            Wr = sb("Wr", [128, 8, 36]); t_Wr = Trk()
            P.dma("sp", Wr[:, :, 0:4], dr["w_group"].rearrange("(c p) g -> p c g", p=128), t_Wr, True)
            P.dma("sp", Wr[:, :, 4:36], dr["w_expert"].rearrange("(c p) g -> p c g", p=128), t_Wr, True)
            lg = sb("lg", [128, 36]); t_lg = Trk()
            ms = sb("ms", [128, 32]); t_ms = Trk()
            r1 = sb("r1", [128, 32]); r2 = sb("r2", [128, 32]); r3 = sb("r3", [128, 32]); r4 = sb("r4", [128, 32])
            t_r = Trk()
            comb = sb("comb", [128, TBT, NE]); t_comb = [Trk() for _ in range(TBT)]
            wsg = [sb("wsg%d" % i, [128, 8, DE]) for i in range(2)]
            wsu = [sb("wsu%d" % i, [128, 8, DE]) for i in range(2)]
            wsd = [sb("wsd%d" % i, [128, 2, D]) for i in range(2)]
            t_wsg = [Trk(), Trk()]; t_wsu = [Trk(), Trk()]; t_wsd = [Trk(), Trk()]
            wbg = [sb("wbg%d" % i, [128, 8, DE], BF16) for i in range(2)]
            wbu = [sb("wbu%d" % i, [128, 8, DE], BF16) for i in range(2)]
            wbd = [sb("wbd%d" % i, [128, 2, D], BF16) for i in range(2)]
            t_wbg = [Trk(), Trk()]; t_wbu = [Trk(), Trk()]; t_wbd = [Trk(), Trk()]
            sg = [sb("sg%d" % i, [128, 512]) for i in range(2)]; t_sg = [Trk(), Trk()]
            hTb = [sb("hTb%d" % i, [128, 2, 512], BF16) for i in range(2)]; t_hTb = [Trk(), Trk()]
            flat = lambda ap: ap.rearrange("p a b -> p (a b)")
            wq = {"i": 0}

            def load_expert(e):
                i = e % 2
                q1 = "sp" if wq["i"] % 2 == 0 else "pool"
                q2 = "pool" if wq["i"] % 2 == 0 else "sp"
                wq["i"] += 1
                P.dma(q1, wsg[i][:], dr["w_gate"][e].rearrange("(c p) f -> p c f", p=128), t_wsg[i], True)
                P.dma(q2, wsu[i][:], dr["w_up"][e].rearrange("(c p) f -> p c f", p=128), t_wsu[i], True)
                P.dma(q1, wsd[i][:], dr["w_down"][e].rearrange("(c p) n -> p c n", p=128), t_wsd[i], True)
                cp("pool", flat(wbg[i][:]), flat(wsg[i][:]), [t_wsg[i]], [t_wbg[i]])
                cp("pool", flat(wbu[i][:]), flat(wsu[i][:]), [t_wsu[i]], [t_wbu[i]])
                cp("act", flat(wbd[i][:]), flat(wsd[i][:]), [t_wsd[i]], [t_wbd[i]])

            for blk in range(NB):
                for ti in range(TBT):
                    tok0 = blk * TB + ti * 128
                    P.dma("sp" if ti % 2 == 0 else "pool", yacc[:, ti, :], xmid[tok0:tok0 + 128, :], t_yacc[ti], True)
                    act(mjunk[:], yacc[:, ti, :], AF.Square, [t_yacc[ti]], [t_mj, t_ms], accum=ms[:, 0:1])
                    rsqrt_inplace(ms[:, 0:1], t_ms, 1.0 / D, 1e-6)
                    ts("dve", hn_f[:], yacc[:, ti, :], ms[:, 0:1], ALU.mult, [t_yacc[ti], t_ms], [t_hnf])
                    tt("pool", hn_f[:], hn_f[:], gffn_bc[:], ALU.mult, [t_hnf, t_gfb], [t_hnf])
                    cp("act", hn_b[:], hn_f[:], [t_hnf], [t_hnb])
                    bk, tb = bank()
                    bkb = bk[:].bitcast(BF16)
                    for c in range(8):
                        tr(bkb[:, c * 128:(c + 1) * 128], hn_b[:, c * 128:(c + 1) * 128], ident_b[:], [t_hnb, t_id], [tb], sig=(c == 7))
                    cp("act", xT[:, :, ti * 128:(ti + 1) * 128], bkb[:, :].rearrange("p (c t) -> p c t", c=8),
                       [tb], [t_xT[ti]])
                    for hh in range(2):
                        bk, tb = bank()
                        for c4 in range(4):
                            c = hh * 4 + c4
                            tr(bk[:, c4 * 128:(c4 + 1) * 128], hn_f[:, c * 128:(c + 1) * 128], ident_f[:],
                               [t_hnf, t_id], [tb], sig=(c4 == 3))
                        cp("dve", hnT_f[:, hh * 4:(hh + 1) * 4, :], bk[:, :].rearrange("p (c t) -> p c t", c=4),
                           [tb], [t_hnT])
                    bk, tb = bank()
                    for c in range(8):
                        mm(bk[:, 0:36], hnT_f[:, c, :], Wr[:, c, :], c == 0, c == 7, [t_hnT, t_Wr], [tb])
                    tt("dve", lg[:], bk[:, 0:36], bgb[:], ALU.add, [tb, t_bgb], [t_lg])
                    P.op("dve", lambda e: e.tensor_reduce(out=ms[:, 1:2], in_=lg[:, 0:4], axis=AX.X, op=ALU.max),
                         [t_lg], [t_ms])
                    ts("dve", r1[:, 0:4], lg[:, 0:4], ms[:, 1:2], ALU.is_equal, [t_lg, t_ms], [t_r])
                    ts("dve", ms[:, 2:3], ms[:, 1:2], -1.0, ALU.mult, [t_ms], [t_ms])
                    act(r2[:, 0:4], lg[:, 0:4], AF.Exp, [t_lg, t_ms], [t_r, t_ms], bias=ms[:, 2:3], accum=ms[:, 3:4])
                    P.op("dve", lambda e: e.reciprocal(out=ms[:, 3:4], in_=ms[:, 3:4]), [t_ms], [t_ms])
                    ts("dve", r1[:, 4:8], r1[:, 0:4], -1.0, ALU.add, [t_r], [t_r], s2=1e30, op1=ALU.mult)
                    tt("dve", r3[:].rearrange("p (g e) -> p g e", g=4), lg[:, 4:36].rearrange("p (g e) -> p g e", g=4),
                       r1[:, 4:8].unsqueeze(2).to_broadcast([128, 4, 8]), ALU.add, [t_lg, t_r], [t_r])
                    P.op("dve", lambda e: e.tensor_reduce(out=ms[:, 4:5], in_=r3[:], axis=AX.X, op=ALU.max),
                         [t_r], [t_ms])
                    ts("dve", r2[:], r3[:], ms[:, 4:5], ALU.is_equal, [t_r, t_ms], [t_r])
                    stt(r4[:], r2[:], -1e30, r3[:], ALU.mult, ALU.add, [t_r], [t_r])
                    P.op("dve", lambda e: e.tensor_reduce(out=ms[:, 5:6], in_=r4[:], axis=AX.X, op=ALU.max),
                         [t_r], [t_ms])
                    ts("dve", r3[:], r4[:], ms[:, 5:6], ALU.is_equal, [t_r, t_ms], [t_r])
                    tt("dve", ms[:, 6:7], ms[:, 5:6], ms[:, 4:5], ALU.subtract, [t_ms], [t_ms])
                    act(ms[:, 6:7], ms[:, 6:7], AF.Exp, [t_ms], [t_ms])
                    ts("dve", ms[:, 7:8], ms[:, 6:7], 1.0, ALU.add, [t_ms], [t_ms])
                    P.op("dve", lambda e: e.reciprocal(out=ms[:, 7:8], in_=ms[:, 7:8]), [t_ms], [t_ms])
                    tt("dve", ms[:, 8:9], ms[:, 6:7], ms[:, 7:8], ALU.mult, [t_ms], [t_ms])
                    tt("dve", ms[:, 7:8], ms[:, 7:8], ms[:, 3:4], ALU.mult, [t_ms], [t_ms])
                    tt("dve", ms[:, 8:9], ms[:, 8:9], ms[:, 3:4], ALU.mult, [t_ms], [t_ms])
                    ts("dve", r4[:], r2[:], ms[:, 7:8], ALU.mult, [t_r, t_ms], [t_r])
                    stt(comb[:, ti, :], r3[:], ms[:, 8:9], r4[:], ALU.mult, ALU.add, [t_r, t_ms], [t_comb[ti]])
                for e in range(NE):
                    i = e % 2
                    load_expert(e)
                    for grp in range(NG):
                        gs = slice(grp * 512, (grp + 1) * 512)
                        xtr = [t_xT[grp * 4 + j] for j in range(4)]
                        hb_, thb_ = hTb[grp % 2], t_hTb[grp % 2]
                        for fc in range(2):
                            bkg, tbg = bank()
                            for c in range(8):
                                mm(bkg[:, :], wbg[i][:, c, fc * 128:(fc + 1) * 128], xT[:, c, gs], c == 0, c == 7,
                                   [t_wbg[i], xtr], [tbg])
                            bku, tbu = bank()
                            for c in range(8):
                                mm(bku[:, :], wbu[i][:, c, fc * 128:(fc + 1) * 128], xT[:, c, gs], c == 0, c == 7,
                                   [t_wbu[i], xtr], [tbu])
                            act(sg[fc][:], bkg[:, :], AF.Silu, [tbg], [t_sg[fc]])
                            tt("dve", hb_[:, fc, :], bku[:, :], sg[fc][:], ALU.mult, [tbu, t_sg[fc]], [thb_])
                        for j in range(4):
                            ti = grp * 4 + j
                            for half in range(2):
                                bk, tb = bank()
                                for fc in range(2):
                                    mm(bk[:, :], hb_[:, fc, j * 128:(j + 1) * 128],
                                       wbd[i][:, fc, half * 512:(half + 1) * 512], fc == 0, fc == 1,
                                       [thb_, t_wbd[i]], [tb])
                                ysl = yacc[:, ti, half * 512:(half + 1) * 512]
                                stt(ysl, bk[:, :], comb[:, ti, e:e + 1], ysl, ALU.mult, ALU.add,
                                    [tb, t_comb[ti], t_yacc[ti]], [t_yacc[ti]])
                for ti in range(TBT):
                    tok0 = blk * TB + ti * 128
                    P.dma("sp" if ti % 2 == 0 else "pool", out[tok0:tok0 + 128, :], yacc[:, ti, :], t_yacc[ti], False)

        P.barrier()
        for t in range(NT):
            phase1(t)
            rwkv(t)
            if t % 4 == 3 and t >= OWN0:
                for j in range(4):
                    qside(j)
                attention(t // 4)
        P.barrier()
        mes.close()
        cures["es"] = es
        moe_phase()
        P.final_wait("sp")
        with nc.Block() as block:
            P.emit(block)
    return nc


_CACHE = {}


def kernel(**inputs):
    NT = 64
    if NT not in _CACHE:
        _CACHE[NT] = build(NT)
    nc = _CACHE[NT]
    x = np.asarray(inputs["x"], dtype=np.float32)
    pos = np.asarray(inputs["positions"], dtype=np.int32)
    w = {}
    for n, shp in WNAMES:
        w[n] = np.ascontiguousarray(np.asarray(inputs[n], dtype=np.float32).reshape(shp))
    H = 4096
    in_maps = []
    for c in range(8):
        b, p = c // 2, c % 2
        kb = np.full((128, NT), -8.0, np.float32)
        if p == 1:
            xc = x[b]
            pc = pos[b]
        else:
            xc = np.concatenate([np.zeros((H, D), np.float32), x[b, :H]], axis=0)
            pc = np.concatenate([np.zeros((H,), np.int32), pos[b, :H]], axis=0)
            kb[:, :NT // 2] = -30000.0
        m = {"x": np.ascontiguousarray(xc), "pos": np.ascontiguousarray(pc), "kbias": kb}
        m.update(w)
        in_maps.append(m)
    res = run_bass_kernel_spmd(nc, in_maps, core_ids=list(range(8)))
    out = np.empty((4, 2 * H, D), np.float32)
    for c in range(8):
        b, p = c // 2, c % 2
        out[b, p * H:(p + 1) * H] = res.results[c]["out"]
    return out
```

```python
import math
from contextlib import ExitStack

import numpy as np
import concourse.bass as bass
import concourse.mybir as mybir
from concourse.bass_utils import run_bass_kernel_spmd

F32 = mybir.dt.float32
BF16 = mybir.dt.bfloat16
I32 = mybir.dt.int32
AF = mybir.ActivationFunctionType
ALU = mybir.AluOpType
AX = mybir.AxisListType

D = 1024
NH = 8
QK = 96
NOPE = 64
ROPE = 32
INW = 2208
NE = 32
DE = 256
C0 = -math.exp(-0.5)


class Trk:
    __slots__ = ("w", "r", "dsem", "dcnt", "q")

    def __init__(self):
        self.w = None
        self.r = {}
        self.dsem = None
        self.dcnt = 0
        self.q = None


class Prog:
    ENG = ("pe", "act", "dve", "pool", "sp")

    def __init__(self, nc):
        self.nc = nc
        self.streams = {e: [] for e in self.ENG}
        self.cnt = {e: 0 for e in self.ENG}
        self.sems = {}
        for e in ("pe", "act", "dve", "pool"):
            self.sems[e] = nc.alloc_semaphore(name="s_" + e)
        self.seen = {e: {} for e in self.ENG}
        self.ndsem = 0
        self.dtrk = []
        self.pending = {e: {} for e in self.ENG}

    def _need(self, eng, waits, key, val):
        if val <= 0 or self.seen[eng].get(key, 0) >= val:
            return
        if key == eng and val > self.cnt[eng]:
            return
        if waits.get(key, 0) < val:
            waits[key] = val

    def _deps(self, eng, reads, writes):
        waits = {}
        if self.pending[eng]:
            for k, v in self.pending[eng].items():
                self._need(eng, waits, k, v)
            self.pending[eng] = {}
        for t in reads:
            if t.w is not None:
                self._need(eng, waits, t.w[0], t.w[1])
        for t in writes:
            if t.w is not None:
                self._need(eng, waits, t.w[0], t.w[1])
            for k, v in t.r.items():
                self._need(eng, waits, k, v)
        for k, v in waits.items():
            self.seen[eng][k] = v
        return waits

    @staticmethod
    def _flat(ts):
        o = []
        for t in ts:
            if isinstance(t, (list, tuple)):
                o.extend(Prog._flat(t))
            else:
                o.append(t)
        return o

    def op(self, eng, fn, reads=(), writes=(), sig=True):
        reads = self._flat(reads)
        writes = self._flat(writes)
        waits = self._deps(eng, reads, writes)
        if sig:
            self.cnt[eng] += 1
            val = self.cnt[eng]
        else:
            val = self.cnt[eng] + 1
        for t in reads:
            if t.r.get(eng, 0) < val:
                t.r[eng] = val
        for t in writes:
            t.w = (eng, val)
            t.r = {}
        self.streams[eng].append((fn, waits, (eng, 1) if sig else None))

    def dma(self, q, out, in_, sb, load, **kw):
        if sb.q is None:
            sb.q = q
        q = sb.q
        if sb.dsem is None:
            sb.dsem = self.nc.alloc_semaphore(name="d%d" % self.ndsem)
            self.ndsem += 1
            self.dtrk.append(sb)
        key = ("d", id(sb))
        self.sems[key] = sb.dsem
        ex = self._flat(kw.pop("extra", []))
        reads, writes = (list(ex), [sb]) if load else ([sb], list(ex))
        if not load:
            reads, writes = [sb] + [], []
            reads = [sb]
            reads = [sb] + list(ex)
        else:
            writes = [sb] + list(ex)
            reads = []
        waits = self._deps(q, reads, writes)
        sb.dcnt += 16
        val = sb.dcnt
        for t in writes:
            t.w = (key, val)
            t.r = {}
        for t in reads:
            if t.r.get(key, 0) < val:
                t.r[key] = val

        def fn(e, out=out, in_=in_, kw=kw):
            return e.dma_start(out=out, in_=in_, **kw)
        self.streams[q].append((fn, waits, (key, 16)))

    def barrier(self):
        waits = {}
        for e in ("pe", "act", "dve", "pool"):
            if self.cnt[e] > 0:
                waits[e] = self.cnt[e]
        for t in self.dtrk:
            waits[("d", id(t))] = t.dcnt
        for e in self.ENG:
            for k, v in waits.items():
                if self.pending[e].get(k, 0) < v:
                    self.pending[e][k] = v

    def final_wait(self, eng="sp"):
        waits = {}
        for t in self.dtrk:
            self._need(eng, waits, ("d", id(t)), t.dcnt)

        def fn(e, waits=waits):
            for k, v in waits.items():
                e.wait_ge(self.sems[k], v)
            return None
        self.streams[eng].append((fn, {}, None))

    def emit(self, block):
        engmap = {"pe": "tensor", "act": "scalar", "dve": "vector", "pool": "gpsimd", "sp": "sync"}
        for e in self.ENG:
            stream = self.streams[e]
            if not stream:
                continue

            def body(engobj, stream=stream):
                for fn, waits, inc in stream:
                    for k, v in waits.items():
                        engobj.wait_ge(self.sems[k], v)
                    ins = fn(engobj)
                    if inc is not None:
                        ins.then_inc(self.sems[inc[0]], inc[1])
            getattr(block, engmap[e])(body)


WNAMES = [
    ("g_mix", [D]), ("w_in", [D, INW]), ("g_cq", [256]), ("w_uq", [256, 768]), ("g_ckv", [128]),
    ("w_uk", [128, 512]), ("w_uv", [128, 512]), ("g_qn", [QK]), ("g_kn", [QK]), ("g_mla_out", [512]),
    ("rw_mu", [1792]), ("rw_w0", [512]), ("rw_w2", [64, 512]), ("rw_a0", [512]), ("rw_a2", [64, 512]),
    ("rw_g2", [128, 512]), ("rw_k_k", [512]), ("rw_k_a", [512]), ("rw_r_k", [512]), ("rw_lnx_g", [512]),
    ("rw_lnx_b", [512]), ("w_o", [D, D]), ("g_ffn", [D]), ("w_group", [D, 4]), ("b_group", [4]),
    ("w_expert", [D, NE]), ("b_expert", [NE]), ("w_gate", [NE, D, DE]), ("w_up", [NE, D, DE]),
    ("w_down", [NE, DE, D]),
]


def build(NT, dbg=False):
    S = NT * 128
    OWN0 = NT // 2
    SO = (NT - OWN0) * 128
    nc = bass.Bass("TRN2", target_bir_lowering=False)
    dr = {}
    dr["x"] = nc.dram_tensor("x", [S, D], F32, kind="ExternalInput").ap()
    dr["pos"] = nc.dram_tensor("pos", [S], I32, kind="ExternalInput").ap()
    for n, shp in WNAMES:
        dr[n] = nc.dram_tensor(n, shp, F32, kind="ExternalInput").ap()
    dr["kbias"] = nc.dram_tensor("kbias", [128, NT], F32, kind="ExternalInput").ap()
    out = nc.dram_tensor("out", [SO, D], F32, kind="ExternalOutput").ap()
    xmid = nc.dram_tensor("xmid", [SO, D], F32, kind="Internal").ap()
    dbgo = {}
    if dbg:
        for n, w in (("d_mla", 512), ("d_rw", 512), ("d_xmid", 1024)):
            dbgo[n] = nc.dram_tensor(n, [SO, w], F32, kind="ExternalOutput").ap()
    P = Prog(nc)
    x = dr["x"]

    with ExitStack() as es:
        mes = ExitStack()
        cures = {"es": es}

        def sb(name, shape, dt=F32):
            return cures["es"].enter_context(nc.sbuf_tensor(name, shape, dt))

        banks = [es.enter_context(nc.psum_tensor("bank%d" % i, [128, 512], F32)) for i in range(8)]
        btrk = [Trk() for _ in range(8)]
        bstate = {"i": 0}

        def bank():
            n = bstate.get("n", 8)
            i = bstate["i"] % n
            bstate["i"] = (i + 1) % n
            return banks[i], btrk[i]

        def mm(o, lhsT, rhs, start, stop, reads, writes, sig=None):
            P.op("pe", lambda e: e.matmul(o, lhsT=lhsT, rhs=rhs, start=start, stop=stop,
                                          skip_group_check=True), reads, writes, sig=(stop if sig is None else sig))

        def tr(o, in_, ident, reads, writes, sig=True):
            P.op("pe", lambda e: e.transpose(out=o, in_=in_, identity=ident), reads, writes, sig=sig)

        def cp(eng, o, i, reads, writes):
            if eng == "act":
                P.op("act", lambda e: e.activation(out=o, in_=i, func=AF.Copy), reads, writes)
            else:
                P.op(eng, lambda e: e.tensor_copy(out=o, in_=i), reads, writes)

        def tt(eng, o, a, b, op_, reads, writes):
            P.op(eng, lambda e: e.tensor_tensor(out=o, in0=a, in1=b, op=op_), reads, writes)

        def ts(eng, o, a, s1, op0, reads, writes, s2=None, op1=None):
            if op1 is None:
                P.op(eng, lambda e: e.tensor_scalar(out=o, in0=a, scalar1=s1, scalar2=None, op0=op0), reads, writes)
            else:
                P.op(eng, lambda e: e.tensor_scalar(out=o, in0=a, scalar1=s1, scalar2=s2, op0=op0, op1=op1), reads, writes)

        def stt(o, a, s, b, op0, op1, reads, writes):
            P.op("dve", lambda e: e.scalar_tensor_tensor(out=o, in0=a, scalar=s, in1=b, op0=op0, op1=op1), reads, writes)

        def act(o, i, func, reads, writes, scale=None, bias=None, accum=None):
            kw = {}
            if scale is not None:
                kw["scale"] = scale
            if bias is not None:
                kw["bias"] = bias
            if accum is not None:
                kw["accum_out"] = accum
            P.op("act", lambda e: e.activation(out=o, in_=i, func=func, **kw), reads, writes)

        def rsqrt_inplace(v, tv, mult, eps):
            ts("dve", v, v, mult, ALU.mult, [tv], [tv], s2=eps, op1=ALU.add)
            act(v, v, AF.Sqrt, [tv], [tv])
            P.op("dve", lambda e: e.reciprocal(out=v, in_=v), [tv], [tv])

        ones_f = sb("ones_f", [128, 512], BF16); t_ones = Trk()
        P.op("pool", lambda e: e.memset(ones_f[:], 1.0), [], [t_ones])
        ones_c = sb("ones_c", [128, 1])
        P.op("pool", lambda e: e.memset(ones_c[:], 1.0), [], [t_ones])
        ident_f = sb("ident_f", [128, 128]); ident_b = sb("ident_b", [128, 128], BF16); t_id = Trk()
        P.op("pool", lambda e: e.affine_select(out=ident_f[:], in_=ones_f[:, 0:128], pattern=[[-1, 128]],
                                               compare_op=ALU.is_equal, fill=0.0, base=0, channel_multiplier=1),
             [t_ones], [t_id])
        cp("dve", ident_b[:], ident_f[:], [t_id], [t_id])
        cures["es"] = mes
        tri_f = sb("tri_f", [128, 128]); t_tri = Trk()
        P.op("pool", lambda e: e.affine_select(out=tri_f[:], in_=ones_f[:, 0:128], pattern=[[1, 128]],
                                               compare_op=ALU.is_ge, fill=0.0, base=0, channel_multiplier=-1),
             [t_ones], [t_tri])
        P.op("pool", lambda e: e.memset(tri_f[0:64, 64:128], 0.0), [], [t_tri])
        sel63 = sb("sel63", [128, 64]); sel127 = sb("sel127", [128, 64]); t_sel = Trk()
        P.op("pool", lambda e: e.affine_select(out=sel63[:], in_=ones_f[:, 0:64], pattern=[[0, 64]],
                                               compare_op=ALU.is_equal, fill=0.0, base=-63, channel_multiplier=1),
             [t_ones], [t_sel])
        P.op("pool", lambda e: e.affine_select(out=sel127[:], in_=ones_f[:, 0:64], pattern=[[0, 64]],
                                               compare_op=ALU.is_equal, fill=0.0, base=-127, channel_multiplier=1),
             [t_ones], [t_sel])
        shift_hi = sb("shift_hi", [128, 64], BF16); t_sh = Trk()
        P.op("pool", lambda e: e.affine_select(out=shift_hi[:], in_=ones_f[:, 0:64], pattern=[[-1, 64]],
                                               compare_op=ALU.is_equal, fill=0.0, base=-64, channel_multiplier=1),
             [t_ones], [t_sh])
        m_su = sb("m_su", [64, 8, 64], BF16); m_ui = sb("m_ui", [64, 8, 64], BF16); m_sl = sb("m_sl", [64, 8, 64], BF16); t_msk = Trk()
        ones3 = ones_f[0:64, :].rearrange("p (h t) -> p h t", h=8)
        P.op("pool", lambda e: e.affine_select(out=m_su[:], in_=ones3, pattern=[[0, 8], [1, 64]],
                                               compare_op=ALU.is_gt, fill=0.0, base=0, channel_multiplier=-1),
             [t_ones], [t_msk])
        P.op("pool", lambda e: e.affine_select(out=m_ui[:], in_=ones3, pattern=[[0, 8], [1, 64]],
                                               compare_op=ALU.is_ge, fill=0.0, base=0, channel_multiplier=-1),
             [t_ones], [t_msk])
        P.op("pool", lambda e: e.affine_select(out=m_sl[:], in_=ones3, pattern=[[0, 8], [-1, 64]],
                                               compare_op=ALU.is_gt, fill=0.0, base=0, channel_multiplier=1),
             [t_ones], [t_msk])
        id8 = sb("id8", [64, 8, 64], BF16); t_id8 = Trk()
        P.op("pool", lambda e: e.affine_select(out=id8[:], in_=ones3, pattern=[[0, 8], [-1, 64]],
                                               compare_op=ALU.is_equal, fill=0.0, base=0, channel_multiplier=1),
             [t_ones], [t_id8])
        sel_lo_hi = sb("sel_lo_hi", [64, 128]); t_selhi = Trk()
        P.op("pool", lambda e: e.affine_select(out=sel_lo_hi[:], in_=ones_f[0:64, 0:128], pattern=[[1, 128]],
                                               compare_op=ALU.is_equal, fill=0.0, base=-64, channel_multiplier=-1),
             [t_ones], [t_selhi])
        jidx = sb("jidx", [128, 16]); invf = sb("invf", [128, 16]); t_invf = Trk()
        P.op("pool", lambda e: e.iota(jidx[:], pattern=[[1, 16]], base=0, channel_multiplier=0,
                                      allow_small_or_imprecise_dtypes=True), [], [t_invf])
        act(invf[:], jidx[:], AF.Exp, [t_invf], [t_invf], scale=-math.log(10000.0) / 16.0)

        t_vec = Trk()

        def bc_load(name, src, n):
            tile_ = sb(name, [128, n])
            trk = Trk()
            P.dma("sp", tile_[:], src.partition_broadcast(128), trk, True)
            return tile_, trk

        def col_load(name, src, ncol):
            tile_ = sb(name, [128, ncol])
            trk = Trk()
            P.dma("sp", tile_[:], src.rearrange("(c p) -> p c", p=128), trk, True,
                  allow_slow_non_contiguous=True)
            return tile_, trk

        gmix_c, t_gmix = col_load("gmix_c", dr["g_mix"], 8)
        gcq_c, t_gcq = col_load("gcq_c", dr["g_cq"], 2)
        gckv_c, t_gckv = col_load("gckv_c", dr["g_ckv"], 1)
        gmo_c, t_gmo = col_load("gmo_c", dr["g_mla_out"], 4)
        mu_bc, t_mu = bc_load("mu_bc", dr["rw_mu"][0:1536], 1536)
        mul_c = sb("mul_c", [128, 3]); t_mul = Trk()
        P.dma("sp", mul_c[0:64, 0:1], dr["rw_mu"][1536:1600].rearrange("(p o) -> p o", o=1), t_mul, True)
        P.dma("sp", mul_c[0:64, 1:2], dr["rw_mu"][1600:1664].rearrange("(p o) -> p o", o=1), t_mul, True)
        P.dma("sp", mul_c[:, 2:3], dr["rw_mu"][1664:1792].rearrange("(p o) -> p o", o=1), t_mul, True)
        w0_bc, t_w0 = bc_load("w0_bc", dr["rw_w0"], 512)
        a0_bc, t_a0 = bc_load("a0_bc", dr["rw_a0"], 512)
        kk_bc, t_kk = bc_load("kk_bc", dr["rw_k_k"], 512)
        ka_bc, t_ka = bc_load("ka_bc", dr["rw_k_a"], 512)
        rk_bc, t_rk = bc_load("rk_bc", dr["rw_r_k"], 512)
        lng_bc, t_lng = bc_load("lng_bc", dr["rw_lnx_g"], 512)
        lnb_bc, t_lnb = bc_load("lnb_bc", dr["rw_lnx_b"], 512)
        gqn_bc, t_gqn = bc_load("gqn_bc", dr["g_qn"], QK)
        gkn_bc, t_gkn = bc_load("gkn_bc", dr["g_kn"], QK)
        gqk_bc = sb("gqk_bc", [128, 64]); t_gqk = Trk()
        tt("dve", gqk_bc[:], gqn_bc[:, 0:64], gkn_bc[:, 0:64], ALU.mult, [t_gqn, t_gkn], [t_gqk])

        NPG = 31
        AR = sb("arena", [128, NPG * 512])
        ARb = AR[:].bitcast(BF16)
        pg_t = [Trk() for _ in range(NPG)]

        def af(p0, ncols, rows=128):
            npg = (ncols + 511) // 512
            return AR[0:rows, p0 * 512:p0 * 512 + ncols], pg_t[p0:p0 + npg]

        def ab(p0, off, ncols, rows=128):
            c0 = p0 * 1024 + off
            p1 = (c0 + ncols - 1) // 1024
            return ARb[0:rows, c0:c0 + ncols], pg_t[c0 // 1024:p1 + 1]

        stage = [AR[:, 0:2208], AR[:, 9 * 512:9 * 512 + 2208]]
        t_stage = [pg_t[0:5], pg_t[9:14]]
        sstate = {"i": 0}

        def load_scaled(dst, dst_trk, src_ap, ncols, rows=128, scale_col=None, scale_trk=None, q="sp"):
            i = sstate["i"]
            sstate["i"] = 1 - i
            st, tst = stage[i], t_stage[i]
            P.dma(q, st[0:rows, 0:ncols], src_ap, tst[0], True, extra=tst[1:])
            if scale_col is None:
                cp("dve", dst, st[0:rows, 0:ncols], [tst], [dst_trk])
            else:
                ts("dve", dst, st[0:rows, 0:ncols], scale_col, ALU.mult, [tst, scale_trk], [dst_trk])

        Win = sb("Win", [128, 8, INW], BF16); t_Win = Trk()
        for c in range(8):
            load_scaled(Win[:, c, :], t_Win, dr["w_in"][c * 128:(c + 1) * 128, :], INW,
                        scale_col=gmix_c[:, c:c + 1], scale_trk=t_gmix, q=("sp" if c % 2 == 0 else "pool"))
        Wuq = sb("Wuq", [128, 2, 768], BF16); t_Wuq = Trk()
        for c in range(2):
            load_scaled(Wuq[:, c, :], t_Wuq, dr["w_uq"][c * 128:(c + 1) * 128, :], 768,
                        scale_col=gcq_c[:, c:c + 1], scale_trk=t_gcq)
        Wuk = sb("Wuk", [128, 512], BF16); t_Wuk = Trk()
        load_scaled(Wuk[:], t_Wuk, dr["w_uk"][:, :], 512, scale_col=gckv_c[:, 0:1], scale_trk=t_gckv)
        Wuv = sb("Wuv", [128, 512], BF16); t_Wuv = Trk()
        load_scaled(Wuv[:], t_Wuv, dr["w_uv"][:, :], 512, scale_col=gckv_c[:, 0:1], scale_trk=t_gckv)
        W2 = sb("W2", [64, 512], BF16); t_W2 = Trk()
        load_scaled(W2[:], t_W2, dr["rw_w2"][:, :], 512, rows=64)
        A2 = sb("A2", [64, 512], BF16); t_A2 = Trk()
        load_scaled(A2[:], t_A2, dr["rw_a2"][:, :], 512, rows=64)
        G2 = sb("G2", [128, 512], BF16); t_G2 = Trk()
        load_scaled(G2[:], t_G2, dr["rw_g2"][:, :], 512)
        WukT = sb("WukT", [64, 8, 128], BF16); t_WukT = Trk()
        bk, tb = bank()
        bkb = bk[:].bitcast(BF16)
        for h in range(8):
            tr(bkb[0:64, h * 128:(h + 1) * 128], Wuk[:, h * 64:(h + 1) * 64], ident_b[:], [t_Wuk, t_id], [tb], sig=(h == 7))
        cp("dve", WukT[:].rearrange("p h r -> p (h r)"), bkb[0:64, :], [tb], [t_WukT])

        ckvT_all = sb("ckvT_all", [128, S], BF16)
        krT_all = sb("krT_all", [128, S], BF16)
        t_krz = Trk()
        P.op("pool", lambda e: e.memset(krT_all[:, :], 0.0), [], [t_krz])
        ckv_tok = sb("ckv_tok", [128, NT, 128], BF16)
        sc_all = sb("sc_all", [128, NT, 8])
        t_kv = [Trk() for _ in range(NT)]
        for t_ in t_kv:
            t_.w = t_krz.w
        kb_all = sb("kb_all", [128, NT]); t_nb = Trk()
        P.dma("sp", kb_all[:], dr["kbias"][:, :], t_nb, True)
        stTb = sb("stTb", [64, 512], BF16); t_stb = Trk()
        P.op("pool", lambda e: e.memset(stTb[:], 0.0), [], [t_stb])
        hT1 = sb("hT1", [128, 8, 129], BF16); t_hT1 = Trk()
        P.op("pool", lambda e: e.memset(hT1[:].rearrange("p c t -> p (c t)"), 0.0), [], [t_hT1])

        sm = sb("sm", [128, 64]); t_sm = Trk()
        t_smx, t_smkv, t_smks, t_smcq, t_smmla, t_smq, t_smkk, t_smgn = [Trk() for _ in range(8)]
        mla_f = sb("mla_f", [128, 416]); t_mla = Trk()
        feat = sb("feat", [128, 1536]); t_feat = Trk()
        lor = sb("lor", [128, 3, 128]); t_lor = Trk()
        lorb = sb("lorb", [128, 3, 128], BF16); t_lorb = Trk()
        posi = sb("posi", [128, 1], I32); posf = sb("posf", [128, 1]); t_pos = Trk()
        ang = sb("ang", [128, 32]); angi = sb("angi", [128, 32], I32); t_ang = Trk()
        cs4 = sb("cs4", [128, 4, 32]); t_cs4 = [Trk() for _ in range(4)]
        kr = sb("kr", [128, 64]); t_kr = Trk()
        krb = sb("krb", [128, 32], BF16); t_krb = Trk()
        cqn4 = sb("cqn4", [128, 4, 256], BF16); t_cqn4 = [Trk() for _ in range(4)]
        bonus = sb("bonus", [128, 8]); t_bonus = Trk()
        rden = sb("rden", [128, 8]); t_rden = Trk()
        mixr = sb("mixr", [128, 4, 512], BF16); t_mixr = [Trk() for _ in range(4)]

        rw_sig, t_sig = af(0, 512)
        rw_a, t_rwa = af(1, 512)
        rw_g, t_rwg = af(2, 512)
        cur_f, t_cur = af(3, 512); kkn, t_kkn = cur_f, t_cur
        dif_f, t_dif = af(4, 512); kp, t_kp = dif_f, t_dif
        tmp1, t_tmp1 = af(5, 512)
        tmp2, t_tmp2 = af(6, 512)
        Lraw, t_L = af(7, 512); y_f, t_y = Lraw, t_L
        Ebuf, t_E = af(8, 512)
        DCb = []; t_DC = []
        for i in range(2):
            a_, t_ = af(9 + i, 512, rows=64)
            DCb.append(a_); t_DC.append(t_)
        XB = {}; t_XB = {}
        for i, n in enumerate(("a", "b", "k", "r", "v")):
            XB[n], t_XB[n] = ab(11, i * 512, 512)
        XH = {}; t_XH = {}
        for i, n in enumerate(("a", "b", "k", "v")):
            XH[n], t_XH[n] = ab(14, i * 512, 512, rows=64)
        XF = {}; t_XF = {}
        for i, n in enumerate(("a", "b", "k", "r")):
            a_, t_ = ab(16 + i, 0, 1024, rows=64)
            XF[n] = a_.rearrange("p (h t) -> p h t", h=8); t_XF[n] = t_
        yc = [tmp1[0:64, :], tmp2[0:64, :]]; t_yc = [t_tmp1, t_tmp2]
        xt_, t_xt_ = af(20, 1024)
        junk, t_junk = ab(22, 0, 1024)
        hb, t_hb = ab(23, 0, 1024)
        sq, t_sq = af(24, 768)
        ckvn_, t_ckvn_ = ab(26, 0, 128)

        CBNAMES = ("N", "NT", "Arb", "Ark", "AakT", "M", "MT", "M2", "MT2", "T", "T2")
        CBALIAS = {"P1T": "N", "P2T": "NT", "Q1": "M", "Q2": "MT", "G1": "M2", "G2": "MT2"}
        CB = []
        for ch in range(2):
            d = {}
            for i, n in enumerate(CBNAMES):
                idx = ch * 11 + i
                a_, t_ = ab(20 + idx // 2, (idx % 2) * 512, 512, rows=64)
                d[n] = a_.rearrange("p (h t) -> p h t", h=8)
                d["t_" + n] = t_
            for n, o in CBALIAS.items():
                d[n] = d[o]
                d["t_" + n] = d["t_" + o]
            CB.append(d)

        cqT_, t_cqT = ab(0, 0, 256)
        cqT = cqT_.rearrange("p (c t) -> p c t", c=2)
        q_f_, t_q = af(1, 768)
        q_f = q_f_.rearrange("p (h d) -> p h d", h=8)
        sqq, t_sqq = af(3, 768)
        qnb_, t_qnb = ab(5, 0, 512); qnb = qnb_.rearrange("p (h d) -> p h d", h=8)
        qr1_, t_qr1 = af(6, 256); qr1 = qr1_.rearrange("p (h d) -> p h d", h=8)
        qr2_, t_qr2 = af(7, 256); qr2 = qr2_.rearrange("p (h d) -> p h d", h=8)
        qrb_, t_qrb = ab(8, 0, 256); qrb = qrb_.rearrange("p (h d) -> p h d", h=8)
        qnT_, t_qnT = ab(9, 0, 1024, rows=64); qnT = qnT_.rearrange("p (h t) -> p h t", h=8)
        qlatT_, t_qlat = ab(10, 0, 4096); qlatT = qlatT_.rearrange("p (h t) -> p h t", h=8)
        qrT_, t_qrT = ab(14, 0, 4096); qrT = qrT_.rearrange("p (h t) -> p h t", h=8)
        pT = []; t_pT = []
        for i in range(3):
            a_, t_ = ab(18 + i, 0, 512)
            pT.append(a_); t_pT.append(t_)
        den, t_den = af(21, 512)
        oT, t_oT = ab(22, 0, 512)
        pT2 = [[], []]; t_pT2 = [[], []]
        for i_, pgs in enumerate(((18, 19, 20), (0, 1, 2))):
            for p_ in pgs:
                a_, t_ = ab(p_, 0, 512)
                pT2[i_].append(a_); t_pT2[i_].append(t_)
        den3_, t_den3 = af(3, 512)
        den2 = [den, den3_]; t_den2 = [t_den, t_den3]
        oT4_, t_oT4 = ab(4, 0, 512)
        oT2 = [oT, oT4_]; t_oT2 = [t_oT, t_oT4]
        attn_, t_attn = af(23, 2048); attn = attn_.rearrange("p (j d) -> p j d", j=4)
        xr, t_xr = af(0, 1024)
        xm, t_xm = af(2, 1024)
        Woh_, t_Woh = ab(4, 0, 4096); Woh = Woh_.rearrange("p (c n) -> p c n", c=8)
        wst0, t_wst0 = af(8, 512)
        wst1, t_wst1 = af(29, 512)
        mixT_, t_mixT = ab(9, 0, 1024); mixT = mixT_.rearrange("p (c t) -> p c t", c=8)
        mixm_, t_mixm = ab(27, 0, 2048); mixm = mixm_.rearrange("p (j d) -> p j d", j=4)

        h3 = lambda ap: ap.rearrange("p (h d) -> p h d", h=8)

        def phase1(t):
            j = t % 4
            own = t >= OWN0
            xb, txb = xt_, t_xt_
            P.dma("sp", xb, x[t * 128:(t + 1) * 128, :], txb[0], True, extra=txb[1:])
            P.dma("sp", posi[:], dr["pos"][t * 128:(t + 1) * 128].rearrange("(p o) -> p o", o=1), t_pos, True)
            cs = cs4[:, j, :]
            t_cs = t_cs4[j]
            cp("dve", posf[:], posi[:], [t_pos], [t_pos])
            ts("dve", ang[:, 16:32], invf[:], posf[:, 0:1], ALU.mult, [t_pos, t_invf], [t_ang],
               s2=1.0 / (2 * math.pi), op1=ALU.mult)
            ts("dve", ang[:, 0:16], ang[:, 16:32], 0.25, ALU.add, [t_ang], [t_ang])
            cp("dve", angi[:], ang[:], [t_ang], [t_ang])
            cp("dve", cs, angi[:], [t_ang], [t_cs])
            tt("dve", ang[:], ang[:], cs, ALU.subtract, [t_ang, t_cs], [t_ang])
            act(cs, ang[:], AF.Sin, [t_ang], [t_cs], scale=6.2831845)
            act(junk, xb, AF.Square, [txb], [t_junk, t_smx], accum=sm[:, 0:1])
            rsqrt_inplace(sm[:, 0:1], t_smx, 1.0 / D, 1e-6)
            ts("dve", hb, xb, sm[:, 0:1], ALU.mult, [txb, t_smx], [t_hb])
            hcur, thc = hT1, t_hT1
            bk, tb = bank()
            bkb = bk[:].bitcast(BF16)
            for c in range(8):
                tr(bkb[:, c * 128:(c + 1) * 128], hb[:, c * 128:(c + 1) * 128], ident_b[:], [t_hb, t_id], [tb], sig=(c == 7))
            cp("dve", hcur[:, :, 0:1], hcur[:, :, 128:129], [thc], [thc])
            cp("act", hcur[:, :, 1:129], bkb[:, :].rearrange("p (c t) -> p c t", c=8), [tb], [thc])
            bk, tb = bank()
            for c in range(8):
                mm(bk[:, 0:416], hcur[:, c, 1:129], Win[:, c, 0:416], c == 0, c == 7, [thc, t_Win], [tb])
            cp("act", mla_f[:], bk[:, 0:416], [tb], [t_mla])
            for g in range(3):
                if g == 0 and not own:
                    continue
                c0 = 416 + g * 512
                bkc, tbc = bank()
                for c in range(8):
                    mm(bkc[:, :], hcur[:, c, 1:129], Win[:, c, c0:c0 + 512], c == 0, c == 7, [thc, t_Win], [tbc])
                bkp, tbp = bank()
                for c in range(8):
                    mm(bkp[:, :], hcur[:, c, 0:128], Win[:, c, c0:c0 + 512], c == 0, c == 7, [thc, t_Win], [tbp])
                cp("act", cur_f, bkc[:, :], [tbc], [t_cur])
                tt("dve", dif_f, bkp[:, :], cur_f, ALU.subtract, [tbp, t_cur], [t_dif])
                tt("pool", dif_f, dif_f, mu_bc[:, g * 512:(g + 1) * 512], ALU.mult, [t_dif, t_mu], [t_dif])
                tt("pool", feat[:, g * 512:(g + 1) * 512], dif_f, cur_f, ALU.add, [t_dif, t_cur], [t_feat])
            for li, (c0, n) in enumerate(((1952, 64), (2016, 64), (2080, 128))):
                if li == 2 and not own:
                    continue
                bkc, tbc = bank()
                for c in range(8):
                    mm(bkc[0:n, 0:128], Win[:, c, c0:c0 + n], hcur[:, c, 1:129], c == 0, c == 7, [thc, t_Win], [tbc])
                for c in range(8):
                    mm(bkc[0:n, 128:256], Win[:, c, c0:c0 + n], hcur[:, c, 0:128], c == 0, c == 7, [thc, t_Win], [tbc])
                cp("act", lor[0:n, li, :], bkc[0:n, 0:128], [tbc], [t_lor])
                tt("dve", dif_f[0:n, 0:128], bkc[0:n, 128:256], lor[0:n, li, :], ALU.subtract, [tbc, t_lor], [t_dif])
                stt(lor[0:n, li, :], dif_f[0:n, 0:128], mul_c[0:n, li:li + 1], lor[0:n, li, :], ALU.mult, ALU.add,
                    [t_dif, t_mul, t_lor], [t_lor])
            act(lorb[0:64, 0, :], lor[0:64, 0, :], AF.Tanh, [t_lor], [t_lorb])
            cp("dve", lorb[0:64, 1, :], lor[0:64, 1, :], [t_lor], [t_lorb])
            if own:
                act(lorb[:, 2, :], lor[:, 2, :], AF.Sigmoid, [t_lor], [t_lorb])
            bk, tb = bank()
            mm(bk[:, :], lorb[0:64, 0, :], W2[:, :], True, True, [t_lorb, t_W2], [tb])
            tt("dve", rw_sig, bk[:, :], w0_bc[:], ALU.add, [tb, t_w0], [t_sig])
            act(rw_sig, rw_sig, AF.Sigmoid, [t_sig], [t_sig])
            bk, tb = bank()
            mm(bk[:, :], lorb[0:64, 1, :], A2[:, :], True, True, [t_lorb, t_A2], [tb])
            tt("dve", rw_a, bk[:, :], a0_bc[:], ALU.add, [tb, t_a0], [t_rwa])
            act(rw_a, rw_a, AF.Sigmoid, [t_rwa], [t_rwa])
            if own:
                bk, tb = bank()
                mm(bk[:, :], lorb[:, 2, :], G2[:, :], True, True, [t_lorb, t_G2], [tb])
                cp("act", rw_g, bk[:, :], [tb], [t_rwg])

            tk = t_kv[t]
            act(junk[:, 0:128], mla_f[:, 256:384], AF.Square, [t_mla], [t_junk, t_smkv], accum=sm[:, 1:2])
            rsqrt_inplace(sm[:, 1:2], t_smkv, 1.0 / 128, 1e-6)
            ts("dve", ckv_tok[:, t, :], mla_f[:, 256:384], sm[:, 1:2], ALU.mult, [t_mla, t_smkv], [tk])
            bk, tb = bank()
            bkb = bk[:].bitcast(BF16)
            tr(bkb[:, 0:128], ckv_tok[:, t, :], ident_b[:], [tk, t_id], [tb])
            cp("dve", ckvT_all[:, t * 128:(t + 1) * 128], bkb[:, 0:128], [tb], [tk])
            bk, tb = bank()
            mm(bk[:, :], ckvT_all[:, t * 128:(t + 1) * 128], Wuk[:, :], True, True, [tk, t_Wuk], [tb])
            act(sq[:, 0:512], bk[:, :], AF.Square, [tb], [t_sq])
            P.op("dve", lambda e: e.tensor_reduce(out=sm[:, 8:16], in_=h3(sq[:, 0:512]), axis=AX.X, op=ALU.add),
                 [t_sq], [t_smks])
            act(junk[:, 0:32], mla_f[:, 384:416], AF.Square, [t_mla], [t_junk, t_smks], accum=sm[:, 2:3])
            ts("dve", sm[:, 8:16], sm[:, 8:16], sm[:, 2:3], ALU.add, [t_smks], [t_smks])
            rsqrt_inplace(sm[:, 8:16], t_smks, 1.0 / QK, 1e-6)
            ts("dve", sc_all[:, t, :], sm[:, 8:16], QK ** -0.5, ALU.mult, [t_smks], [tk])
            tt("dve", kr[:, 0:32], mla_f[:, 384:416], gkn_bc[:, 64:96], ALU.mult, [t_mla, t_gkn], [t_kr])
            tt("dve", kr[:, 32:48], kr[:, 0:16], cs[:, 0:16], ALU.mult, [t_kr, t_cs], [t_kr])
            tt("dve", kr[:, 48:64], kr[:, 16:32], cs[:, 16:32], ALU.mult, [t_kr, t_cs], [t_kr])
            tt("dve", krb[:, 0:16], kr[:, 32:48], kr[:, 48:64], ALU.subtract, [t_kr], [t_krb])
            tt("dve", kr[:, 32:48], kr[:, 16:32], cs[:, 0:16], ALU.mult, [t_kr, t_cs], [t_kr])
            tt("dve", kr[:, 48:64], kr[:, 0:16], cs[:, 16:32], ALU.mult, [t_kr, t_cs], [t_kr])
            tt("dve", krb[:, 16:32], kr[:, 32:48], kr[:, 48:64], ALU.add, [t_kr], [t_krb])
            bk, tb = bank()
            bkb = bk[:].bitcast(BF16)
            tr(bkb[0:32, 0:128], krb[:], ident_b[:], [t_krb, t_id], [tb])
            cp("dve", krT_all[0:32, t * 128:(t + 1) * 128], bkb[0:32, 0:128], [tb], [tk])
            if t >= OWN0:
                act(junk[:, 0:256], mla_f[:, 0:256], AF.Square, [t_mla], [t_junk, t_smcq], accum=sm[:, 3:4])
                rsqrt_inplace(sm[:, 3:4], t_smcq, 1.0 / 256, 1e-6)
                ts("dve", cqn4[:, j, :], mla_f[:, 0:256], sm[:, 3:4], ALU.mult, [t_mla, t_smcq], [t_cqn4[j]])

        def qside(j):
            cs = cs4[:, j, :]
            t_cs = t_cs4[j]
            bk, tb = bank()
            bkb = bk[:].bitcast(BF16)
            for c in range(2):
                tr(bkb[:, c * 128:(c + 1) * 128], cqn4[:, j, c * 128:(c + 1) * 128], ident_b[:], [t_cqn4[j], t_id], [tb], sig=(c == 1))
            cp("dve", cqT_, bkb[:, 0:256], [tb], [t_cqT])
            bk1, tb1 = bank()
            for c in range(2):
                mm(bk1[:, :], cqT[:, c, :], Wuq[:, c, 0:512], c == 0, c == 1, [t_cqT, t_Wuq], [tb1])
            bk2, tb2 = bank()
            for c in range(2):
                mm(bk2[:, 0:256], cqT[:, c, :], Wuq[:, c, 512:768], c == 0, c == 1, [t_cqT, t_Wuq], [tb2])
            cp("act", q_f_[:, 0:512], bk1[:, :], [tb1], [t_q])
            cp("act", q_f_[:, 512:768], bk2[:, 0:256], [tb2], [t_q])
            act(sqq, q_f_, AF.Square, [t_q], [t_sqq])
            P.op("dve", lambda e: e.tensor_reduce(out=sm[:, 16:24], in_=sqq.rearrange("p (h d) -> p h d", h=8),
                                                  axis=AX.X, op=ALU.add), [t_sqq], [t_sm])
            rsqrt_inplace(sm[:, 16:24], t_sm, 1.0 / QK, 1e-6)
            rq_b64 = sm[:, 16:24].unsqueeze(2).to_broadcast([128, 8, 64])
            rq_b32 = sm[:, 16:24].unsqueeze(2).to_broadcast([128, 8, 32])
            tt("dve", q_f[:, :, 0:64], q_f[:, :, 0:64], rq_b64, ALU.mult, [t_q, t_sm], [t_q])
            tt("dve", qnb, q_f[:, :, 0:64], gqk_bc[:].unsqueeze(1).to_broadcast([128, 8, 64]), ALU.mult,
               [t_q, t_gqk], [t_qnb])
            tt("dve", q_f[:, :, 64:96], q_f[:, :, 64:96], rq_b32, ALU.mult, [t_q, t_sm], [t_q])
            tt("dve", q_f[:, :, 64:96], q_f[:, :, 64:96], gqn_bc[:, 64:96].unsqueeze(1).to_broadcast([128, 8, 32]),
               ALU.mult, [t_q, t_gqn], [t_q])
            cosb = cs[:, 0:16].unsqueeze(1).to_broadcast([128, 8, 16])
            sinb = cs[:, 16:32].unsqueeze(1).to_broadcast([128, 8, 16])
            tt("dve", qr1[:, :, 0:16], q_f[:, :, 64:80], cosb, ALU.mult, [t_q, t_cs], [t_qr1])
            tt("dve", qr1[:, :, 16:32], q_f[:, :, 80:96], sinb, ALU.mult, [t_q, t_cs], [t_qr1])
            tt("dve", qr2[:, :, 0:16], q_f[:, :, 80:96], cosb, ALU.mult, [t_q, t_cs], [t_qr2])
            tt("dve", qr2[:, :, 16:32], q_f[:, :, 64:80], sinb, ALU.mult, [t_q, t_cs], [t_qr2])
            tt("dve", qrb[:, :, 0:16], qr1[:, :, 0:16], qr1[:, :, 16:32], ALU.subtract, [t_qr1], [t_qrb])
            tt("dve", qrb[:, :, 16:32], qr2[:, :, 0:16], qr2[:, :, 16:32], ALU.add, [t_qr2], [t_qrb])
            bk, tb = bank()
            bkb = bk[:].bitcast(BF16)
            for h in range(8):
                tr(bkb[0:64, h * 128:(h + 1) * 128], qnb[:, h, :], ident_b[:], [t_qnb, t_id], [tb], sig=(h == 7))
            cp("dve", qnT_, bkb[0:64, :], [tb], [t_qnT])
            bk, tb = bank()
            bkb = bk[:].bitcast(BF16)
            for h in range(8):
                tr(bkb[0:32, h * 128:(h + 1) * 128], qrb[:, h, :], ident_b[:], [t_qrb, t_id], [tb], sig=(h == 7))
            if j == 0:
                P.op("pool", lambda e: e.memset(qrT_[:, :], 0.0), [], [t_qrT])
            cp("act", qrT[0:32, :, j * 128:(j + 1) * 128], bkb[0:32, :].rearrange("p (h t) -> p h t", h=8),
               [tb], [t_qrT])
            for hh in range(2):
                bk, tb = bank()
                for h4 in range(4):
                    h = hh * 4 + h4
                    mm(bk[:, h4 * 128:(h4 + 1) * 128], WukT[:, h, :], qnT[:, h, :], True, True,
                       [t_WukT, t_qnT], [tb], sig=(h4 == 3))
                cp("act", qlatT[:, hh * 4:(hh + 1) * 4, j * 128:(j + 1) * 128],
                   bk[:, :].rearrange("p (h t) -> p h t", h=4), [tb], [t_qlat])

        def rwkv(t):
            own = t >= OWN0
            r_ = feat[:, 0:512]; k_ = feat[:, 512:1024]; v_ = feat[:, 1024:1536]
            tt("dve", kkn, k_, kk_bc[:], ALU.mult, [t_feat, t_kk], [t_kkn])
            act(tmp1, kkn, AF.Square, [t_kkn], [t_tmp1])
            P.op("dve", lambda e: e.tensor_reduce(out=sm[:, 24:32], in_=h3(tmp1), axis=AX.X, op=ALU.add),
                 [t_tmp1], [t_smkk])
            rsqrt_inplace(sm[:, 24:32], t_smkk, 1.0, 1e-12)
            tt("dve", h3(kkn), h3(kkn), sm[:, 24:32].unsqueeze(2).to_broadcast([128, 8, 64]), ALU.mult,
               [t_kkn, t_smkk], [t_kkn])
            stt(tmp1, rw_a, -1.0, ka_bc[:], ALU.add, ALU.mult, [t_rwa, t_ka], [t_tmp1])
            stt(kp, tmp1, 1.0, k_, ALU.add, ALU.mult, [t_tmp1, t_feat], [t_kp])
            if own:
                tt("pool", tmp2, r_, kp, ALU.mult, [t_feat, t_kp], [t_tmp2])
                tt("pool", tmp2, tmp2, rk_bc[:], ALU.mult, [t_tmp2, t_rk], [t_tmp2])
                P.op("dve", lambda e: e.tensor_reduce(out=bonus[:], in_=h3(tmp2), axis=AX.X, op=ALU.add),
                     [t_tmp2], [t_bonus])
            bk, tb = bank()
            mm(bk[:, :], tri_f[:], rw_sig, True, True, [t_tri, t_sig], [tb])
            cp("act", Lraw, bk[:, :], [tb], [t_L])
            for ch, sel in enumerate((sel63, sel127)):
                bk, tb = bank()
                mm(bk[0:64, :], sel[:], Lraw, True, True, [t_sel, t_L], [tb])
                act(DCb[ch], bk[0:64, :], AF.Exp, [tb], [t_DC[ch]], scale=C0)
            if own:
                act(Ebuf, Lraw, AF.Exp, [t_L], [t_E], scale=C0)
                tt("dve", XB["r"], r_, Ebuf, ALU.mult, [t_feat, t_E], [t_XB["r"]])
            act(Ebuf, Lraw, AF.Exp, [t_L], [t_E], scale=-C0)
            tt("pool", XB["k"], kp, Ebuf, ALU.mult, [t_kp, t_E], [t_XB["k"]])
            tt("dve", tmp1, kkn, rw_a, ALU.mult, [t_kkn, t_rwa], [t_tmp1])
            tt("pool", XB["b"], tmp1, Ebuf, ALU.mult, [t_tmp1, t_E], [t_XB["b"]])
            tt("dve", tmp2, Lraw, rw_sig, ALU.subtract, [t_L, t_sig], [t_tmp2])
            act(Ebuf, tmp2, AF.Exp, [t_tmp2], [t_E], scale=C0)
            stt(XB["a"], kkn, -1.0, Ebuf, ALU.mult, ALU.mult, [t_kkn, t_E], [t_XB["a"]])
            cp("pool", XB["v"], v_, [t_feat], [t_XB["v"]])
            for n in (("a", "b", "k", "r") if own else ("a", "b", "k")):
                bk, tb = bank()
                bkb = bk[:].bitcast(BF16)
                for h in range(8):
                    tr(bkb[0:64, h * 128:(h + 1) * 128], XB[n][:, h * 64:(h + 1) * 64], ident_b[:],
                       [t_XB[n], t_id], [tb], sig=(h == 7))
                cp("act" if n in ("a", "k") else "dve", XF[n], bkb[0:64, :].rearrange("p (h t) -> p h t", h=8),
                   [tb], [t_XF[n]])
            for n in ("a", "b", "k", "v"):
                bk, tb = bank()
                mm(bk[0:64, :], shift_hi[:], XB[n], True, True, [t_sh, t_XB[n]], [tb])
                cp("act" if n in ("a", "k") else "dve", XH[n], bk[0:64, :], [tb], [t_XH[n]])

            def tok(n, ch):
                return (XB[n][0:64, :], t_XB[n]) if ch == 0 else (XH[n], t_XH[n])

            def hmm(lhs, rhs, post):
                bk, tb = bank()
                for h in range(8):
                    l, tl = lhs(h)
                    r, trr = rhs(h)
                    mm(bk[0:64, h * 64:(h + 1) * 64], l, r, True, True, [tl, trr], [tb], sig=(h == 7))
                post(bk[0:64, :].rearrange("p (h t) -> p h t", h=8), tb)

            def FM(n, ch):
                return lambda h: (XF[n][:, h, ch * 64:(ch + 1) * 64], t_XF[n])

            def TM(n, ch):
                ap, trk = tok(n, ch)
                return lambda h: (ap[:, h * 64:(h + 1) * 64], trk)

            def CBm(ch, n):
                return lambda h: (CB[ch][n][:, h, :], CB[ch]["t_" + n])

            est = {"i": 0}

            def nxt():
                est["i"] ^= 1
                return ("dve", "act")[est["i"]]

            def to_masked(ch, n, mask):
                def post(ps, tb):
                    tt("dve", CB[ch][n], ps, mask[:], ALU.mult, [tb, t_msk], [CB[ch]["t_" + n]])
                return post

            def to_plain(ch, n):
                def post(ps, tb):
                    cp(nxt(), CB[ch][n], ps, [tb], [CB[ch]["t_" + n]])
                return post

            for ch in range(2):
                hmm(FM("b", ch), FM("a", ch), to_masked(ch, "N", m_su))
                hmm(FM("a", ch), FM("b", ch), to_masked(ch, "NT", m_sl))
                if own:
                    hmm(FM("b", ch), FM("r", ch), to_masked(ch, "Arb", m_ui))
                    hmm(FM("k", ch), FM("r", ch), to_masked(ch, "Ark", m_ui))
                hmm(FM("a", ch), FM("k", ch), to_masked(ch, "AakT", m_sl))
            for ch in range(2):
                tt("pool", CB[ch]["T"], CB[ch]["N"], id8[:], ALU.add, [CB[ch]["t_N"], t_id8], [CB[ch]["t_T"]])
            for ch in range(2):
                hmm(CBm(ch, "NT"), CBm(ch, "N"), to_plain(ch, "M"))
                hmm(CBm(ch, "N"), CBm(ch, "NT"), to_plain(ch, "MT"))
            cur = ("M", "MT", "T")
            alt = ("M2", "MT2", "T2")
            for jlev in range(1, 6):
                Mn, MTn, Tn = cur
                Mo, MTo, To = alt
                for ch in range(2):
                    def post_T(ps, tb, ch=ch, Tn=Tn, To=To):
                        tt("dve", CB[ch][To], ps, CB[ch][Tn], ALU.add, [tb, CB[ch]["t_" + Tn]],
                           [CB[ch]["t_" + To]])
                    hmm(CBm(ch, MTn), CBm(ch, Tn), post_T)
                    if jlev < 5:
                        hmm(CBm(ch, MTn), CBm(ch, Mn), to_plain(ch, Mo))
                        hmm(CBm(ch, Mn), CBm(ch, MTn), to_plain(ch, MTo))
                cur, alt = (Mo, MTo, To), (Mn, MTn, Tn)
            Tfin = cur[2]
            for ch in range(2):
                hmm(CBm(ch, Tfin), TM("a", ch), to_plain(ch, "P1T"))
                hmm(CBm(ch, Tfin), CBm(ch, "AakT"), to_plain(ch, "P2T"))
            t13 = tmp1[0:64, :].rearrange("p (h t) -> p h t", h=8)
            t23 = tmp2[0:64, :].rearrange("p (h t) -> p h t", h=8)
            for ch in range(2):
                dc3 = DCb[ch].rearrange("p (h t) -> p h t", h=8)

                def post_Q1(ps, tb, ch=ch):
                    tt("dve", CB[ch]["Q1"], ps, XF["r"][:, :, ch * 64:(ch + 1) * 64], ALU.add,
                       [tb, t_XF["r"]], [CB[ch]["t_Q1"]])
                if own:
                    hmm(CBm(ch, "P1T"), CBm(ch, "Arb"), post_Q1)

                def post_G1(ps, tb, ch=ch, dc3=dc3):
                    tt("dve", t13, ps, id8[:], ALU.add, [tb, t_id8], [t_tmp1])
                    tt("pool", CB[ch]["G1"], t13, dc3, ALU.mult, [t_tmp1, t_DC[ch]], [CB[ch]["t_G1"]])
                hmm(CBm(ch, "P1T"), TM("b", ch), post_G1)

                def post_Q2(ps, tb, ch=ch):
                    tt("dve", CB[ch]["Q2"], ps, CB[ch]["Ark"], ALU.add, [tb, CB[ch]["t_Ark"]], [CB[ch]["t_Q2"]])
                if own:
                    hmm(CBm(ch, "P2T"), CBm(ch, "Arb"), post_Q2)

                def post_G2(ps, tb, ch=ch, dc3=dc3):
                    kap, ktrk = tok("k", ch)
                    tt("dve", t23, ps, kap.rearrange("p (h t) -> p h t", h=8), ALU.add, [tb, ktrk], [t_tmp2])
                    tt("pool", CB[ch]["G2"], t23, dc3, ALU.mult, [t_tmp2, t_DC[ch]], [CB[ch]["t_G2"]])
                hmm(CBm(ch, "P2T"), TM("b", ch), post_G2)
            for ch in range(2):
                vap, vtrk = tok("v", ch)
                if own:
                    bky, tby = bank()
                    for h in range(8):
                        hs = slice(h * 64, (h + 1) * 64)
                        mm(bky[0:64, hs], CB[ch]["Q1"][:, h, :], stTb[:, hs], True, False, [CB[ch]["t_Q1"], t_stb], [tby])
                        mm(bky[0:64, hs], CB[ch]["Q2"][:, h, :], vap[:, hs], False, True, [CB[ch]["t_Q2"], vtrk], [tby], sig=(h == 7))
                bks, tbs = bank()
                for h in range(8):
                    hs = slice(h * 64, (h + 1) * 64)
                    mm(bks[0:64, hs], CB[ch]["G1"][:, h, :], stTb[:, hs], True, False, [CB[ch]["t_G1"], t_stb], [tbs])
                    mm(bks[0:64, hs], CB[ch]["G2"][:, h, :], vap[:, hs], False, True, [CB[ch]["t_G2"], vtrk], [tbs], sig=(h == 7))
                if own:
                    cp("act", yc[ch], bky[0:64, :], [tby], [t_yc[ch]])
                cp("dve", stTb[:], bks[0:64, :], [tbs], [t_stb])
            if not own:
                return
            bk, tb = bank()
            mm(bk[:, :], ident_f[0:64, :], yc[0], True, False, [t_id, t_yc[0]], [tb])
            mm(bk[:, :], sel_lo_hi[:], yc[1], False, True, [t_selhi, t_yc[1]], [tb])
            cp("act", y_f, bk[:, :], [tb], [t_y])
            P.op("dve", lambda e: e.tensor_reduce(out=sm[:, 32:40], in_=h3(y_f), axis=AX.X, op=ALU.add),
                 [t_y], [t_sm])
            ts("dve", sm[:, 32:40], sm[:, 32:40], 1.0 / 64, ALU.mult, [t_sm], [t_sm])
            tt("dve", h3(y_f), h3(y_f), sm[:, 32:40].unsqueeze(2).to_broadcast([128, 8, 64]), ALU.subtract,
               [t_y, t_sm], [t_y])
            act(tmp1, y_f, AF.Square, [t_y], [t_tmp1])
            P.op("dve", lambda e: e.tensor_reduce(out=sm[:, 40:48], in_=h3(tmp1), axis=AX.X, op=ALU.add),
                 [t_tmp1], [t_sm])
            rsqrt_inplace(sm[:, 40:48], t_sm, 1.0 / 64, 64e-5)
            tt("dve", h3(y_f), h3(y_f), sm[:, 40:48].unsqueeze(2).to_broadcast([128, 8, 64]), ALU.mult,
               [t_y, t_sm], [t_y])
            tt("pool", y_f, y_f, lng_bc[:], ALU.mult, [t_y, t_lng], [t_y])
            tt("pool", y_f, y_f, lnb_bc[:], ALU.add, [t_y, t_lnb], [t_y])
            tt("dve", h3(tmp2), h3(v_), bonus[:].unsqueeze(2).to_broadcast([128, 8, 64]), ALU.mult,
               [t_feat, t_bonus], [t_tmp2])
            tt("dve", y_f, y_f, tmp2, ALU.add, [t_y, t_tmp2], [t_y])
            j = t % 4
            tt("dve", mixr[:, j, :], y_f, rw_g, ALU.mult, [t_y, t_rwg], [t_mixr[j]])
            if dbg:
                tt("dve", tmp1, y_f, rw_g, ALU.mult, [t_y, t_rwg], [t_tmp1])
                P.dma("sp", dbgo["d_rw"][(t - OWN0) * 128:(t - OWN0 + 1) * 128, :], tmp1, t_tmp1[0], False)

        def attention(B):
            nk = 4 * B + 4
            LA = 2
            bstate["n"] = 6
            bstate["i"] = 0
            for hp in range(4):
                H2 = (2 * hp, 2 * hp + 1)
                bko = [banks[6], banks[7]]
                tbo = [btrk[6], btrk[7]]

                def qk(i, kt):
                    h = H2[i]
                    bks, tbs = bank()
                    ks = slice(kt * 128, (kt + 1) * 128)
                    mm(bks[:, :], ckvT_all[:, ks], qlatT[:, h, :], True, False, [t_kv[kt], t_qlat], [tbs])
                    mm(bks[:, :], krT_all[:, ks], qrT[:, h, :], False, True, [t_kv[kt], t_qrT], [tbs])
                    return bks, tbs
                pend = [[], []]
                for k_ in range(min(LA, nk)):
                    for i in range(2):
                        pend[i].append(qk(i, k_))
                for kt in range(nk):
                    m = kt - 4 * B
                    cur = [pend[i].pop(0) for i in range(2)]
                    if kt + LA < nk:
                        for i in range(2):
                            pend[i].append(qk(i, kt + LA))
                    pts = []
                    for i in range(2):
                        h = H2[i]
                        bks, tbs = cur[i]
                        pt, tpt = pT2[i][kt % 3], t_pT2[i][kt % 3]
                        pts.append((pt, tpt))
                        act(pt, bks[:, :], AF.Exp, [tbs, t_kv[kt], t_nb], [tpt], scale=sc_all[:, kt, h:h + 1],
                            bias=kb_all[:, kt:kt + 1])
                        if m >= 0:
                            P.op("pool", lambda e, pt=pt, m=m: e.affine_select(
                                out=pt, in_=pt, pattern=[[1, 512]], compare_op=ALU.is_ge, fill=0.0,
                                base=-m * 128, channel_multiplier=-1), [tpt], [tpt])
                    for i in range(2):
                        pt, tpt = pts[i]
                        mm(bko[i][:, :], ckv_tok[:, kt, :], pt, kt == 0, kt == nk - 1, [t_kv[kt], tpt], [tbo[i]],
                           sig=(i == 1 and kt + LA >= nk))
                        if kt == 0:
                            cp("dve", den2[i], pt, [tpt], [t_den2[i]])
                        else:
                            tt("dve", den2[i], den2[i], pt, ALU.add, [t_den2[i], tpt], [t_den2[i]])
                for i in range(2):
                    h = H2[i]
                    cp("act", oT2[i], bko[i][:, :], [tbo[i]], [t_oT2[i]])
                    bkf, tbf = bank()
                    for j in range(4):
                        mm(bkf[:, j * 66:j * 66 + 64], oT2[i][:, j * 128:(j + 1) * 128], Wuv[:, h * 64:(h + 1) * 64],
                           True, True, [t_oT2[i], t_Wuv], [tbf], sig=False)
                        mm(bkf[:, j * 66 + 64:j * 66 + 65], den2[i][:, j * 128:(j + 1) * 128], ones_c[:, 0:1], True, True,
                           [t_den2[i], t_ones], [tbf], sig=(j == 3))
                    acc3 = bkf[:, 0:264].rearrange("p (j d) -> p j d", j=4)
                    rd = rden[:, i * 4:(i + 1) * 4]
                    P.op("dve", lambda e, acc3=acc3, rd=rd: e.reciprocal(out=rd.unsqueeze(2), in_=acc3[:, :, 64:65]),
                         [tbf], [t_rden])
                    tt("dve", attn[:, :, h * 64:(h + 1) * 64], acc3[:, :, 0:64],
                       rd.unsqueeze(2).to_broadcast([128, 4, 64]), ALU.mult, [tbf, t_rden], [t_attn])
            for j in range(4):
                act(junk[:, 0:512], attn[:, j, :], AF.Square, [t_attn], [t_junk, t_sm], accum=sm[:, 4 + j:5 + j])
            rsqrt_inplace(sm[:, 4:8], t_sm, 1.0 / 512, 1e-6)
            for j in range(4):
                t = 4 * B + j
                ts("dve", mixm[:, j, :], attn[:, j, :], sm[:, 4 + j:5 + j], ALU.mult, [t_attn, t_sm], [t_mixm])
                if dbg:
                    ts("dve", tmp1, attn[:, j, :], sm[:, 4 + j:5 + j], ALU.mult, [t_attn, t_sm], [t_tmp1])
                    P.dma("sp", dbgo["d_mla"][(t - OWN0) * 128:(t - OWN0 + 1) * 128, :], tmp1, t_tmp1[0], False)
            for half in range(2):
                for c in range(8):
                    wst, t_wst = (wst0, t_wst0) if c % 2 == 0 else (wst1, t_wst1)
                    P.dma("sp", wst, dr["w_o"][c * 128:(c + 1) * 128, half * 512:(half + 1) * 512],
                          t_wst[0], True)
                    if c < 4:
                        ts("dve", Woh[:, c, :], wst, gmo_c[:, c:c + 1], ALU.mult, [t_wst, t_gmo], [t_Woh])
                    else:
                        cp("dve", Woh[:, c, :], wst, [t_wst], [t_Woh])
                for j in range(4):
                    t = 4 * B + j
                    if half == 0:
                        pass
                    bk, tb = bank()
                    bkb = bk[:].bitcast(BF16)
                    for c in range(8):
                        src_ = mixm[:, j, c * 128:(c + 1) * 128] if c < 4 else mixr[:, j, (c - 4) * 128:(c - 3) * 128]
                        tr(bkb[:, c * 128:(c + 1) * 128], src_, ident_b[:], [t_mixm, t_mixr[j], t_id], [tb], sig=(c == 7))
                    cp("act", mixT_, bkb[:, :], [tb], [t_mixT])
                    P.dma("pool", xr[:, 0:512], x[t * 128:(t + 1) * 128, half * 512:(half + 1) * 512], t_xr[0], True)
                    bk, tb = bank()
                    for c in range(8):
                        mm(bk[:, :], mixT[:, c, :], Woh[:, c, :], c == 0, c == 7, [t_mixT, t_Woh], [tb])
                    tt("dve", xm[:, 0:512], bk[:, :], xr[:, 0:512], ALU.add, [tb, t_xr[0]], [t_xm[0]])
                    P.dma("sp", xmid[(t - OWN0) * 128:(t - OWN0 + 1) * 128, half * 512:(half + 1) * 512], xm[:, 0:512], t_xm[0], False)
                    if dbg:
                        P.dma("sp", dbgo["d_xmid"][(t - OWN0) * 128:(t - OWN0 + 1) * 128, half * 512:(half + 1) * 512], xm[:, 0:512],
                              t_xm[0], False)

        def moe_phase():
            TB = min(2048, SO)
            TBT = TB // 128
            NB = SO // TB
            NG = TB // 512
            yacc = sb("yacc", [128, TBT, D]); t_yacc = [Trk() for _ in range(TBT)]
            xT = sb("xT", [128, 8, TB], BF16); t_xT = [Trk() for _ in range(TBT)]
            hn_f = sb("hn_f", [128, D]); t_hnf = Trk()
            hn_b = sb("hn_b", [128, D], BF16); t_hnb = Trk()
            hnT_f = sb("hnT_f", [128, 8, 128]); t_hnT = Trk()
            mjunk = sb("mjunk", [128, D], BF16); t_mj = Trk()
            gffn_bc = sb("gffn_bc", [128, D]); t_gfb = Trk()
            P.dma("sp", gffn_bc[:], dr["g_ffn"].partition_broadcast(128), t_gfb, True)
            bgb = sb("bgb", [128, 36]); t_bgb = Trk()
            P.dma("sp", bgb[:, 0:4], dr["b_group"].partition_broadcast(128), t_bgb, True)
            P.dma("sp", bgb[:, 4:36], dr["b_expert"].partition_broadcast(128), t_bgb, True)
            Wr = sb("Wr", [128, 8, 36]); t_Wr = Trk()
            P.dma("sp", Wr[:, :, 0:4], dr["w_group"].rearrange("(c p) g -> p c g", p=128), t_Wr, True)
            P.dma("sp", Wr[:, :, 4:36], dr["w_expert"].rearrange("(c p) g -> p c g", p=128), t_Wr, True)
            lg = sb("lg", [128, 36]); t_lg = Trk()
            ms = sb("ms", [128, 32]); t_ms = Trk()
            r1 = sb("r1", [128, 32]); r2 = sb("r2", [128, 32]); r3 = sb("r3", [128, 32]); r4 = sb("r4", [128, 32])
            t_r = Trk()
            comb = sb("comb", [128, TBT, NE]); t_comb = [Trk() for _ in range(TBT)]
            wsg = [sb("wsg%d" % i, [128, 8, DE]) for i in range(2)]
            wsu = [sb("wsu%d" % i, [128, 8, DE]) for i in range(2)]
            wsd = [sb("wsd%d" % i, [128, 2, D]) for i in range(2)]
            t_wsg = [Trk(), Trk()]; t_wsu = [Trk(), Trk()]; t_wsd = [Trk(), Trk()]
            wbg = [sb("wbg%d" % i, [128, 8, DE], BF16) for i in range(2)]
            wbu = [sb("wbu%d" % i, [128, 8, DE], BF16) for i in range(2)]
            wbd = [sb("wbd%d" % i, [128, 2, D], BF16) for i in range(2)]
            t_wbg = [Trk(), Trk()]; t_wbu = [Trk(), Trk()]; t_wbd = [Trk(), Trk()]
            sg = [sb("sg%d" % i, [128, 512]) for i in range(2)]; t_sg = [Trk(), Trk()]
            hTb = [sb("hTb%d" % i, [128, 2, 512], BF16) for i in range(2)]; t_hTb = [Trk(), Trk()]
            flat = lambda ap: ap.rearrange("p a b -> p (a b)")
            wq = {"i": 0}

            def load_expert(e):
                i = e % 2
                q1 = "sp" if wq["i"] % 2 == 0 else "pool"
                q2 = "pool" if wq["i"] % 2 == 0 else "sp"
                wq["i"] += 1
                P.dma(q1, wsg[i][:], dr["w_gate"][e].rearrange("(c p) f -> p c f", p=128), t_wsg[i], True)
                P.dma(q2, wsu[i][:], dr["w_up"][e].rearrange("(c p) f -> p c f", p=128), t_wsu[i], True)
                P.dma(q1, wsd[i][:], dr["w_down"][e].rearrange("(c p) n -> p c n", p=128), t_wsd[i], True)
                cp("pool", flat(wbg[i][:]), flat(wsg[i][:]), [t_wsg[i]], [t_wbg[i]])
                cp("pool", flat(wbu[i][:]), flat(wsu[i][:]), [t_wsu[i]], [t_wbu[i]])
                cp("act", flat(wbd[i][:]), flat(wsd[i][:]), [t_wsd[i]], [t_wbd[i]])

            for blk in range(NB):
                for ti in range(TBT):
                    tok0 = blk * TB + ti * 128
                    P.dma("sp" if ti % 2 == 0 else "pool", yacc[:, ti, :], xmid[tok0:tok0 + 128, :], t_yacc[ti], True)
                    act(mjunk[:], yacc[:, ti, :], AF.Square, [t_yacc[ti]], [t_mj, t_ms], accum=ms[:, 0:1])
                    rsqrt_inplace(ms[:, 0:1], t_ms, 1.0 / D, 1e-6)
                    ts("dve", hn_f[:], yacc[:, ti, :], ms[:, 0:1], ALU.mult, [t_yacc[ti], t_ms], [t_hnf])
                    tt("pool", hn_f[:], hn_f[:], gffn_bc[:], ALU.mult, [t_hnf, t_gfb], [t_hnf])
                    cp("act", hn_b[:], hn_f[:], [t_hnf], [t_hnb])
                    bk, tb = bank()
                    bkb = bk[:].bitcast(BF16)
                    for c in range(8):
                        tr(bkb[:, c * 128:(c + 1) * 128], hn_b[:, c * 128:(c + 1) * 128], ident_b[:], [t_hnb, t_id], [tb], sig=(c == 7))
                    cp("act", xT[:, :, ti * 128:(ti + 1) * 128], bkb[:, :].rearrange("p (c t) -> p c t", c=8),
                       [tb], [t_xT[ti]])
                    for hh in range(2):
                        bk, tb = bank()
                        for c4 in range(4):
                            c = hh * 4 + c4
                            tr(bk[:, c4 * 128:(c4 + 1) * 128], hn_f[:, c * 128:(c + 1) * 128], ident_f[:],
                               [t_hnf, t_id], [tb], sig=(c4 == 3))
                        cp("dve", hnT_f[:, hh * 4:(hh + 1) * 4, :], bk[:, :].rearrange("p (c t) -> p c t", c=4),
                           [tb], [t_hnT])
                    bk, tb = bank()
                    for c in range(8):
                        mm(bk[:, 0:36], hnT_f[:, c, :], Wr[:, c, :], c == 0, c == 7, [t_hnT, t_Wr], [tb])
                    tt("dve", lg[:], bk[:, 0:36], bgb[:], ALU.add, [tb, t_bgb], [t_lg])
                    P.op("dve", lambda e: e.tensor_reduce(out=ms[:, 1:2], in_=lg[:, 0:4], axis=AX.X, op=ALU.max),
                         [t_lg], [t_ms])
                    ts("dve", r1[:, 0:4], lg[:, 0:4], ms[:, 1:2], ALU.is_equal, [t_lg, t_ms], [t_r])
                    ts("dve", ms[:, 2:3], ms[:, 1:2], -1.0, ALU.mult, [t_ms], [t_ms])
                    act(r2[:, 0:4], lg[:, 0:4], AF.Exp, [t_lg, t_ms], [t_r, t_ms], bias=ms[:, 2:3], accum=ms[:, 3:4])
                    P.op("dve", lambda e: e.reciprocal(out=ms[:, 3:4], in_=ms[:, 3:4]), [t_ms], [t_ms])
                    ts("dve", r1[:, 4:8], r1[:, 0:4], -1.0, ALU.add, [t_r], [t_r], s2=1e30, op1=ALU.mult)
                    tt("dve", r3[:].rearrange("p (g e) -> p g e", g=4), lg[:, 4:36].rearrange("p (g e) -> p g e", g=4),
                       r1[:, 4:8].unsqueeze(2).to_broadcast([128, 4, 8]), ALU.add, [t_lg, t_r], [t_r])
                    P.op("dve", lambda e: e.tensor_reduce(out=ms[:, 4:5], in_=r3[:], axis=AX.X, op=ALU.max),
                         [t_r], [t_ms])
                    ts("dve", r2[:], r3[:], ms[:, 4:5], ALU.is_equal, [t_r, t_ms], [t_r])
                    stt(r4[:], r2[:], -1e30, r3[:], ALU.mult, ALU.add, [t_r], [t_r])
                    P.op("dve", lambda e: e.tensor_reduce(out=ms[:, 5:6], in_=r4[:], axis=AX.X, op=ALU.max),
                         [t_r], [t_ms])
                    ts("dve", r3[:], r4[:], ms[:, 5:6], ALU.is_equal, [t_r, t_ms], [t_r])
                    tt("dve", ms[:, 6:7], ms[:, 5:6], ms[:, 4:5], ALU.subtract, [t_ms], [t_ms])
                    act(ms[:, 6:7], ms[:, 6:7], AF.Exp, [t_ms], [t_ms])
                    ts("dve", ms[:, 7:8], ms[:, 6:7], 1.0, ALU.add, [t_ms], [t_ms])
                    P.op("dve", lambda e: e.reciprocal(out=ms[:, 7:8], in_=ms[:, 7:8]), [t_ms], [t_ms])
                    tt("dve", ms[:, 8:9], ms[:, 6:7], ms[:, 7:8], ALU.mult, [t_ms], [t_ms])
                    tt("dve", ms[:, 7:8], ms[:, 7:8], ms[:, 3:4], ALU.mult, [t_ms], [t_ms])
                    tt("dve", ms[:, 8:9], ms[:, 8:9], ms[:, 3:4], ALU.mult, [t_ms], [t_ms])
                    ts("dve", r4[:], r2[:], ms[:, 7:8], ALU.mult, [t_r, t_ms], [t_r])
                    stt(comb[:, ti, :], r3[:], ms[:, 8:9], r4[:], ALU.mult, ALU.add, [t_r, t_ms], [t_comb[ti]])
                for e in range(NE):
                    i = e % 2
                    load_expert(e)
                    for grp in range(NG):
                        gs = slice(grp * 512, (grp + 1) * 512)
                        xtr = [t_xT[grp * 4 + j] for j in range(4)]
                        hb_, thb_ = hTb[grp % 2], t_hTb[grp % 2]
                        for fc in range(2):
                            bkg, tbg = bank()
                            for c in range(8):
                                mm(bkg[:, :], wbg[i][:, c, fc * 128:(fc + 1) * 128], xT[:, c, gs], c == 0, c == 7,
                                   [t_wbg[i], xtr], [tbg])
                            bku, tbu = bank()
                            for c in range(8):
                                mm(bku[:, :], wbu[i][:, c, fc * 128:(fc + 1) * 128], xT[:, c, gs], c == 0, c == 7,
                                   [t_wbu[i], xtr], [tbu])
                            act(sg[fc][:], bkg[:, :], AF.Silu, [tbg], [t_sg[fc]])
                            tt("dve", hb_[:, fc, :], bku[:, :], sg[fc][:], ALU.mult, [tbu, t_sg[fc]], [thb_])
                        for j in range(4):
                            ti = grp * 4 + j
                            for half in range(2):
                                bk, tb = bank()
                                for fc in range(2):
                                    mm(bk[:, :], hb_[:, fc, j * 128:(j + 1) * 128],
                                       wbd[i][:, fc, half * 512:(half + 1) * 512], fc == 0, fc == 1,
                                       [thb_, t_wbd[i]], [tb])
                                ysl = yacc[:, ti, half * 512:(half + 1) * 512]
                                stt(ysl, bk[:, :], comb[:, ti, e:e + 1], ysl, ALU.mult, ALU.add,
                                    [tb, t_comb[ti], t_yacc[ti]], [t_yacc[ti]])
                for ti in range(TBT):
                    tok0 = blk * TB + ti * 128
                    P.dma("sp" if ti % 2 == 0 else "pool", out[tok0:tok0 + 128, :], yacc[:, ti, :], t_yacc[ti], False)

        P.barrier()
        for t in range(NT):
            phase1(t)
            rwkv(t)
            if t % 4 == 3 and t >= OWN0:
                for j in range(4):
                    qside(j)
                attention(t // 4)
                bstate["n"] = 8
        P.barrier()
        mes.close()
        cures["es"] = es
        moe_phase()
        P.final_wait("sp")
        with nc.Block() as block:
            P.emit(block)
    return nc


_CACHE = {}


def kernel(**inputs):
    NT = 64
    if NT not in _CACHE:
        _CACHE[NT] = build(NT)
    nc = _CACHE[NT]
    x = np.asarray(inputs["x"], dtype=np.float32)
    pos = np.asarray(inputs["positions"], dtype=np.int32)
    w = {}
    for n, shp in WNAMES:
        w[n] = np.ascontiguousarray(np.asarray(inputs[n], dtype=np.float32).reshape(shp))
    H = 4096
    in_maps = []
    for c in range(8):
        b, p = c // 2, c % 2
        kb = np.full((128, NT), -8.0, np.float32)
        if p == 1:
            xc = x[b]
            pc = pos[b]
        else:
            xc = np.concatenate([np.zeros((H, D), np.float32), x[b, :H]], axis=0)
            pc = np.concatenate([np.zeros((H,), np.int32), pos[b, :H]], axis=0)
            kb[:, :NT // 2] = -30000.0
        m = {"x": np.ascontiguousarray(xc), "pos": np.ascontiguousarray(pc), "kbias": kb}
        m.update(w)
        in_maps.append(m)
    res = run_bass_kernel_spmd(nc, in_maps, core_ids=list(range(8)))
    out = np.empty((4, 2 * H, D), np.float32)
    for c in range(8):
        b, p = c // 2, c % 2
        out[b, p * H:(p + 1) * H] = res.results[c]["out"]
    return out
```
